# Optimizing a Trainium2 kernel written in Bass

```python
import math
import jax, jax.numpy as jnp
from jax import lax
import numpy as np

D_MODEL = 1024
BATCH = 16
SEQ = 2048
DEPTH = 1

N_ATTN_HEADS = 8
N_KV_HEADS = 2
HEAD_DIM = 64
ATTN_WIDTH = N_ATTN_HEADS * HEAD_DIM
N_IDX_HEADS = 4
IDX_DIM = 64
TOPK_MAX = 256
Q_BLOCK = 128
NUM_BUCKETS = 32
MAX_DISTANCE = 128
SSD_D_INNER = D_MODEL // 2
SSD_HEAD_DIM = 64
SSD_N_HEADS = SSD_D_INNER // SSD_HEAD_DIM
SSD_N_GROUPS = 2
SSD_D_STATE = 128
CONV_WIDTH = 4
CHUNK = 128
CONV_DIM = SSD_D_INNER + 2 * SSD_N_GROUPS * SSD_D_STATE
D_FF = 4 * D_MODEL
MIX_WIDTH = ATTN_WIDTH + SSD_D_INNER
EPS = 1e-6

IN_SPLIT_SIZES = (ATTN_WIDTH, N_KV_HEADS * HEAD_DIM, N_KV_HEADS * HEAD_DIM,
                  N_IDX_HEADS * IDX_DIM, IDX_DIM, N_IDX_HEADS,
                  SSD_D_INNER, CONV_DIM, SSD_N_HEADS)
IN_PROJ_DIM = (ATTN_WIDTH + 4 * N_KV_HEADS * HEAD_DIM // 2 + N_IDX_HEADS * IDX_DIM
               + IDX_DIM + N_IDX_HEADS + SSD_D_INNER + CONV_DIM + SSD_N_HEADS)

kernel_name = "hymba_dsa_ssd_hybrid_layer"


def rms_norm(x, w):
    xf = x.astype(jnp.float32)
    y = xf * lax.rsqrt(jnp.mean(xf * xf, axis=-1, keepdims=True) + EPS)
    return (y * w.astype(jnp.float32)).astype(x.dtype)


def layer_norm(x, w, b):
    xf = x.astype(jnp.float32)
    mu = jnp.mean(xf, axis=-1, keepdims=True)
    var = jnp.mean(jnp.square(xf - mu), axis=-1, keepdims=True)
    y = (xf - mu) * lax.rsqrt(var + EPS)
    return (y * w.astype(jnp.float32) + b.astype(jnp.float32)).astype(x.dtype)


def t5_bucket(dist):
    max_exact = NUM_BUCKETS // 2
    is_small = dist < max_exact
    df = jnp.maximum(dist, 1).astype(jnp.float32)
    large = max_exact + (jnp.log(df / max_exact) / math.log(MAX_DISTANCE / max_exact)
                         * (NUM_BUCKETS - max_exact)).astype(jnp.int32)
    large = jnp.minimum(large, NUM_BUCKETS - 1)
    return jnp.where(is_small, dist, large)


def sparse_attention(q, k, v, q_idx, k_idx, w_idx, rel_bias):
    Bn, L = q.shape[0], q.shape[1]
    n_sel = min(TOPK_MAX, L // 4)
    n_blocks = L // Q_BLOCK
    rep = N_ATTN_HEADS // N_KV_HEADS
    key_pos = jnp.arange(L, dtype=jnp.int32)
    k_idx_f = k_idx.astype(jnp.float32)

    def block(i):
        t0 = i * Q_BLOCK
        qb = lax.dynamic_slice_in_dim(q, t0, Q_BLOCK, axis=1)
        qib = lax.dynamic_slice_in_dim(q_idx, t0, Q_BLOCK, axis=1)
        wb = lax.dynamic_slice_in_dim(w_idx, t0, Q_BLOCK, axis=1)
        q_pos = t0 + jnp.arange(Q_BLOCK, dtype=jnp.int32)
        dots = jnp.einsum("bthd,bsd->bths", qib.astype(jnp.float32), k_idx_f) * (IDX_DIM ** -0.5)
        score = jnp.einsum("bth,bths->bts", wb.astype(jnp.float32), jax.nn.relu(dots))
        causal = key_pos[None, :] <= q_pos[:, None]
        score = jnp.where(causal[None], score, -jnp.inf)
        _, idx = lax.top_k(score, n_sel)
        k_sel = jax.vmap(lambda kb, ib: kb[ib])(k, idx)
        v_sel = jax.vmap(lambda vb, ib: vb[ib])(v, idx)
        qg = qb.reshape(Bn, Q_BLOCK, N_KV_HEADS, rep, HEAD_DIM)
        logits = jnp.einsum("btgrd,btkgd->btgrk", qg, k_sel).astype(jnp.float32) * (HEAD_DIM ** -0.5)
        dist = q_pos[None, :, None] - idx
        valid = dist >= 0
        bias = rel_bias[t5_bucket(jnp.maximum(dist, 0))]
        bias = bias.reshape(Bn, Q_BLOCK, n_sel, N_KV_HEADS, rep).transpose(0, 1, 3, 4, 2)
        logits = jnp.where(valid[:, :, None, None, :], logits + bias.astype(jnp.float32), -jnp.inf)
        p = jax.nn.softmax(logits, axis=-1).astype(v.dtype)
        o = jnp.einsum("btgrk,btkgd->btgrd", p, v_sel)
        return o.reshape(Bn, Q_BLOCK, ATTN_WIDTH)

    out = lax.map(block, jnp.arange(n_blocks, dtype=jnp.int32))
    return out.transpose(1, 0, 2, 3).reshape(Bn, L, ATTN_WIDTH)


def segsum(a):
    T = a.shape[-1]
    cs = jnp.cumsum(a, axis=-1)
    diff = cs[..., :, None] - cs[..., None, :]
    mask = jnp.tril(jnp.ones((T, T), dtype=bool))
    return jnp.where(mask, diff, -jnp.inf)


def ssd_mixer(z, xbc, dt, conv_w, conv_b, dt_bias, a_log, d_skip, norm_w):
    Bn, L, _ = xbc.shape
    out_dtype = z.dtype
    xbc = lax.conv_general_dilated(
        xbc, conv_w[:, None, :].astype(xbc.dtype), window_strides=(1,),
        padding=[(CONV_WIDTH - 1, 0)], dimension_numbers=("NWC", "WIO", "NWC"),
        feature_group_count=CONV_DIM) + conv_b
    xbc = jax.nn.silu(xbc).astype(jnp.float32)
    xs = xbc[..., :SSD_D_INNER].reshape(Bn, L, SSD_N_HEADS, SSD_HEAD_DIM)
    gn = SSD_N_GROUPS * SSD_D_STATE
    Bm = xbc[..., SSD_D_INNER:SSD_D_INNER + gn].reshape(Bn, L, SSD_N_GROUPS, SSD_D_STATE)
    Cm = xbc[..., SSD_D_INNER + gn:].reshape(Bn, L, SSD_N_GROUPS, SSD_D_STATE)
    heads_per_group = SSD_N_HEADS // SSD_N_GROUPS
    Bh = jnp.repeat(Bm, heads_per_group, axis=2)
    Ch = jnp.repeat(Cm, heads_per_group, axis=2)
    dt = jax.nn.softplus(dt.astype(jnp.float32) + dt_bias.astype(jnp.float32))
    A = -jnp.exp(a_log.astype(jnp.float32))
    nc = L // CHUNK
    X = (xs * dt[..., None]).reshape(Bn, nc, CHUNK, SSD_N_HEADS, SSD_HEAD_DIM)
    Adt = (dt * A).reshape(Bn, nc, CHUNK, SSD_N_HEADS).transpose(0, 3, 1, 2)
    Bc = Bh.reshape(Bn, nc, CHUNK, SSD_N_HEADS, SSD_D_STATE)
    Cc = Ch.reshape(Bn, nc, CHUNK, SSD_N_HEADS, SSD_D_STATE)
    A_cs = jnp.cumsum(Adt, axis=-1)
    Lmat = jnp.exp(segsum(Adt))
    cb = jnp.einsum("bclhn,bcshn->bhcls", Cc, Bc) * Lmat
    y_diag = jnp.einsum("bhcls,bcshp->bclhp", cb, X)
    decay_states = jnp.exp(A_cs[..., -1:] - A_cs)
    states = jnp.einsum("bclhn,bhcl,bclhp->bchpn", Bc, decay_states, X)
    states = jnp.concatenate([jnp.zeros_like(states[:, :1]), states], axis=1)
    chunk_decay = jnp.exp(segsum(jnp.pad(A_cs[..., -1], ((0, 0), (0, 0), (1, 0)))))
    states = jnp.einsum("bhzc,bchpn->bzhpn", chunk_decay, states)[:, :-1]
    y_off = jnp.einsum("bclhn,bchpn,bhcl->bclhp", Cc, states, jnp.exp(A_cs))
    y = (y_diag + y_off).reshape(Bn, L, SSD_N_HEADS, SSD_HEAD_DIM)
    y = y + xs * d_skip.astype(jnp.float32)[:, None]
    y = y.reshape(Bn, L, SSD_D_INNER) * jax.nn.silu(z.astype(jnp.float32))
    yg = y.reshape(Bn, L, SSD_N_GROUPS, SSD_D_INNER // SSD_N_GROUPS)
    yg = yg * lax.rsqrt(jnp.mean(yg * yg, axis=-1, keepdims=True) + EPS)
    y = yg.reshape(Bn, L, SSD_D_INNER) * norm_w.astype(jnp.float32)
    return y.astype(out_dtype)


def setup_inputs(seed: int = 0) -> dict:
    key = jax.random.key(seed)
    ks = jax.random.split(key, 20)
    f32 = jnp.float32

    def gain(k, n):
        return 1.0 + 0.05 * jax.random.normal(k, (DEPTH, n), f32)

    dt0 = jnp.exp(jax.random.uniform(ks[9], (DEPTH, SSD_N_HEADS), f32)
                  * (math.log(0.1) - math.log(0.001)) + math.log(0.001))
    return {
        "x": jax.random.normal(ks[0], (BATCH, SEQ, D_MODEL), f32),
        "norm_pre_mix": gain(ks[1], D_MODEL),
        "norm_post_mix": gain(ks[2], D_MODEL),
        "norm_pre_mlp": gain(ks[3], D_MODEL),
        "norm_post_mlp": gain(ks[4], D_MODEL),
        "w_in": jax.random.normal(ks[5], (DEPTH, D_MODEL, IN_PROJ_DIM), f32) * D_MODEL ** -0.5,
        "k_idx_ln_w": gain(ks[6], IDX_DIM),
        "k_idx_ln_b": 0.02 * jax.random.normal(ks[7], (DEPTH, IDX_DIM), f32),
        "conv_w": jax.random.normal(ks[8], (DEPTH, CONV_WIDTH, CONV_DIM), f32) * CONV_WIDTH ** -0.5,
        "conv_b": 0.02 * jax.random.normal(ks[10], (DEPTH, CONV_DIM), f32),
        "dt_bias": dt0 + jnp.log(-jnp.expm1(-dt0)),
        "a_log": jnp.log(jax.random.uniform(ks[11], (DEPTH, SSD_N_HEADS), f32, 1.0, 16.0)),
        "d_skip": 1.0 + 0.1 * jax.random.normal(ks[12], (DEPTH, SSD_N_HEADS), f32),
        "ssd_norm_w": gain(ks[13], SSD_D_INNER),
        "w_out": jax.random.normal(ks[14], (DEPTH, MIX_WIDTH, D_MODEL), f32) * MIX_WIDTH ** -0.5,
        "w_mlp_up": jax.random.normal(ks[15], (DEPTH, D_MODEL, D_FF), f32) * D_MODEL ** -0.5,
        "w_mlp_down": jax.random.normal(ks[16], (DEPTH, D_FF, D_MODEL), f32) * D_FF ** -0.5,
        "rel_bias": 0.5 * jax.random.normal(ks[17], (NUM_BUCKETS, N_ATTN_HEADS), f32),
    }


def reference(x, norm_pre_mix, norm_post_mix, norm_pre_mlp, norm_post_mlp, w_in,
              k_idx_ln_w, k_idx_ln_b, conv_w, conv_b, dt_bias, a_log, d_skip,
              ssd_norm_w, w_out, w_mlp_up, w_mlp_down, rel_bias):
    Bn, L, _ = x.shape
    offsets = [int(o) for o in np.cumsum(IN_SPLIT_SIZES)[:-1]]
    h = x
    for i in range(DEPTH):
        u = rms_norm(h, norm_pre_mix[i])
        proj = u @ w_in[i]
        q, k, v, qi, ki, wi, z, xbc, dt = jnp.split(proj, offsets, axis=-1)
        q = q.reshape(Bn, L, N_ATTN_HEADS, HEAD_DIM)
        k = k.reshape(Bn, L, N_KV_HEADS, HEAD_DIM)
        v = v.reshape(Bn, L, N_KV_HEADS, HEAD_DIM)
        qi = qi.reshape(Bn, L, N_IDX_HEADS, IDX_DIM)
        ki = layer_norm(ki, k_idx_ln_w[i], k_idx_ln_b[i])
        wi = wi * (N_IDX_HEADS ** -0.5)
        attn = sparse_attention(q, k, v, qi, ki, wi, rel_bias)
        ssd = ssd_mixer(z, xbc, dt, conv_w[i], conv_b[i], dt_bias[i], a_log[i],
                        d_skip[i], ssd_norm_w[i])
        mix = jnp.concatenate([attn, ssd], axis=-1) @ w_out[i]
        h = h + rms_norm(mix, norm_post_mix[i])
        f = rms_norm(h, norm_pre_mlp[i]) @ w_mlp_up[i]
        f = jnp.square(jax.nn.relu(f)) @ w_mlp_down[i]
        h = h + rms_norm(f, norm_post_mlp[i])
    return h
```

```python
import numpy as np
from contextlib import ExitStack
import concourse.bass as bass
import concourse.mybir as mybir
from concourse.bass_utils import run_bass_kernel_spmd

F32 = mybir.dt.float32
BF16 = mybir.dt.bfloat16
AF = mybir.ActivationFunctionType
ALU = mybir.AluOpType
AX = mybir.AxisListType

D_MODEL = 1024
L_FULL = 2048
N_CORES = 8
EPS = 1e-6
NEG_BIG = -1.0e30

OFF_Q, OFF_K, OFF_V, OFF_QI, OFF_KI, OFF_WI, OFF_Z, OFF_XBC, OFF_DT = (
    0, 512, 640, 768, 1024, 1088, 1092, 1604, 2628)
N_FM = 17
C_TM1 = N_FM * 128
C_Z = C_TM1 + 140
W_IN_COLS = C_Z + 512


def _w_in_perm():
    cols = []
    for p in range(4):
        g, j = p // 2, p % 2
        for h in (4 * g + j, 4 * g + 2 + j):
            cols += list(range(OFF_Q + 64 * h, OFF_Q + 64 * h + 64))
    for g in range(2):
        for _ in range(2):
            cols += list(range(OFF_K + 64 * g, OFF_K + 64 * g + 64))
    for p in range(2):
        for h in (2 * p, 2 * p + 1):
            cols += list(range(OFF_QI + 64 * h, OFF_QI + 64 * h + 64))
    for _ in range(2):
        cols += list(range(OFF_KI, OFF_KI + 64))
    cols += list(range(OFF_XBC, OFF_XBC + 1024))
    cols += list(range(OFF_V, OFF_V + 128))
    cols += list(range(OFF_WI, OFF_WI + 4))
    cols += list(range(OFF_DT, OFF_DT + 8))
    cols += list(range(OFF_Z, OFF_Z + 512))
    assert len(cols) == W_IN_COLS
    return np.array(cols, dtype=np.int64)


def _t5_bucket_np(d):
    d = np.asarray(d)
    max_exact = 16
    df = np.maximum(d, 1).astype(np.float32)
    large = max_exact + (np.log(df / max_exact) / np.float32(np.log(128 / max_exact))
                         * (32 - max_exact)).astype(np.int32)
    large = np.minimum(large, 31)
    return np.where(d < max_exact, d, large)


CO_IDENT = 0
CO_U = 128
CO_NEGTRI = 256
CO_NEGL = 384
CO_BD64 = 512
CO_PERT = 640
CO_OH = 640 + 2048
CO_ONES = CO_OH + 384
CO_NU = CO_ONES + 128
CO_N = CO_NU + 128


def _consts():
    c = np.zeros((128, CO_N), np.float32)
    idx = np.arange(128)
    c[:, CO_IDENT:CO_IDENT + 128] = np.eye(128, dtype=np.float32)
    c[:, CO_U:CO_U + 128] = (idx[:, None] <= idx[None, :]).astype(np.float32)
    c[:, CO_NEGTRI:CO_NEGTRI + 128] = np.where(idx[None, :] > idx[:, None], NEG_BIG, 0.0)
    c[:, CO_NEGL:CO_NEGL + 128] = np.where(idx[None, :] < idx[:, None], -30000.0, 0.0)
    bd = np.zeros((128, 128), np.float32)
    bd[:64, :64] = 1.0 / 64
    bd[64:, 64:] = 1.0 / 64
    c[:, CO_BD64:CO_BD64 + 128] = bd
    c[:, CO_PERT:CO_PERT + 2048] = (-(2.0 ** -23) * (np.arange(2048) + 1)).astype(np.float32)[None, :]
    m = np.arange(383)
    b = _t5_bucket_np(np.maximum(m - 127, 0))
    oh = np.zeros((32, 384), np.float32)
    oh[b, m] = 1.0
    c[:32, CO_OH:CO_OH + 384] = oh
    c[:, CO_ONES:CO_ONES + 128] = 1.0
    c[:, CO_NU:CO_NU + 128] = np.eye(128, dtype=np.float32) - bd
    return c


class _Op:
    __slots__ = ("eng", "fn", "deps", "flag", "is_dma", "key", "tick")

    def __init__(self, eng, fn, is_dma, key):
        self.eng = eng
        self.fn = fn
        self.deps = []
        self.flag = False
        self.is_dma = is_dma
        self.key = key
        self.tick = 0


class Prog:
    ENGS = ("pe", "act", "dve", "pool", "sp")
    EPOCH = 12000

    def __init__(self, nc):
        self.nc = nc
        self.q = {e: [] for e in self.ENGS}
        self.last_w = {}
        self.readers = {}
        self.dma_ops = []
        self.bg = {}
        self.bg_rate = {"dve": 6, "act": 3}

    def bg_push(self, eng, fn, reads=(), writes=()):
        self.bg.setdefault(eng, []).append((fn, reads, writes))

    def bg_flush(self, eng=None, n=None):
        for e in ([eng] if eng else list(self.bg.keys())):
            q = self.bg.get(e, [])
            k = 0
            while q and (n is None or k < n):
                fn, reads, writes = q.pop(0)
                self._add(e, fn, reads, writes, None)
                k += 1

    def add(self, eng, fn, reads=(), writes=(), dma_key=None):
        if self.bg.get(eng):
            self.bg_flush(eng, self.bg_rate.get(eng, 2))
        return self._add(eng, fn, reads, writes, dma_key)

    def _add(self, eng, fn, reads=(), writes=(), dma_key=None):
        is_dma = dma_key is not None
        op = _Op(eng, fn, is_dma, dma_key)
        cand = {}
        for t in reads:
            w = self.last_w.get(t)
            if w is not None:
                cand[id(w)] = (w, True)
        for t in writes:
            w = self.last_w.get(t)
            if w is not None and id(w) not in cand:
                cand[id(w)] = (w, False)
            for r in self.readers.get(t, {}).values():
                if id(r) not in cand:
                    cand[id(r)] = (r, False)
        for d, raw in cand.values():
            if d is op:
                continue
            keep = True
            if not d.is_dma and d.eng == eng:
                keep = (eng in ("act", "dve", "pool")) or (is_dma and eng != "sp")
            if keep:
                d.flag = True
                op.deps.append(d)
        rk = ("dma", id(op)) if is_dma else eng
        for t in reads:
            self.readers.setdefault(t, {})[rk] = op
        for t in writes:
            self.last_w[t] = op
            self.readers[t] = {}
        self.q[eng].append(op)
        if is_dma:
            self.dma_ops.append(op)
        return op

    def emit(self, es, final_wait_keys=()):
        nc = self.nc
        n_epochs = {}
        for e in self.ENGS:
            cnt = 0
            for op in self.q[e]:
                if op.is_dma:
                    continue
                if op.flag:
                    cnt += 1
                    op.tick = cnt
            n_epochs[e] = (cnt + self.EPOCH - 1) // self.EPOCH
        dma_cnt = {}
        for e in self.ENGS:
            for op in self.q[e]:
                if op.is_dma:
                    dma_cnt[op.key] = dma_cnt.get(op.key, 0) + 1
                    op.tick = dma_cnt[op.key] * 16
        sems = {}
        for e in self.ENGS:
            for k in range(n_epochs[e]):
                sems[(e, k)] = es.enter_context(nc.semaphore(f"s_{e}_{k}"))
        for k in dma_cnt:
            sems[("dma", k)] = es.enter_context(nc.semaphore(f"d_{k}"))

        def ev(op):
            if op.is_dma:
                return sems[("dma", op.key)], op.tick
            k = (op.tick - 1) // self.EPOCH
            return sems[(op.eng, k)], op.tick - k * self.EPOCH

        block = es.enter_context(nc.Block())

        def run(eng_name, eng):
            waited = {}
            for op in self.q[eng_name]:
                need = {}
                for d in op.deps:
                    s, v = ev(d)
                    if need.get(s.num, (None, 0))[1] < v:
                        need[s.num] = (s, v)
                for sn, (s, v) in need.items():
                    if waited.get(sn, 0) < v:
                        eng.wait_ge(s, v)
                        waited[sn] = v
                ins = op.fn(eng)
                if op.is_dma:
                    s, _ = ev(op)
                    ins.then_inc(s, 16)
                elif op.flag:
                    s, _ = ev(op)
                    ins.then_inc(s, 1)
            if eng_name == "sp":
                for k in final_wait_keys:
                    if k in dma_cnt:
                        eng.wait_ge(sems[("dma", k)], dma_cnt[k] * 16)

        @block.tensor
        def _(e):
            run("pe", e)

        @block.scalar
        def _(e):
            run("act", e)

        @block.vector
        def _(e):
            run("dve", e)

        @block.gpsimd
        def _(e):
            run("pool", e)

        @block.sync
        def _(e):
            run("sp", e)


I8 = mybir.dt.int8
_DT_SIZE = {F32: 4, BF16: 2, I8: 1}


def build(nseq=2, nt=16, dbg=None, stop_after=None, act_from=12):
    ACT_FROM = act_from
    L = nt * 128
    assert nt % 4 == 0
    topk = min(256, L // 4)
    ntok = nseq * L
    nc = bass.Bass("TRN2", target_bir_lowering=False)
    es = ExitStack()
    P = Prog(nc)

    def dram_in(name, shape, dt=F32):
        return nc.dram_tensor(name, list(shape), dt, kind="ExternalInput").ap()

    x_d = dram_in("x", [ntok, D_MODEL])
    win_d = dram_in("w_in", [D_MODEL, W_IN_COLS])
    wout_d = dram_in("w_out", [1024, 1024])
    wup_d = dram_in("w_up", [1024, 4096])
    wdn_d = dram_in("w_down", [4096, 1024])
    consts_d = dram_in("consts", [128, CO_N])
    pp_d = dram_in("pp", [128, PP_N])
    rows_d = dram_in("rows", [1, ROWS_N])
    gpost_d = dram_in("gpost", [2, 1024])
    convb_d = dram_in("convb", [1, 1024])
    relb_d = dram_in("rel_bias", [32, 8])
    out_d = nc.dram_tensor("out", [ntok, D_MODEL], F32, kind="ExternalOutput").ap()

    wi_bf = nc.dram_tensor("wi_bf", [128, 8, W_IN_COLS], BF16).ap()
    wo_bf = nc.dram_tensor("wo_bf", [128, 8, 1024], BF16).ap()
    wu_bf = nc.dram_tensor("wu_bf", [128, 8, 4096], BF16).ap()
    wd_bf = nc.dram_tensor("wd_bf", [128, 32, 1024], BF16).ap()
    E_d = nc.dram_tensor("E_d", [8, 128 * 384], F32)

    AW = 53200
    arena = es.enter_context(nc.sbuf_tensor("arena", [128, AW], F32))
    cur = [0]

    def take(dt, shape):
        nfree = int(np.prod(shape[1:]))
        nbytes = (nfree * _DT_SIZE[dt] + 63) // 64 * 64
        off = cur[0]
        cur[0] += nbytes
        assert cur[0] <= AW * 4, f"arena overflow {cur[0]} > {AW * 4}"
        v = arena[:, off // 4:(off + nbytes) // 4]
        if dt != F32:
            v = v.bitcast(dt)
        v = v[:, 0:nfree]
        if len(shape) == 3:
            v = v.rearrange("p (a b) -> p a b", a=shape[1])
        elif len(shape) == 4:
            v = v.rearrange("p (a b c) -> p a b c", a=shape[1], b=shape[2])
        if shape[0] < 128:
            v = v[0:shape[0]]
        return v

    def flat(v):
        if len(v.shape) == 3:
            return v.rearrange("p a b -> p (a b)")
        if len(v.shape) == 4:
            return v.rearrange("p a b c -> p (a b c)")
        return v

    consts = take(F32, [128, CO_N])
    pp = take(F32, [128, PP_N])
    rows = take(F32, [128, ROWS_N])
    ident_bf = take(BF16, [128, 128])
    I4 = take(BF16, [128, 4, 128])
    negl4_bf = take(BF16, [128, 4, 128])
    row0 = take(BF16, [1, 1024 + 768 + 128])
    c8row = row0[0:1, 0:1024].rearrange("p (a b) -> p a b", a=8)
    convb_bf = row0[0:1, 1024:1792]
    ones_bf = row0[0:1, 1792:1920]
    B8 = take(BF16, [128, 2, 8, 128])
    Dg = take(BF16, [128, 8, 4, 128])
    A_b = take(F32, [128, 8])
    rb_sb = take(F32, [32, 8])
    r31 = take(F32, [1, 8])
    small = take(F32, [128, 176])

    def sm(a, n):
        return small[:, a:a + n]
    ss = [sm(0, 1), sm(1, 1)]
    sd = [sm(2, 1), sm(3, 1)]
    rstd = [sm(4, 1), sm(5, 1)]
    wI = [sm(8, 4), sm(12, 4)]
    dtraw = [sm(16, 8), sm(24, 8)]
    m8 = [sm(32, 8), sm(40, 8)]
    rec = sm(48, 8)
    dt_sb = sm(56, 8)
    a_sb = sm(64, 8)
    cst = sm(72, 16)
    ecs = sm(88, 8)
    dte = sm(96, 8)
    dtot = sm(104, 8)
    negcs = sm(112, 8)
    ssg = sm(120, 2)
    sdg = sm(122, 2)
    rsg = sm(124, 2)
    ssa = [sm(128, 2), sm(130, 2)]
    ssq = [sm(132, 1), sm(133, 1)]
    sq2 = [sm(134, 1), sm(135, 1)]
    r1 = [sm(136, 1), sm(137, 1)]
    ss2 = [sm(138, 1), sm(139, 1)]
    sd2 = [sm(140, 1), sm(141, 1)]
    r2 = [sm(142, 1), sm(143, 1)]
    ssc = [sm(144, 2), sm(146, 2)]
    ss3 = [sm(148, 1), sm(149, 1)]
    sd3 = [sm(150, 1), sm(151, 1)]
    r3 = [sm(152, 1), sm(153, 1)]
    negth = sm(160, 1)
    sgn = sm(161, 1)
    sg2 = sm(162, 1)
    thf = sm(163, 1)
    lnd = sm(164, 8)

    mixT = take(BF16, [128, 8, L])
    xt = [take(F32, [128, 1024])]
    phase_base = cur[0]

    NST = 4
    stage32 = [take(F32, [128, 4096]) for _ in range(NST)]
    stage16 = [take(BF16, [128, 4096]) for _ in range(NST)]
    lhs_h = [take(F32, [32, 128]) for _ in range(2)]
    Rsb = [take(F32, [128, 384]) for _ in range(2)]
    Bt32 = take(F32, [128, 2, 8, 128])
    convb32 = take(F32, [1, 1024])
    cur[0] = phase_base
    w_in_sb = take(BF16, [128, 8, W_IN_COLS])
    kT = take(BF16, [128, 2, L])
    kiT = take(BF16, [128, L])
    v_aug = take(BF16, [128, nt, 2, 65])
    S2 = [take(F32, [128, L]) for _ in range(2)]
    negm = take(BF16, [128, L])
    junk8 = take(I8, [128, L])
    xb = [take(BF16, [128, 1024]) for _ in range(2)]
    uT = [take(BF16, [128, 8, 128]) for _ in range(2)]
    qTz = [take(BF16, [128, 2, 4, 128]) for _ in range(2)]
    qiT = [take(BF16, [128, 2, 128]) for _ in range(2)]
    kiraw = take(F32, [128, 128])
    kicen = take(F32, [128, 128])
    kisq = take(F32, [128, 128])
    kisd = take(F32, [128, 128])
    kirs = take(F32, [128, 128])
    xbcT = [take(BF16, [128, 8, 131]) for _ in range(2)]
    sz = [take(F32, [128, 512]) for _ in range(2)]
    rrelu = [take(F32, [128, 512]) for _ in range(2)]
    E_sb = [take(BF16, [128, 1024]) for _ in range(2)]
    attn_tok = take(BF16, [128, 512])
    xs_tok = take(F32, [128, 8, 64])
    B_tok = take(BF16, [128, 256])
    BCT = take(BF16, [128, 4, 128])
    rL = take(F32, [128, 8, 128])
    GT_sb = take(F32, [128, 2, 128])
    WT = take(BF16, [128, 8, 128])
    X_sb = take(BF16, [128, 8, 64])
    Xd_sb = take(BF16, [128, 8, 64])
    yoff_sb = take(F32, [128, 8, 64])
    t2_sb = take(F32, [128, 8, 64])
    y_sb = take(F32, [128, 512])
    ssd_tok = take(BF16, [128, 512])
    H_sb = take(F32, [128, 8, 64])
    Ht_sb = t2_sb
    Hbf = take(BF16, [128, 8, 64])
    endM = cur[0]

    cur[0] = phase_base
    xt.append(take(F32, [128, 1024]))
    wo_sb = take(BF16, [128, 8, 1024])
    gpm = take(F32, [128, 1024])
    gpl = take(F32, [128, 1024])
    h1 = take(F32, [128, 4, 1024])
    hn = [take(BF16, [128, 1024]) for _ in range(2)]
    hnT = take(BF16, [128, 8, 512])
    aT = take(BF16, [128, 32, 512])
    wu_sb = [take(BF16, [128, 8, 512]) for _ in range(2)]
    wd_sb = [take(BF16, [128, 4, 1024]) for _ in range(2)]
    r32 = [take(F32, [128, 512]) for _ in range(2)]
    ot = [take(F32, [128, 1024]) for _ in range(2)]
    junkF = take(BF16, [128, 1024])
    endF = cur[0]
    print(f"[build] arena: base={phase_base} endM={endM} endF={endF} cap={AW * 4}")

    ps = [es.enter_context(nc.psum_tensor(f"ps{i}", [128, 512], F32)) for i in range(8)]

    dbg_out = {}

    def dump(name, src_ap, shape, reads=()):
        if dbg is None:
            return
        t = nc.dram_tensor("dbg_" + name, list(shape), src_ap.dtype, kind="ExternalOutput").ap()
        dbg_out[name] = (list(shape), src_ap.dtype)
        P.add("sp", lambda e, t=t, s=src_ap: e.dma_start(out=t, in_=s), reads=reads,
              writes=[("dbg", name)], dma_key="dbg_" + name)
        P.dma_tokens.append(("dbg", name))

    bar_n = [0]

    def barrier():
        n = bar_n[0]
        bar_n[0] += 1
        P.add("pe", lambda e: e.matmul(out=ps[7][0:1, 0:2], lhsT=ones_bf[0:1, 0:1], rhs=ones_bf[0:1, 0:2],
                                       start=True, stop=True),
              reads=["ones_bf"], writes=[("ps", 7), ("bar", n, "pe")])
        P.add("act", lambda e: e.activation(out=small[:, 156:157], in_=small[:, 156:157], func=AF.Copy),
              writes=[("bar", n, "act")])
        P.add("dve", lambda e: e.tensor_copy(out=small[:, 157:158], in_=small[:, 157:158]),
              writes=[("bar", n, "dve")])
        P.add("pool", lambda e: e.tensor_copy(out=small[:, 158:159], in_=small[:, 158:159]),
              writes=[("bar", n, "pool")])
        toks = list(P.dma_tokens)
        P.dma_tokens = []
        P.add("sp", lambda e: e.nop(), reads=toks, writes=[("bar", n, "sp")])
        allb = [("bar", n, e) for e in ("pe", "act", "dve", "pool", "sp")]
        P.add("pe", lambda e: e.matmul(out=ps[7][0:1, 0:2], lhsT=ones_bf[0:1, 0:1], rhs=ones_bf[0:1, 0:2],
                                       start=True, stop=True), reads=allb + ["ones_bf"], writes=[("ps", 7)])
        P.add("act", lambda e: e.activation(out=small[:, 156:157], in_=small[:, 156:157], func=AF.Copy), reads=allb)
        P.add("dve", lambda e: e.tensor_copy(out=small[:, 157:158], in_=small[:, 157:158]), reads=allb)
        P.add("pool", lambda e: e.tensor_copy(out=small[:, 158:159], in_=small[:, 158:159]), reads=allb)
        P.add("sp", lambda e: e.nop(), reads=allb)

    def dma(eng, out, in_, reads, writes, key):
        P.add(eng, lambda e: e.dma_start(out=out, in_=in_), reads=reads, writes=writes, dma_key=key)
        P.dma_tokens.extend(writes)

    P.dma_tokens = []

    dma("sp", consts, consts_d, [], ["consts"], "c0")
    dma("sp", pp, pp_d, [], ["pp"], "c1")
    dma("sp", rows, rows_d.partition_broadcast(128), [], ["rows"], "c2")
    dma("sp", rb_sb, relb_d, [], ["rb_sb"], "c3")
    dma("sp", r31, relb_d[31:32, :], [], ["r31"], "c4")
    dma("sp", convb32, convb_d, [], ["convb32"], "c5")
    P.add("dve", lambda e: e.memset(small, 0.0), writes=["small0"])
    P.add("dve", lambda e: e.tensor_copy(out=ident_bf, in_=consts[:, CO_IDENT:CO_IDENT + 128]),
          reads=["consts"], writes=["ident_bf"])
    P.add("dve", lambda e: e.tensor_copy(out=ones_bf, in_=consts[0:1, CO_ONES:CO_ONES + 128]),
          reads=["consts"], writes=["ones_bf"])
    P.add("dve", lambda e: e.tensor_copy(out=convb_bf, in_=convb32[0:1, 0:768]),
          reads=["convb32"], writes=["convb_bf"])
    for r4 in range(4):
        P.add("dve", lambda e, r4=r4: e.tensor_copy(out=I4[:, r4, :], in_=consts[:, CO_IDENT:CO_IDENT + 128]),
              reads=["consts"], writes=["I4"])
        P.add("dve", lambda e, r4=r4: e.tensor_copy(out=negl4_bf[:, r4, :], in_=consts[:, CO_NEGL:CO_NEGL + 128]),
              reads=["consts"], writes=["negl4"])
    for h in range(8):
        k = h % 2
        P.add("dve", lambda e, h=h, k=k: e.tensor_scalar(out=lhs_h[k], in0=consts[0:32, CO_ONES:CO_ONES + 128],
                                                         scalar1=rb_sb[:, h:h + 1], scalar2=None, op0=ALU.mult),
              reads=["consts", "rb_sb"], writes=[("lhs_h", k)])
        P.add("pe", lambda e, k=k: e.matmul(out=ps[k][:, 0:384], lhsT=lhs_h[k],
                                            rhs=consts[0:32, CO_OH:CO_OH + 384], start=True, stop=True),
              reads=[("lhs_h", k), "consts"], writes=[("ps", k)])
        P.add("act", lambda e, k=k: e.activation(out=Rsb[k], in_=ps[k][:, 0:384], func=AF.Copy),
              reads=[("ps", k)], writes=[("Rsb", k)])
        dma("sp", E_d.ap()[h, :].rearrange("(p m) -> p m", p=128), Rsb[k], [("Rsb", k)], [("E_d", h)], f"Rsb{k}")
        for dl in range(2):
            src = bass.AP(E_d, h * 128 * 384 + 127 + 128 * dl, [[383, 128], [1, 128]])
            dma("sp", Bt32[:, dl, h, :], src, [("E_d", h)], [("Bt32", dl, h)], "Bt32")
        P.add("dve", lambda e, h=h: e.tensor_scalar(out=c8row[0:1, h, :], in0=consts[0:1, CO_ONES:CO_ONES + 128],
                                                    scalar1=r31[0:1, h:h + 1], scalar2=8.0, op0=ALU.mult, op1=ALU.mult),
              reads=["consts", "r31"], writes=["c8row"])
    P.add("dve", lambda e: e.tensor_scalar(out=flat(B8), in0=flat(Bt32), scalar1=8.0, scalar2=None, op0=ALU.mult),
          reads=[("Bt32", dl, h) for dl in range(2) for h in range(8)], writes=["B8"])
    for cc in range(8):
        for k in range(4):
            P.add("dve", lambda e, cc=cc, k=k: e.tensor_scalar(
                out=Dg[:, cc, k, :], in0=consts[:, CO_IDENT:CO_IDENT + 128],
                scalar1=pp[:, PP_CONVW + cc * 4 + k:PP_CONVW + cc * 4 + k + 1], scalar2=None, op0=ALU.mult),
                reads=["consts", "pp"], writes=["Dg"])
    P.add("act", lambda e: e.activation(out=A_b, in_=rows[:, R_ALOG:R_ALOG + 8], func=AF.Exp),
          reads=["rows"], writes=["A_b"])
    P.add("dve", lambda e: e.tensor_scalar(out=A_b, in0=A_b, scalar1=-1.0, scalar2=None, op0=ALU.mult),
          reads=["A_b"], writes=["A_b"])

    prep_i = [0]

    def prep_chunk(src_ap, dst_ap, shape_free, scale_ap, cast_eng):
        k = prep_i[0] % NST
        prep_i[0] += 1
        n = prep_i[0]
        nfree = int(np.prod(shape_free))
        s32 = stage32[k][:, 0:nfree]
        s16 = stage16[k][:, 0:nfree]
        if len(shape_free) == 2:
            s32v = s32.rearrange("p (a b) -> p a b", a=shape_free[0])
            s16v = s16.rearrange("p (a b) -> p a b", a=shape_free[0])
        else:
            s32v, s16v = s32, s16
        dma("sp", s32v, src_ap, [], [("st32", k)], f"st32_{k}")
        if scale_ap is not None:
            P.add("act", lambda e: e.activation(out=s16, in_=s32, func=AF.Copy, scale=scale_ap),
                  reads=[("st32", k), "pp"], writes=[("st16", k)])
        else:
            P.add(cast_eng, lambda e: e.tensor_copy(out=s16, in_=s32), reads=[("st32", k)], writes=[("st16", k)])
        dma("sp", dst_ap, s16v, [("st16", k)], [("wscr", n)], f"st16_{k}")

    win_v = win_d.rearrange("(kc p) n -> p kc n", p=128)
    for kc in range(8):
        prep_chunk(win_v[:, kc, :], wi_bf[:, kc, :], [W_IN_COLS], pp[:, PP_GMIX + kc:PP_GMIX + kc + 1], None)
    wout_v = wout_d.rearrange("(cc p) n -> p cc n", p=128)
    for c2 in range(2):
        prep_chunk(wout_v[:, 4 * c2:4 * c2 + 4, :], wo_bf[:, 4 * c2:4 * c2 + 4, :], [4, 1024], None, "pool")
    wup_v = wup_d.rearrange("(kc p) n -> p kc n", p=128)
    wdn_v = wdn_d.rearrange("(fc p) n -> p fc n", p=128)
    for kc in range(8):
        prep_chunk(wup_v[:, kc, :], wu_bf[:, kc, :], [4096], pp[:, PP_GMLP + kc:PP_GMLP + kc + 1], None)
        prep_chunk(wdn_v[:, 4 * kc:4 * kc + 4, :], wd_bf[:, 4 * kc:4 * kc + 4, :], [4, 1024], None,
                   "dve" if kc % 2 else "pool")
    barrier()

    for s in range(nseq):
        dma("sp", w_in_sb, wi_bf, [], ["w_in_sb"], "w_in_sb")
        P.add("pool", lambda e: e.memset(v_aug[:, :, :, 64:65], 1.0), writes=["v_ones"])
        for kq in range(2):
            P.add("pool", lambda e, kq=kq: e.memset(flat(qTz[kq]), 0.0), writes=[("qT", kq)])
        P.add("pool", lambda e: e.memset(flat(H_sb), 0.0), writes=["H"])
        P.add("pool", lambda e: e.memset(flat(Hbf), 0.0), writes=["Hbf"])
        pend_attn = []
        finalizers = []
        for i in range(nt):
            k2 = i % 2
            r0 = s * L + i * 128
            dma("sp", xt[0], x_d[r0:r0 + 128, :], [], [("xt", 0)], "xt0")
            P.add("act", lambda e, k2=k2: e.activation(out=xb[k2], in_=xt[0], func=AF.Square, accum_out=ss[k2]),
                  reads=[("xt", 0)], writes=[("ss", k2), ("xb", k2)])
            P.add("act", lambda e, k2=k2: e.activation(out=sd[k2], in_=ss[k2], func=AF.Ln, scale=1.0 / 1024,
                                                       bias=pp[:, PP_EPS:PP_EPS + 1]),
                  reads=[("ss", k2), "pp"], writes=[("sd", k2)])
            P.add("act", lambda e, k2=k2: e.activation(out=rstd[k2], in_=sd[k2], func=AF.Exp, scale=-0.5),
                  reads=[("sd", k2)], writes=[("rstd", k2)])
            P.add("act", lambda e, k2=k2: e.activation(out=xb[k2], in_=xt[0], func=AF.Copy, scale=rstd[k2]),
                  reads=[("xt", 0), ("rstd", k2)], writes=[("xb", k2)])
            pT = ps[0][:].bitcast(BF16)
            for kc in range(8):
                P.add("pe", lambda e, k2=k2, kc=kc: e.transpose(out=pT[:, kc * 128:(kc + 1) * 128],
                                                                 in_=xb[k2][:, kc * 128:(kc + 1) * 128],
                                                                 identity=ident_bf),
                      reads=[("xb", k2), "ident_bf"], writes=[("ps", 0)])
            P.add("act", lambda e, k2=k2: e.activation(out=flat(uT[k2]), in_=pT, func=AF.Copy),
                  reads=[("ps", 0)], writes=[("uT", k2)])

            def fm_group(bank, slot, g, k2=k2):
                for kc in range(8):
                    P.add("pe", lambda e, kc=kc: e.matmul(out=ps[bank][:, slot * 128:(slot + 1) * 128],
                                                          lhsT=w_in_sb[:, kc, g * 128:(g + 1) * 128],
                                                          rhs=uT[k2][:, kc, :], start=(kc == 0), stop=(kc == 7)),
                          reads=["w_in_sb", ("uT", k2)], writes=[("ps", bank)])
            for g in range(4):
                fm_group(1, g, g)
            for half in range(2):
                P.add("act", lambda e, k2=k2, half=half: e.activation(
                    out=qTz[k2][half * 64:(half + 1) * 64, half, :, :],
                    in_=ps[1][half * 64:(half + 1) * 64, :].rearrange("p (a b) -> p a b", a=4), func=AF.Copy),
                    reads=[("ps", 1)], writes=[("qT", k2)])
            for g in range(4):
                fm_group(2, g, 4 + g)
            P.add("act", lambda e, i=i: e.activation(out=kT[:, :, i * 128:(i + 1) * 128],
                                                     in_=ps[2][:, 0:256].rearrange("p (a b) -> p a b", a=2),
                                                     func=AF.Copy),
                  reads=[("ps", 2)], writes=[("kT", i)])
            P.add("act", lambda e, k2=k2: e.activation(out=flat(qiT[k2]), in_=ps[2][:, 256:512], func=AF.Copy),
                  reads=[("ps", 2)], writes=[("qiT", k2)])
            fm_group(3, 0, 8)
            P.add("act", lambda e: e.activation(out=kiraw, in_=ps[3][:, 0:128], func=AF.Copy),
                  reads=[("ps", 3)], writes=["kiraw"])
            bd = consts[:, CO_BD64:CO_BD64 + 128]
            imbd = consts[:, CO_NU:CO_NU + 128]
            P.add("pe", lambda e: e.matmul(out=ps[3][:, 128:256], lhsT=imbd, rhs=kiraw, start=True, stop=True),
                  reads=["consts", "kiraw"], writes=[("ps", 3)])
            P.add("act", lambda e: e.activation(out=kisq, in_=ps[3][:, 128:256], func=AF.Square),
                  reads=[("ps", 3)], writes=["kisq"])
            P.add("act", lambda e: e.activation(out=kicen, in_=ps[3][:, 128:256], func=AF.Copy),
                  reads=[("ps", 3)], writes=["kicen"])
            P.add("pe", lambda e: e.matmul(out=ps[3][:, 256:384], lhsT=bd, rhs=kisq, start=True, stop=True),
                  reads=["consts", "kisq"], writes=[("ps", 3)])
            P.add("act", lambda e: e.activation(out=kisd, in_=ps[3][:, 256:384], func=AF.Ln,
                                                bias=pp[:, PP_EPS:PP_EPS + 1]),
                  reads=[("ps", 3), "pp"], writes=["kisd"])
            P.add("act", lambda e: e.activation(out=kirs, in_=kisd, func=AF.Exp, scale=-0.5),
                  reads=["kisd"], writes=["kirs"])
            P.add("pool", lambda e: e.tensor_tensor(out=kicen, in0=kicen, in1=kirs, op=ALU.mult),
                  reads=["kicen", "kirs"], writes=["kicen"])
            P.add("act", lambda e, i=i: e.activation(out=kiT[:, i * 128:(i + 1) * 128], in_=kicen,
                                                     func=AF.Identity, scale=pp[:, PP_LNW:PP_LNW + 1],
                                                     bias=pp[:, PP_LNB:PP_LNB + 1]),
                  reads=["kicen", "pp"], writes=[("kiT", i)])
            for cc in range(8):
                fm_group(4 + cc // 4, cc % 4, 9 + cc)
            if i == 0:
                P.add("pool", lambda e, k2=k2: e.memset(xbcT[k2][:, :, 0:3], 0.0), writes=[("xbcT", k2)])
            else:
                P.add("pool", lambda e, k2=k2: e.tensor_copy(out=xbcT[k2][:, :, 0:3], in_=xbcT[1 - k2][:, :, 128:131]),
                      reads=[("xbcT", 1 - k2)], writes=[("xbcT", k2)])
            for hb in range(2):
                P.add("act", lambda e, k2=k2, hb=hb: e.activation(
                    out=xbcT[k2][:, 4 * hb:4 * hb + 4, 3:131],
                    in_=ps[4 + hb][:].rearrange("p (a b) -> p a b", a=4), func=AF.Copy),
                    reads=[("ps", 4 + hb)], writes=[("xbcT", k2)])
            for kc in range(8):
                P.add("pe", lambda e, kc=kc, k2=k2: e.matmul(out=ps[6][:, 0:140], lhsT=uT[k2][:, kc, :],
                                                             rhs=w_in_sb[:, kc, C_TM1:C_TM1 + 140],
                                                             start=(kc == 0), stop=(kc == 7)),
                      reads=["w_in_sb", ("uT", k2)], writes=[("ps", 6)])
            for kc in range(8):
                P.add("pe", lambda e, kc=kc, k2=k2: e.matmul(out=ps[7][:], lhsT=uT[k2][:, kc, :],
                                                             rhs=w_in_sb[:, kc, C_Z:C_Z + 512],
                                                             start=(kc == 0), stop=(kc == 7)),
                      reads=["w_in_sb", ("uT", k2)], writes=[("ps", 7)])
            P.add("act", lambda e, i=i: e.activation(out=v_aug[:, i, :, 0:64],
                                                     in_=ps[6][:, 0:128].rearrange("p (a b) -> p a b", a=2),
                                                     func=AF.Copy),
                  reads=[("ps", 6)], writes=[("v", i)])
            P.add("act", lambda e, k2=k2: e.activation(out=wI[k2], in_=ps[6][:, 128:132], func=AF.Copy, scale=1.0 / 16),
                  reads=[("ps", 6)], writes=[("wI", k2)])
            P.add("act", lambda e, k2=k2: e.activation(out=dtraw[k2], in_=ps[6][:, 132:140], func=AF.Copy),
                  reads=[("ps", 6)], writes=[("dtraw", k2)])
            P.add("act", lambda e, k2=k2: e.activation(out=sz[k2], in_=ps[7][:], func=AF.Silu),
                  reads=[("ps", 7)], writes=[("sz", k2)])

            n = (i + 1) * 128
            Sb = S2[k2]
            cidx = 0
            nchunk = (n + 511) // 512
            for c in range(nchunk):
                wd = min(512, n - c * 512)
                for h in range(4):
                    pair, half = h // 2, h % 2
                    bk = cidx % 2
                    cidx += 1
                    P.add("pe", lambda e, k2=k2, pair=pair, half=half, bk=bk, c=c, wd=wd: e.matmul(
                        out=ps[bk][:, 0:wd], lhsT=qiT[k2][half * 64:(half + 1) * 64, pair, :],
                        rhs=kiT[half * 64:(half + 1) * 64, c * 512:c * 512 + wd], start=True, stop=True),
                        reads=[("qiT", k2)] + [("kiT", jj) for jj in range(4 * c, min(4 * c + 4, i + 1))],
                        writes=[("ps", bk)])
                    P.add("act", lambda e, bk=bk, wd=wd: e.activation(out=rrelu[bk][:, 0:wd], in_=ps[bk][:, 0:wd],
                                                                       func=AF.Relu),
                          reads=[("ps", bk)], writes=[("rrelu", bk)])
                    prev = (consts[:, CO_PERT + c * 512:CO_PERT + c * 512 + wd] if h == 0
                            else Sb[:, c * 512:c * 512 + wd])
                    P.add("dve", lambda e, k2=k2, h=h, bk=bk, c=c, wd=wd, prev=prev, Sb=Sb: e.scalar_tensor_tensor(
                        out=Sb[:, c * 512:c * 512 + wd], in0=rrelu[bk][:, 0:wd], scalar=wI[k2][:, h:h + 1],
                        in1=prev, op0=ALU.mult, op1=ALU.add),
                        reads=[("rrelu", bk), ("wI", k2), "consts", ("S", k2, c)], writes=[("S", k2, c)])
            cl = i // 4
            P.add("pool", lambda e, i=i, Sb=Sb: e.tensor_tensor(out=Sb[:, i * 128:(i + 1) * 128],
                                                                in0=Sb[:, i * 128:(i + 1) * 128],
                                                                in1=consts[:, CO_NEGTRI:CO_NEGTRI + 128], op=ALU.add),
                  reads=[("S", k2, cl), "consts"], writes=[("S", k2, cl)])
            allS = [("S", k2, c) for c in range(nchunk)]

            def queue_topk(i=i, n=n, Sb=Sb, allS=allS):
                if n > topk and i >= ACT_FROM:
                    K = 28
                    for k in range(K):
                        dk = 8.0 / (2 ** k)
                        if k == 0:
                            P.bg_push("act", lambda e: e.activation(out=junk8[:, 0:n], in_=Sb[:, 0:n], func=AF.Sign,
                                                                    accum_out=sgn),
                                      reads=allS, writes=["sgn", "junk8"])
                        else:
                            P.bg_push("act", lambda e: e.activation(out=junk8[:, 0:n], in_=Sb[:, 0:n], func=AF.Sign,
                                                                    bias=negth, accum_out=sgn),
                                      reads=allS + ["negth"], writes=["sgn", "junk8"])
                        P.bg_push("act", lambda e: e.activation(out=sg2, in_=sgn, func=AF.Sign,
                                                                bias=float(n - (2 * topk - 1))),
                                  reads=["sgn"], writes=["sg2"])
                        if k == 0:
                            P.bg_push("act", lambda e, dk=dk: e.activation(out=negth, in_=sg2, func=AF.Identity,
                                                                          scale=-dk / 2),
                                      reads=["sg2"], writes=["negth"])
                        else:
                            P.bg_push("act", lambda e, dk=dk: e.activation(out=negth, in_=sg2, func=AF.Identity,
                                                                          scale=-dk / 2, bias=negth),
                                      reads=["sg2", "negth"], writes=["negth"])
                    dK = 8.0 / (2 ** K)
                    P.bg_push("act", lambda e: e.activation(out=thf, in_=negth, func=AF.Identity, scale=-1.0, bias=-dK),
                              reads=["negth"], writes=["thf"])
                    finalizers.append(lambda: P.add("dve", lambda e: e.tensor_scalar(
                        out=negm[:, 0:n], in0=Sb[:, 0:n], scalar1=thf, scalar2=-30000.0,
                        op0=ALU.is_lt, op1=ALU.mult),
                        reads=allS + ["thf"], writes=["negm"]))
                elif n > topk:
                    for r in range(topk // 8):
                        P.bg_push("dve", lambda e, r=r: e.max(out=m8[r % 2], in_=Sb[:, 0:n]),
                                  reads=allS, writes=[("m8", r % 2)])
                        P.bg_push("dve", lambda e, r=r: e.match_replace(
                            out=Sb[:, 0:n], in_to_replace=m8[r % 2], in_values=Sb[:, 0:n], imm_value=-3.0e38),
                            reads=[("m8", r % 2)] + allS, writes=allS)
                    finalizers.append(lambda: P.add("dve", lambda e: e.tensor_scalar(
                        out=negm[:, 0:n], in0=Sb[:, 0:n], scalar1=-1.0e38, scalar2=-30000.0,
                        op0=ALU.is_gt, op1=ALU.mult),
                        reads=allS, writes=["negm"]))
                else:
                    finalizers.append(lambda: P.add("dve", lambda e: e.tensor_scalar(
                        out=negm[:, 0:n], in0=Sb[:, 0:n], scalar1=-1.0e29, scalar2=-30000.0,
                        op0=ALU.is_lt, op1=ALU.mult),
                        reads=allS, writes=["negm"]))

            def attention(i=i, k2=k2):
                for j in range(i + 1):
                    ek = j % 2
                    for g in range(2):
                        bank = 2 + g
                        for half in range(2):
                            P.add("pe", lambda e, g=g, half=half, j=j, bank=bank: e.matmul(
                                out=ps[bank][:, half * 256:(half + 1) * 256],
                                lhsT=kT[:, g, j * 128:(j + 1) * 128],
                                rhs=qTz[k2][:, half, 2 * g:2 * g + 2, :],
                                start=(half == 0), stop=False),
                                reads=[("kT", j), ("qT", k2)], writes=[("ps", bank)])
                        P.add("pe", lambda e, j=j, bank=bank: e.matmul(
                            out=ps[bank][:], lhsT=negm[:, j * 128:(j + 1) * 128], rhs=flat(I4), start=False, stop=False),
                            reads=["negm", "I4"], writes=[("ps", bank)])
                        if i - j <= 1:
                            P.add("pe", lambda e, g=g, dl=i - j, bank=bank: e.matmul(
                                out=ps[bank][:], lhsT=ident_bf, rhs=B8[:, dl, 4 * g:4 * g + 4, :], start=False, stop=True),
                                reads=["ident_bf", "B8"], writes=[("ps", bank)])
                        else:
                            P.add("pe", lambda e, g=g, bank=bank: e.matmul(
                                out=ps[bank][:], lhsT=ones_bf[0:1, :], rhs=c8row[0:1, 4 * g:4 * g + 4, :],
                                start=False, stop=True),
                                reads=["ones_bf", "c8row"], writes=[("ps", bank)])
                        P.add("act", lambda e, g=g, ek=ek, bank=bank: e.activation(
                            out=E_sb[ek][:, g * 512:(g + 1) * 512], in_=ps[bank][:], func=AF.Exp, scale=0.125),
                            reads=[("ps", bank)], writes=[("E", ek, g)])
                    for h in range(8):
                        bpv = 4 + h // 4
                        hh = h % 4
                        P.add("pe", lambda e, h=h, hh=hh, bpv=bpv, ek=ek, j=j: e.matmul(
                            out=ps[bpv][:, hh * 65:hh * 65 + 65], lhsT=E_sb[ek][:, h * 128:(h + 1) * 128],
                            rhs=v_aug[:, j, h // 4, :], start=(j == 0 and hh == 0), stop=(j == i), skip_group_check=True),
                            reads=[("E", ek, h // 4), ("v", j), "v_ones"], writes=[("ps", bpv)])
                for b2 in range(2):
                    psv = ps[4 + b2][:, 0:260].rearrange("p (h c) -> p h c", c=65)
                    P.add("act", lambda e, b2=b2, psv=psv: e.activation(out=lnd[:, 4 * b2:4 * b2 + 4], in_=psv[:, :, 64],
                                                                       func=AF.Ln),
                          reads=[("ps", 4 + b2)], writes=[("lnd", b2)])
                    P.add("act", lambda e, b2=b2: e.activation(out=rec[:, 4 * b2:4 * b2 + 4], in_=lnd[:, 4 * b2:4 * b2 + 4],
                                                               func=AF.Exp, scale=-1.0),
                          reads=[("lnd", b2)], writes=[("rec", b2)])
                    for hh in range(4):
                        h = 4 * b2 + hh
                        P.add("act", lambda e, h=h, hh=hh, psv=psv: e.activation(
                            out=attn_tok[:, h * 64:(h + 1) * 64], in_=psv[:, hh, 0:64], func=AF.Copy,
                            scale=rec[:, h:h + 1]),
                            reads=[("ps", 4 + b2), ("rec", b2)], writes=["attn_tok"])
                pT6 = ps[6][:].bitcast(BF16)
                for c4 in range(4):
                    P.add("pe", lambda e, c4=c4: e.transpose(out=pT6[:, c4 * 128:(c4 + 1) * 128],
                                                             in_=attn_tok[:, c4 * 128:(c4 + 1) * 128], identity=ident_bf),
                          reads=["attn_tok", "ident_bf"], writes=[("ps", 6)])
                P.add("act", lambda e: e.activation(out=mixT[:, 0:4, i * 128:(i + 1) * 128],
                                                    in_=pT6[:, 0:512].rearrange("p (a b) -> p a b", a=4),
                                                    func=AF.Copy),
                      reads=[("ps", 6)], writes=[("mixT_a", i)])

            pend_attn.append(attention)
            P.bg_flush()
            while finalizers:
                finalizers.pop(0)()
            queue_topk()
            for e_ in ("dve", "act"):
                nb = min(16, len(P.bg.get(e_, [])))
                if nb > 0:
                    P.bg_flush(e_, nb)

            xk = xbcT[k2]
            for cc in range(6):
                bank = 0 if cc < 4 else 1
                col = (cc % 4) * 128
                for k in range(4):
                    P.add("pe", lambda e, cc=cc, k=k, bank=bank, col=col, xk=xk: e.matmul(
                        out=ps[bank][:, col:col + 128], lhsT=xk[:, cc, k:k + 128], rhs=Dg[:, cc, k, :],
                        start=(k == 0), stop=False),
                        reads=[("xbcT", k2), "Dg"], writes=[("ps", bank)])
                P.add("pe", lambda e, cc=cc, bank=bank, col=col: e.matmul(
                    out=ps[bank][:, col:col + 128], lhsT=ones_bf[0:1, :], rhs=convb_bf[0:1, cc * 128:(cc + 1) * 128],
                    start=False, stop=True),
                    reads=["ones_bf", "convb_bf"], writes=[("ps", bank)])
            P.add("act", lambda e: e.activation(out=flat(xs_tok), in_=ps[0][:], func=AF.Silu),
                  reads=[("ps", 0)], writes=["xs_tok"])
            P.add("act", lambda e: e.activation(out=B_tok, in_=ps[1][:, 0:256], func=AF.Silu),
                  reads=[("ps", 1)], writes=["B_tok"])
            for c4 in range(4):
                cc = 4 + c4
                for k in range(4):
                    P.add("pe", lambda e, cc=cc, c4=c4, k=k, xk=xk: e.matmul(
                        out=ps[2][:, c4 * 128:(c4 + 1) * 128], lhsT=Dg[:, cc, k, :], rhs=xk[:, cc, k:k + 128],
                        start=(k == 0), stop=(k == 3)),
                        reads=[("xbcT", k2), "Dg"], writes=[("ps", 2)])
                P.add("act", lambda e, cc=cc, c4=c4: e.activation(
                    out=BCT[:, c4, :], in_=ps[2][:, c4 * 128:(c4 + 1) * 128], func=AF.Silu,
                    bias=pp[:, PP_CONVB + cc:PP_CONVB + cc + 1]),
                    reads=[("ps", 2), "pp"], writes=["BCT"])
            P.add("pool", lambda e, k2=k2: e.tensor_tensor(out=dt_sb, in0=dtraw[k2], in1=rows[:, R_DTB:R_DTB + 8],
                                                           op=ALU.add),
                  reads=[("dtraw", k2), "rows"], writes=["dt_sb"])
            P.add("act", lambda e: e.activation(out=dt_sb, in_=dt_sb, func=AF.Exp), reads=["dt_sb"], writes=["dt_sb"])
            P.add("act", lambda e: e.activation(out=dt_sb, in_=dt_sb, func=AF.Ln, bias=1.0),
                  reads=["dt_sb"], writes=["dt_sb"])
            P.add("pool", lambda e: e.tensor_tensor(out=a_sb, in0=dt_sb, in1=A_b, op=ALU.mult),
                  reads=["dt_sb", "A_b"], writes=["a_sb"])
            for g in range(2):
                P.add("pe", lambda e, g=g: e.matmul(out=ps[3][:, g * 128:(g + 1) * 128], lhsT=BCT[:, g, :],
                                                    rhs=BCT[:, 2 + g, :], start=True, stop=True),
                      reads=["BCT"], writes=[("ps", 3)])
            P.add("pe", lambda e: e.matmul(out=ps[3][:, 256:264], lhsT=consts[:, CO_U:CO_U + 128], rhs=a_sb,
                                           start=True, stop=True),
                  reads=["consts", "a_sb"], writes=[("ps", 3)])
            P.add("pe", lambda e: e.matmul(out=ps[3][:, 264:272], lhsT=consts[:, CO_ONES:CO_ONES + 128], rhs=a_sb,
                                           start=True, stop=True),
                  reads=["consts", "a_sb"], writes=[("ps", 3)])
            P.add("act", lambda e: e.activation(out=flat(GT_sb), in_=ps[3][:, 0:256], func=AF.Copy),
                  reads=[("ps", 3)], writes=["GT_sb"])
            P.add("act", lambda e: e.activation(out=cst, in_=ps[3][:, 256:272], func=AF.Copy),
                  reads=[("ps", 3)], writes=["cst"])
            P.add("act", lambda e: e.activation(out=ecs, in_=cst[:, 0:8], func=AF.Exp), reads=["cst"], writes=["ecs"])
            P.add("act", lambda e: e.activation(out=dtot, in_=cst[:, 8:16], func=AF.Exp), reads=["cst"], writes=["dtot"])
            P.add("pool", lambda e: e.tensor_tensor(out=dte, in0=cst[:, 8:16], in1=cst[:, 0:8], op=ALU.subtract),
                  reads=["cst"], writes=["dte"])
            P.add("act", lambda e: e.activation(out=dte, in_=dte, func=AF.Exp), reads=["dte"], writes=["dte"])
            P.add("pool", lambda e: e.tensor_scalar(out=negcs, in0=cst[:, 0:8], scalar1=-1.0, scalar2=1.0, op0=ALU.mult,
                                                   op1=ALU.mult),
                  reads=["cst"], writes=["negcs"])
            Ub = consts[:, CO_U:CO_U + 128].unsqueeze(1).broadcast_to([128, 8, 128])
            ab = a_sb.unsqueeze(2).broadcast_to([128, 8, 128])
            P.add("pool", lambda e, Ub=Ub, ab=ab: e.tensor_tensor(out=rL, in0=Ub, in1=ab, op=ALU.mult),
                  reads=["consts", "a_sb"], writes=["rL"])
            for b2 in range(2):
                P.add("pe", lambda e, b2=b2: e.matmul(out=ps[4 + b2][:], lhsT=consts[:, CO_ONES:CO_ONES + 128],
                                                      rhs=rL[:, 4 * b2:4 * b2 + 4, :], start=True, stop=False),
                      reads=["consts", "rL"], writes=[("ps", 4 + b2)])
                P.add("pe", lambda e, b2=b2: e.matmul(out=ps[4 + b2][:], lhsT=ident_bf, rhs=flat(negl4_bf),
                                                      start=False, stop=True),
                      reads=["ident_bf", "negl4"], writes=[("ps", 4 + b2)])
            for h in range(8):
                b2, hh = h // 4, h % 4
                P.add("act", lambda e, h=h, b2=b2, hh=hh: e.activation(
                    out=rL[:, h, :], in_=ps[4 + b2][:, hh * 128:(hh + 1) * 128], func=AF.Exp,
                    bias=negcs[:, h:h + 1]),
                    reads=[("ps", 4 + b2), "negcs"], writes=["rL"])
            for b2 in range(2):
                gtb = GT_sb[:, b2, :].unsqueeze(1).broadcast_to([128, 4, 128])
                P.add("pool", lambda e, b2=b2, gtb=gtb: e.tensor_tensor(out=WT[:, 4 * b2:4 * b2 + 4, :],
                                                                       in0=rL[:, 4 * b2:4 * b2 + 4, :], in1=gtb,
                                                                       op=ALU.mult),
                      reads=["rL", "GT_sb"], writes=[("WT", b2)])
            dtb = dt_sb.unsqueeze(2).broadcast_to([128, 8, 64])
            dteb = dte.unsqueeze(2).broadcast_to([128, 8, 64])
            P.add("pool", lambda e, dtb=dtb: e.tensor_tensor(out=X_sb, in0=xs_tok, in1=dtb, op=ALU.mult),
                  reads=["xs_tok", "dt_sb"], writes=["X_sb"])
            P.add("pool", lambda e, dteb=dteb: e.tensor_tensor(out=Xd_sb, in0=X_sb, in1=dteb, op=ALU.mult),
                  reads=["X_sb", "dte"], writes=["Xd_sb"])
            for h in range(8):
                P.add("pe", lambda e, h=h: e.matmul(out=ps[6][:, h * 64:(h + 1) * 64], lhsT=WT[:, h, :], rhs=X_sb[:, h, :],
                                                    start=True, stop=True),
                      reads=[("WT", h // 4), "X_sb"], writes=[("ps", 6)])
            for g in range(2):
                P.add("pe", lambda e, g=g: e.matmul(out=ps[7][:, g * 256:(g + 1) * 256], lhsT=BCT[:, 2 + g, :],
                                                    rhs=Hbf[:, 4 * g:4 * g + 4, :], start=True, stop=True),
                      reads=["BCT", "Hbf"], writes=[("ps", 7)])
            P.add("act", lambda e: e.activation(out=flat(yoff_sb), in_=ps[7][:], func=AF.Copy),
                  reads=[("ps", 7)], writes=["yoff_sb"])
            ecsb = ecs.unsqueeze(2).broadcast_to([128, 8, 64])
            dskb = rows[:, R_DSKIP:R_DSKIP + 8].unsqueeze(2).broadcast_to([128, 8, 64])
            P.add("pool", lambda e, ecsb=ecsb: e.tensor_tensor(out=yoff_sb, in0=yoff_sb, in1=ecsb, op=ALU.mult),
                  reads=["yoff_sb", "ecs"], writes=["yoff_sb"])
            P.add("pool", lambda e, dskb=dskb: e.tensor_tensor(out=t2_sb, in0=xs_tok, in1=dskb, op=ALU.mult),
                  reads=["xs_tok", "rows"], writes=["t2_sb"])
            P.add("pool", lambda e: e.tensor_tensor(out=yoff_sb, in0=yoff_sb, in1=t2_sb, op=ALU.add),
                  reads=["yoff_sb", "t2_sb"], writes=["yoff_sb"])
            P.add("act", lambda e: e.activation(out=y_sb, in_=ps[6][:], func=AF.Copy),
                  reads=[("ps", 6)], writes=["y_sb"])
            P.add("pool", lambda e: e.tensor_tensor(out=y_sb, in0=y_sb, in1=flat(yoff_sb), op=ALU.add),
                  reads=["y_sb", "yoff_sb"], writes=["y_sb"])
            P.add("pool", lambda e, k2=k2: e.tensor_tensor(out=y_sb, in0=y_sb, in1=sz[k2], op=ALU.mult),
                  reads=["y_sb", ("sz", k2)], writes=["y_sb"])
            for g in range(2):
                P.add("act", lambda e, g=g: e.activation(out=flat(t2_sb)[:, g * 256:(g + 1) * 256],
                                                         in_=y_sb[:, g * 256:(g + 1) * 256],
                                                         func=AF.Square, accum_out=ssg[:, g:g + 1]),
                      reads=["y_sb"], writes=[("ssg", g), "t2_sb"])
            P.add("act", lambda e: e.activation(out=sdg, in_=ssg, func=AF.Ln, scale=1.0 / 256,
                                                bias=pp[:, PP_EPS:PP_EPS + 1]),
                  reads=[("ssg", 0), ("ssg", 1), "pp"], writes=["sdg"])
            P.add("act", lambda e: e.activation(out=rsg, in_=sdg, func=AF.Exp, scale=-0.5), reads=["sdg"], writes=["rsg"])
            for g in range(2):
                P.add("act", lambda e, g=g: e.activation(out=flat(t2_sb)[:, g * 256:(g + 1) * 256],
                                                         in_=y_sb[:, g * 256:(g + 1) * 256], func=AF.Copy,
                                                         scale=rsg[:, g:g + 1]),
                      reads=["y_sb", "rsg"], writes=["t2_sb"])
                P.add("pool", lambda e, g=g: e.tensor_tensor(
                    out=ssd_tok[:, g * 256:(g + 1) * 256], in0=flat(t2_sb)[:, g * 256:(g + 1) * 256],
                    in1=rows[:, R_SSDNW + g * 256:R_SSDNW + (g + 1) * 256], op=ALU.mult),
                    reads=["t2_sb", "rows"], writes=["ssd_tok"])
            pT7 = ps[7][:].bitcast(BF16)
            for c4 in range(4):
                P.add("pe", lambda e, c4=c4: e.transpose(out=pT7[:, c4 * 128:(c4 + 1) * 128],
                                                         in_=ssd_tok[:, c4 * 128:(c4 + 1) * 128], identity=ident_bf),
                      reads=["ssd_tok", "ident_bf"], writes=[("ps", 7)])
            P.add("act", lambda e, i=i: e.activation(out=mixT[:, 4:8, i * 128:(i + 1) * 128],
                                                     in_=pT7[:, 0:512].rearrange("p (a b) -> p a b", a=4), func=AF.Copy),
                  reads=[("ps", 7)], writes=[("mixT_s", i)])
            for g in range(2):
                P.add("pe", lambda e, g=g: e.matmul(out=ps[0][:, g * 256:(g + 1) * 256], lhsT=B_tok[:, g * 128:(g + 1) * 128],
                                                    rhs=Xd_sb[:, 4 * g:4 * g + 4, :], start=True, stop=True),
                      reads=["B_tok", "Xd_sb"], writes=[("ps", 0)])
            dtotb = dtot.unsqueeze(2).broadcast_to([128, 8, 64])
            P.add("pool", lambda e, dtotb=dtotb: e.tensor_tensor(out=Ht_sb, in0=H_sb, in1=dtotb, op=ALU.mult),
                  reads=["H", "dtot"], writes=["t2_sb"])
            P.add("act", lambda e: e.activation(out=flat(yoff_sb), in_=ps[0][:], func=AF.Copy),
                  reads=[("ps", 0)], writes=["yoff_sb"])
            P.add("pool", lambda e: e.tensor_tensor(out=flat(H_sb), in0=flat(yoff_sb), in1=flat(Ht_sb), op=ALU.add),
                  reads=["yoff_sb", "t2_sb"], writes=["H"])
            P.add("act", lambda e: e.activation(out=flat(Hbf), in_=flat(H_sb), func=AF.Copy),
                  reads=["H"], writes=["Hbf"])
            if len(pend_attn) == 2:
                pend_attn.pop(0)()
        P.bg_flush()
        while finalizers:
            finalizers.pop(0)()
        if dbg is not None and s == 0:
            dump("negm", negm, [128, L], reads=["negm"])
            dump("S", S2[(nt - 1) % 2], [128, L], reads=[("S", (nt - 1) % 2, c) for c in range((L + 511) // 512)])
            dump("thf", small[:, 160:164], [128, 4], reads=["thf", "negth", "sgn", "sg2"])
        while pend_attn:
            pend_attn.pop(0)()
        if dbg is not None and s == 0:
            dump("mixT", mixT, [128, 8, L], reads=[("mixT_a", jj) for jj in range(nt)] + [("mixT_s", jj) for jj in range(nt)])
        barrier()
        if stop_after == "M":
            continue

        dma("sp", wo_sb, wo_bf, [], ["wo_sb"], "wo_sb")
        dma("sp", gpm, gpost_d[0:1, :].partition_broadcast(128), [], ["gpm"], "gpm")
        dma("sp", gpl, gpost_d[1:2, :].partition_broadcast(128), [], ["gpl"], "gpl")
        for c in range(nt // 4):
            for tt in range(4):
                i = 4 * c + tt
                k2 = i % 2
                r0 = s * L + i * 128
                dma("sp", xt[k2], x_d[r0:r0 + 128, :], [], [("xt", k2)], f"xt{k2}")
                for nh in range(2):
                    for cc in range(8):
                        P.add("pe", lambda e, nh=nh, cc=cc, i=i: e.matmul(
                            out=ps[nh][:], lhsT=mixT[:, cc, i * 128:(i + 1) * 128],
                            rhs=wo_sb[:, cc, nh * 512:(nh + 1) * 512], start=(cc == 0), stop=(cc == 7)),
                            reads=[("mixT_a", i), ("mixT_s", i), "wo_sb"], writes=[("ps", nh)])
                    P.add("act", lambda e, nh=nh, k2=k2: e.activation(out=junkF[:, nh * 512:(nh + 1) * 512], in_=ps[nh][:], func=AF.Square,
                                                                      accum_out=ssa[k2][:, nh:nh + 1]),
                          reads=[("ps", nh)], writes=[("ssa", k2, nh), ("junkF", nh)])
                P.add("dve", lambda e, k2=k2: e.tensor_tensor(out=ssq[k2], in0=ssa[k2][:, 0:1], in1=ssa[k2][:, 1:2],
                                                              op=ALU.add),
                      reads=[("ssa", k2, 0), ("ssa", k2, 1)], writes=[("ssq", k2)])
                P.add("act", lambda e, k2=k2: e.activation(out=sq2[k2], in_=ssq[k2], func=AF.Sqrt, scale=1.0 / 1024,
                                                           bias=pp[:, PP_EPS:PP_EPS + 1]),
                      reads=[("ssq", k2), "pp"], writes=[("sq2", k2)])
                P.add("dve", lambda e, k2=k2: e.reciprocal(out=r1[k2], in_=sq2[k2]), reads=[("sq2", k2)],
                      writes=[("r1", k2)])
                for nh in range(2):
                    P.add("dve", lambda e, nh=nh, k2=k2, tt=tt: e.scalar_tensor_tensor(
                        out=h1[:, tt, nh * 512:(nh + 1) * 512], in0=ps[nh][:], scalar=r1[k2],
                        in1=gpm[:, nh * 512:(nh + 1) * 512], op0=ALU.mult, op1=ALU.mult),
                        reads=[("ps", nh), ("r1", k2), "gpm"], writes=[("h1", tt)])
                P.add("pool", lambda e, k2=k2, tt=tt: e.tensor_tensor(out=h1[:, tt, :], in0=h1[:, tt, :], in1=xt[k2],
                                                                      op=ALU.add),
                      reads=[("h1", tt), ("xt", k2)], writes=[("h1", tt)])
                P.add("act", lambda e, k2=k2, tt=tt: e.activation(out=hn[k2], in_=h1[:, tt, :], func=AF.Square,
                                                                  accum_out=ss2[k2]),
                      reads=[("h1", tt)], writes=[("ss2", k2), ("hn", k2)])
                P.add("act", lambda e, k2=k2: e.activation(out=sd2[k2], in_=ss2[k2], func=AF.Sqrt, scale=1.0 / 1024,
                                                           bias=pp[:, PP_EPS:PP_EPS + 1]),
                      reads=[("ss2", k2), "pp"], writes=[("sd2", k2)])
                P.add("dve", lambda e, k2=k2: e.reciprocal(out=r2[k2], in_=sd2[k2]), reads=[("sd2", k2)],
                      writes=[("r2", k2)])
                P.add("act", lambda e, k2=k2, tt=tt: e.activation(out=hn[k2], in_=h1[:, tt, :], func=AF.Copy,
                                                                  scale=r2[k2]),
                      reads=[("h1", tt), ("r2", k2)], writes=[("hn", k2)])
                pT2 = ps[2][:].bitcast(BF16)
                for kc in range(8):
                    P.add("pe", lambda e, k2=k2, kc=kc: e.transpose(out=pT2[:, kc * 128:(kc + 1) * 128],
                                                                     in_=hn[k2][:, kc * 128:(kc + 1) * 128],
                                                                     identity=ident_bf),
                          reads=[("hn", k2), "ident_bf"], writes=[("ps", 2)])
                P.add("act", lambda e, tt=tt: e.activation(out=hnT[:, :, tt * 128:(tt + 1) * 128],
                                                           in_=pT2.rearrange("p (a b) -> p a b", a=8), func=AF.Copy),
                      reads=[("ps", 2)], writes=[("hnT", tt)])
            ub = 0
            for fg in range(8):
                wk = fg % 2
                dma("sp", wu_sb[wk], wu_bf[:, :, fg * 512:(fg + 1) * 512], [], [("wu", wk)], f"wu{wk}")
                for f4 in range(4):
                    fc = fg * 4 + f4
                    bank = 3 + (ub % 4)
                    rk = ub % 2
                    ub += 1
                    for kc in range(8):
                        P.add("pe", lambda e, wk=wk, f4=f4, kc=kc, bank=bank: e.matmul(
                            out=ps[bank][:], lhsT=wu_sb[wk][:, kc, f4 * 128:(f4 + 1) * 128], rhs=hnT[:, kc, :],
                            start=(kc == 0), stop=(kc == 7)),
                            reads=[("wu", wk)] + [("hnT", t4) for t4 in range(4)], writes=[("ps", bank)])
                    P.add("act", lambda e, bank=bank, rk=rk: e.activation(out=r32[rk], in_=ps[bank][:], func=AF.Relu),
                          reads=[("ps", bank)], writes=[("r32", rk)])
                    P.add("pool", lambda e, fc=fc, rk=rk: e.tensor_tensor(out=aT[:, fc, :], in0=r32[rk], in1=r32[rk],
                                                                          op=ALU.mult),
                          reads=[("r32", rk)], writes=[("aT", fc)])
            for dg in range(8):
                wk = dg % 2
                dma("sp", wd_sb[wk], wd_bf[:, dg * 4:(dg + 1) * 4, :], [], [("wd", wk)], f"wd{wk}")
                for tt in range(4):
                    for nh in range(2):
                        for f4 in range(4):
                            fc = dg * 4 + f4
                            P.add("pe", lambda e, wk=wk, tt=tt, nh=nh, f4=f4, fc=fc, dg=dg: e.matmul(
                                out=ps[2 * tt + nh][:], lhsT=aT[:, fc, tt * 128:(tt + 1) * 128],
                                rhs=wd_sb[wk][:, f4, nh * 512:(nh + 1) * 512],
                                start=(dg == 0 and f4 == 0), stop=(dg == 7 and f4 == 3)),
                                reads=[("wd", wk), ("aT", fc)], writes=[("ps", 2 * tt + nh)])
            for tt in range(4):
                i = 4 * c + tt
                k2 = i % 2
                r0 = s * L + i * 128
                for nh in range(2):
                    P.add("act", lambda e, nh=nh, k2=k2, tt=tt: e.activation(
                        out=junkF[:, nh * 512:(nh + 1) * 512], in_=ps[2 * tt + nh][:], func=AF.Square,
                        accum_out=ssc[k2][:, nh:nh + 1]),
                        reads=[("ps", 2 * tt + nh)], writes=[("ssc", k2, nh), ("junkF", nh)])
                P.add("dve", lambda e, k2=k2: e.tensor_tensor(out=ss3[k2], in0=ssc[k2][:, 0:1], in1=ssc[k2][:, 1:2],
                                                              op=ALU.add),
                      reads=[("ssc", k2, 0), ("ssc", k2, 1)], writes=[("ss3", k2)])
                P.add("act", lambda e, k2=k2: e.activation(out=sd3[k2], in_=ss3[k2], func=AF.Sqrt, scale=1.0 / 1024,
                                                           bias=pp[:, PP_EPS:PP_EPS + 1]),
                      reads=[("ss3", k2), "pp"], writes=[("sd3", k2)])
                P.add("dve", lambda e, k2=k2: e.reciprocal(out=r3[k2], in_=sd3[k2]), reads=[("sd3", k2)],
                      writes=[("r3", k2)])
                for nh in range(2):
                    P.add("dve", lambda e, nh=nh, k2=k2, tt=tt: e.scalar_tensor_tensor(
                        out=ot[k2][:, nh * 512:(nh + 1) * 512], in0=ps[2 * tt + nh][:], scalar=r3[k2],
                        in1=gpl[:, nh * 512:(nh + 1) * 512], op0=ALU.mult, op1=ALU.mult),
                        reads=[("ps", 2 * tt + nh), ("r3", k2), "gpl"], writes=[("ot", k2)])
                P.add("pool", lambda e, k2=k2, tt=tt: e.tensor_tensor(out=ot[k2], in0=ot[k2], in1=h1[:, tt, :], op=ALU.add),
                      reads=[("ot", k2), ("h1", tt)], writes=[("ot", k2)])
                dma("sp", out_d[r0:r0 + 128, :], ot[k2], [("ot", k2)], [("ot", k2)], f"ot{k2}")
        barrier()

    final_keys = ["dbg_" + n for n in dbg_out] + ["ot0", "ot1"]
    P.emit(es, final_wait_keys=final_keys)
    es.close()
    if dbg is not None:
        dbg.update(dbg_out)
    return nc


PP_GMIX = 0
PP_GMLP = 8
PP_EPS = 16
PP_LNW = 17
PP_LNB = 18
PP_CONVW = 19
PP_CONVB = 51
PP_N = 59
R_DTB = 0
R_ALOG = 8
R_DSKIP = 16
R_SSDNW = 24
ROWS_N = 536


def _host_inputs(inputs, core, nseq, L):
    f = lambda a: np.ascontiguousarray(np.asarray(a, dtype=np.float32))
    x = f(inputs["x"])
    xs = x[core * nseq:(core + 1) * nseq, :L].reshape(nseq * L, D_MODEL)
    w_in = f(inputs["w_in"])[0][:, _w_in_perm()]
    pp = np.zeros((128, PP_N), np.float32)
    pp[:, PP_GMIX:PP_GMIX + 8] = f(inputs["norm_pre_mix"])[0].reshape(8, 128).T
    pp[:, PP_GMLP:PP_GMLP + 8] = f(inputs["norm_pre_mlp"])[0].reshape(8, 128).T
    pp[:, PP_EPS] = EPS
    pp[:, PP_LNW] = np.tile(f(inputs["k_idx_ln_w"])[0], 2)
    pp[:, PP_LNB] = np.tile(f(inputs["k_idx_ln_b"])[0], 2)
    cw = f(inputs["conv_w"])[0]
    pp[:, PP_CONVW:PP_CONVW + 32] = cw.reshape(4, 8, 128).transpose(2, 1, 0).reshape(128, 32)
    pp[:, PP_CONVB:PP_CONVB + 8] = f(inputs["conv_b"])[0].reshape(8, 128).T
    rows = np.zeros((1, ROWS_N), np.float32)
    rows[0, R_DTB:R_DTB + 8] = f(inputs["dt_bias"])[0]
    rows[0, R_ALOG:R_ALOG + 8] = f(inputs["a_log"])[0]
    rows[0, R_DSKIP:R_DSKIP + 8] = f(inputs["d_skip"])[0]
    rows[0, R_SSDNW:R_SSDNW + 512] = f(inputs["ssd_norm_w"])[0]
    return {
        "x": np.ascontiguousarray(xs),
        "w_in": np.ascontiguousarray(w_in),
        "w_out": f(inputs["w_out"])[0],
        "w_up": f(inputs["w_mlp_up"])[0],
        "w_down": f(inputs["w_mlp_down"])[0],
        "consts": _consts(),
        "pp": pp,
        "rows": rows,
        "gpost": np.ascontiguousarray(np.stack([f(inputs["norm_post_mix"])[0], f(inputs["norm_post_mlp"])[0]], 0)),
        "convb": f(inputs["conv_b"])[0].reshape(1, 1024),
        "rel_bias": f(inputs["rel_bias"]),
    }


def kernel(**inputs):
    nseq, nt = 2, 16
    nc = build(nseq=nseq, nt=nt)
    in_maps = [_host_inputs(inputs, c, nseq, nt * 128) for c in range(N_CORES)]
    res = run_bass_kernel_spmd(nc, in_maps, core_ids=list(range(N_CORES)))
    outs = [np.asarray(r["out"]).reshape(nseq, nt * 128, D_MODEL) for r in res.results]
    return np.concatenate(outs, axis=0).astype(np.float32)
```

```python
import numpy as np
from contextlib import ExitStack
import concourse.bass as bass
import concourse.mybir as mybir
from concourse.bass_utils import run_bass_kernel_spmd

F32 = mybir.dt.float32
BF16 = mybir.dt.bfloat16
AF = mybir.ActivationFunctionType
ALU = mybir.AluOpType
AX = mybir.AxisListType

D_MODEL = 1024
L_FULL = 2048
N_CORES = 8
EPS = 1e-6
NEG_BIG = -1.0e30

OFF_Q, OFF_K, OFF_V, OFF_QI, OFF_KI, OFF_WI, OFF_Z, OFF_XBC, OFF_DT = (
    0, 512, 640, 768, 1024, 1088, 1092, 1604, 2628)
N_FM = 17
C_TM1 = N_FM * 128
C_Z = C_TM1 + 140
W_IN_COLS = C_Z + 512


def _w_in_perm():
    cols = []
    for p in range(4):
        g, j = p // 2, p % 2
        for h in (4 * g + j, 4 * g + 2 + j):
            cols += list(range(OFF_Q + 64 * h, OFF_Q + 64 * h + 64))
    for g in range(2):
        for _ in range(2):
            cols += list(range(OFF_K + 64 * g, OFF_K + 64 * g + 64))
    for p in range(2):
        for h in (2 * p, 2 * p + 1):
            cols += list(range(OFF_QI + 64 * h, OFF_QI + 64 * h + 64))
    for _ in range(2):
        cols += list(range(OFF_KI, OFF_KI + 64))
    cols += list(range(OFF_XBC, OFF_XBC + 1024))
    cols += list(range(OFF_V, OFF_V + 128))
    cols += list(range(OFF_WI, OFF_WI + 4))
    cols += list(range(OFF_DT, OFF_DT + 8))
    cols += list(range(OFF_Z, OFF_Z + 512))
    assert len(cols) == W_IN_COLS
    return np.array(cols, dtype=np.int64)


def _t5_bucket_np(d):
    d = np.asarray(d)
    max_exact = 16
    df = np.maximum(d, 1).astype(np.float32)
    large = max_exact + (np.log(df / max_exact) / np.float32(np.log(128 / max_exact))
                         * (32 - max_exact)).astype(np.int32)
    large = np.minimum(large, 31)
    return np.where(d < max_exact, d, large)


CO_IDENT = 0
CO_U = 128
CO_NEGTRI = 256
CO_NEGL = 384
CO_BD64 = 512
CO_PERT = 640
CO_OH = 640 + 2048
CO_ONES = CO_OH + 384
CO_NU = CO_ONES + 128
CO_N = CO_NU + 128


def _consts():
    c = np.zeros((128, CO_N), np.float32)
    idx = np.arange(128)
    c[:, CO_IDENT:CO_IDENT + 128] = np.eye(128, dtype=np.float32)
    c[:, CO_U:CO_U + 128] = (idx[:, None] <= idx[None, :]).astype(np.float32)
    c[:, CO_NEGTRI:CO_NEGTRI + 128] = np.where(idx[None, :] > idx[:, None], NEG_BIG, 0.0)
    c[:, CO_NEGL:CO_NEGL + 128] = np.where(idx[None, :] < idx[:, None], -30000.0, 0.0)
    bd = np.zeros((128, 128), np.float32)
    bd[:64, :64] = 1.0 / 64
    bd[64:, 64:] = 1.0 / 64
    c[:, CO_BD64:CO_BD64 + 128] = bd
    c[:, CO_PERT:CO_PERT + 2048] = (-(2.0 ** -23) * (np.arange(2048) + 1)).astype(np.float32)[None, :]
    m = np.arange(383)
    b = _t5_bucket_np(np.maximum(m - 127, 0))
    oh = np.zeros((32, 384), np.float32)
    oh[b, m] = 1.0
    c[:32, CO_OH:CO_OH + 384] = oh
    c[:, CO_ONES:CO_ONES + 128] = 1.0
    c[:, CO_NU:CO_NU + 128] = np.eye(128, dtype=np.float32) - bd
    return c


class _Op:
    __slots__ = ("eng", "fn", "deps", "flag", "is_dma", "key", "tick")

    def __init__(self, eng, fn, is_dma, key):
        self.eng = eng
        self.fn = fn
        self.deps = []
        self.flag = False
        self.is_dma = is_dma
        self.key = key
        self.tick = 0


class Prog:
    ENGS = ("pe", "act", "dve", "pool", "sp")
    EPOCH = 12000

    def __init__(self, nc):
        self.nc = nc
        self.q = {e: [] for e in self.ENGS}
        self.last_w = {}
        self.readers = {}
        self.dma_ops = []
        self.bg = {}
        self.bg_rate = {"dve": 6, "act": 3}

    def bg_push(self, eng, fn, reads=(), writes=()):
        self.bg.setdefault(eng, []).append((fn, reads, writes))

    def bg_flush(self, eng=None, n=None):
        for e in ([eng] if eng else list(self.bg.keys())):
            q = self.bg.get(e, [])
            k = 0
            while q and (n is None or k < n):
                fn, reads, writes = q.pop(0)
                self._add(e, fn, reads, writes, None)
                k += 1

    def add(self, eng, fn, reads=(), writes=(), dma_key=None):
        if self.bg.get(eng):
            self.bg_flush(eng, self.bg_rate.get(eng, 2))
        return self._add(eng, fn, reads, writes, dma_key)

    def _add(self, eng, fn, reads=(), writes=(), dma_key=None):
        is_dma = dma_key is not None
        op = _Op(eng, fn, is_dma, dma_key)
        cand = {}
        for t in reads:
            w = self.last_w.get(t)
            if w is not None:
                cand[id(w)] = (w, True)
        for t in writes:
            w = self.last_w.get(t)
            if w is not None and id(w) not in cand:
                cand[id(w)] = (w, False)
            for r in self.readers.get(t, {}).values():
                if id(r) not in cand:
                    cand[id(r)] = (r, False)
        for d, raw in cand.values():
            if d is op:
                continue
            keep = True
            if not d.is_dma and d.eng == eng:
                keep = (eng in ("act", "dve", "pool")) or (is_dma and eng != "sp")
            if keep:
                d.flag = True
                op.deps.append(d)
        rk = ("dma", id(op)) if is_dma else eng
        for t in reads:
            self.readers.setdefault(t, {})[rk] = op
        for t in writes:
            self.last_w[t] = op
            self.readers[t] = {}
        self.q[eng].append(op)
        if is_dma:
            self.dma_ops.append(op)
        return op

    def emit(self, es, final_wait_keys=()):
        nc = self.nc
        n_epochs = {}
        for e in self.ENGS:
            cnt = 0
            for op in self.q[e]:
                if op.is_dma:
                    continue
                if op.flag:
                    cnt += 1
                    op.tick = cnt
            n_epochs[e] = (cnt + self.EPOCH - 1) // self.EPOCH
        dma_cnt = {}
        for e in self.ENGS:
            for op in self.q[e]:
                if op.is_dma:
                    dma_cnt[op.key] = dma_cnt.get(op.key, 0) + 1
                    op.tick = dma_cnt[op.key] * 16
        sems = {}
        for e in self.ENGS:
            for k in range(n_epochs[e]):
                sems[(e, k)] = es.enter_context(nc.semaphore(f"s_{e}_{k}"))
        for k in dma_cnt:
            sems[("dma", k)] = es.enter_context(nc.semaphore(f"d_{k}"))

        def ev(op):
            if op.is_dma:
                return sems[("dma", op.key)], op.tick
            k = (op.tick - 1) // self.EPOCH
            return sems[(op.eng, k)], op.tick - k * self.EPOCH

        block = es.enter_context(nc.Block())

        def run(eng_name, eng):
            waited = {}
            for op in self.q[eng_name]:
                need = {}
                for d in op.deps:
                    s, v = ev(d)
                    if need.get(s.num, (None, 0))[1] < v:
                        need[s.num] = (s, v)
                for sn, (s, v) in need.items():
                    if waited.get(sn, 0) < v:
                        eng.wait_ge(s, v)
                        waited[sn] = v
                ins = op.fn(eng)
                if op.is_dma:
                    s, _ = ev(op)
                    ins.then_inc(s, 16)
                elif op.flag:
                    s, _ = ev(op)
                    ins.then_inc(s, 1)
            if eng_name == "sp":
                for k in final_wait_keys:
                    if k in dma_cnt:
                        eng.wait_ge(sems[("dma", k)], dma_cnt[k] * 16)

        @block.tensor
        def _(e):
            run("pe", e)

        @block.scalar
        def _(e):
            run("act", e)

        @block.vector
        def _(e):
            run("dve", e)

        @block.gpsimd
        def _(e):
            run("pool", e)

        @block.sync
        def _(e):
            run("sp", e)


I8 = mybir.dt.int8
_DT_SIZE = {F32: 4, BF16: 2, I8: 1}


def build(nseq=2, nt=16, dbg=None, stop_after=None, act_from=12):
    ACT_FROM = act_from
    L = nt * 128
    assert nt % 4 == 0
    topk = min(256, L // 4)
    ntok = nseq * L
    nc = bass.Bass("TRN2", target_bir_lowering=False)
    es = ExitStack()
    P = Prog(nc)

    def dram_in(name, shape, dt=F32):
        return nc.dram_tensor(name, list(shape), dt, kind="ExternalInput").ap()

    x_d = dram_in("x", [ntok, D_MODEL])
    win_d = dram_in("w_in", [D_MODEL, W_IN_COLS])
    wout_d = dram_in("w_out", [1024, 1024])
    wup_d = dram_in("w_up", [1024, 4096])
    wdn_d = dram_in("w_down", [4096, 1024])
    consts_d = dram_in("consts", [128, CO_N])
    pp_d = dram_in("pp", [128, PP_N])
    rows_d = dram_in("rows", [1, ROWS_N])
    gpost_d = dram_in("gpost", [2, 1024])
    convb_d = dram_in("convb", [1, 1024])
    relb_d = dram_in("rel_bias", [32, 8])
    out_d = nc.dram_tensor("out", [ntok, D_MODEL], F32, kind="ExternalOutput").ap()

    wi_bf = nc.dram_tensor("wi_bf", [128, 8, W_IN_COLS], BF16).ap()
    wo_bf = nc.dram_tensor("wo_bf", [128, 8, 1024], BF16).ap()
    wu_bf = nc.dram_tensor("wu_bf", [128, 8, 4096], BF16).ap()
    wd_bf = nc.dram_tensor("wd_bf", [128, 32, 1024], BF16).ap()
    E_d = nc.dram_tensor("E_d", [8, 128 * 384], F32)

    AW = 53200
    arena = es.enter_context(nc.sbuf_tensor("arena", [128, AW], F32))
    cur = [0]

    def take(dt, shape):
        nfree = int(np.prod(shape[1:]))
        nbytes = (nfree * _DT_SIZE[dt] + 63) // 64 * 64
        off = cur[0]
        cur[0] += nbytes
        assert cur[0] <= AW * 4, f"arena overflow {cur[0]} > {AW * 4}"
        v = arena[:, off // 4:(off + nbytes) // 4]
        if dt != F32:
            v = v.bitcast(dt)
        v = v[:, 0:nfree]
        if len(shape) == 3:
            v = v.rearrange("p (a b) -> p a b", a=shape[1])
        elif len(shape) == 4:
            v = v.rearrange("p (a b c) -> p a b c", a=shape[1], b=shape[2])
        if shape[0] < 128:
            v = v[0:shape[0]]
        return v

    def flat(v):
        if len(v.shape) == 3:
            return v.rearrange("p a b -> p (a b)")
        if len(v.shape) == 4:
            return v.rearrange("p a b c -> p (a b c)")
        return v

    consts = take(F32, [128, CO_N])
    pp = take(F32, [128, PP_N])
    rows = take(F32, [128, ROWS_N])
    ident_bf = take(BF16, [128, 128])
    I4 = take(BF16, [128, 4, 128])
    negl4_bf = take(BF16, [128, 4, 128])
    row0 = take(BF16, [1, 1024 + 768 + 128])
    c8row = row0[0:1, 0:1024].rearrange("p (a b) -> p a b", a=8)
    convb_bf = row0[0:1, 1024:1792]
    ones_bf = row0[0:1, 1792:1920]
    B8 = take(BF16, [128, 2, 8, 128])
    Dg = take(BF16, [128, 8, 4, 128])
    A_b = take(F32, [128, 8])
    rb_sb = take(F32, [32, 8])
    r31 = take(F32, [1, 8])
    small = take(F32, [128, 176])

    def sm(a, n):
        return small[:, a:a + n]
    ss = [sm(0, 1), sm(1, 1)]
    sd = [sm(2, 1), sm(3, 1)]
    rstd = [sm(4, 1), sm(5, 1)]
    wI = [sm(8, 4), sm(12, 4)]
    dtraw = [sm(16, 8), sm(24, 8)]
    m8 = [sm(32, 8), sm(40, 8)]
    rec = sm(48, 8)
    dt_sb = sm(56, 8)
    a_sb = sm(64, 8)
    cst = sm(72, 16)
    ecs = sm(88, 8)
    dte = sm(96, 8)
    dtot = sm(104, 8)
    negcs = sm(112, 8)
    ssg = sm(120, 2)
    sdg = sm(122, 2)
    rsg = sm(124, 2)
    ssa = [sm(128, 2), sm(130, 2)]
    ssq = [sm(132, 1), sm(133, 1)]
    sq2 = [sm(134, 1), sm(135, 1)]
    r1 = [sm(136, 1), sm(137, 1)]
    ss2 = [sm(138, 1), sm(139, 1)]
    sd2 = [sm(140, 1), sm(141, 1)]
    r2 = [sm(142, 1), sm(143, 1)]
    ssc = [sm(144, 2), sm(146, 2)]
    ss3 = [sm(148, 1), sm(149, 1)]
    sd3 = [sm(150, 1), sm(151, 1)]
    r3 = [sm(152, 1), sm(153, 1)]
    negth = sm(160, 1)
    sgn = sm(161, 1)
    sg2 = sm(162, 1)
    thf = sm(163, 1)
    lnd = sm(164, 8)

    mixT = take(BF16, [128, 8, L])
    xt = [take(F32, [128, 1024])]
    phase_base = cur[0]

    NST = 4
    stage32 = [take(F32, [128, 4096]) for _ in range(NST)]
    stage16 = [take(BF16, [128, 4096]) for _ in range(NST)]
    lhs_h = [take(F32, [32, 128]) for _ in range(2)]
    Rsb = [take(F32, [128, 384]) for _ in range(2)]
    Bt32 = take(F32, [128, 2, 8, 128])
    convb32 = take(F32, [1, 1024])
    cur[0] = phase_base
    w_in_sb = take(BF16, [128, 8, W_IN_COLS])
    kT = take(BF16, [128, 2, L])
    kiT = take(BF16, [128, L])
    v_aug = take(BF16, [128, nt, 2, 65])
    S2 = [take(F32, [128, L]) for _ in range(2)]
    negm = take(BF16, [128, L])
    junk8 = take(I8, [128, L])
    xb = [take(BF16, [128, 1024]) for _ in range(2)]
    uT = [take(BF16, [128, 8, 128]) for _ in range(2)]
    qTz = [take(BF16, [128, 2, 4, 128]) for _ in range(2)]
    qiT = [take(BF16, [128, 2, 128]) for _ in range(2)]
    kiraw = take(F32, [128, 128])
    kicen = take(F32, [128, 128])
    kisq = take(F32, [128, 128])
    kisd = take(F32, [128, 128])
    kirs = take(F32, [128, 128])
    xbcT = [take(BF16, [128, 8, 131]) for _ in range(2)]
    sz = [take(F32, [128, 512]) for _ in range(2)]
    rrelu = [take(F32, [128, 512]) for _ in range(2)]
    E_sb = [take(BF16, [128, 1024]) for _ in range(2)]
    attn_tok = take(BF16, [128, 512])
    xs_tok = take(F32, [128, 8, 64])
    B_tok = take(BF16, [128, 256])
    BCT = take(BF16, [128, 4, 128])
    rL = take(F32, [128, 8, 128])
    GT_sb = take(F32, [128, 2, 128])
    WT = take(BF16, [128, 8, 128])
    X_sb = take(BF16, [128, 8, 64])
    Xd_sb = take(BF16, [128, 8, 64])
    yoff_sb = take(F32, [128, 8, 64])
    t2_sb = take(F32, [128, 8, 64])
    y_sb = take(F32, [128, 512])
    ssd_tok = take(BF16, [128, 512])
    H_sb = take(F32, [128, 8, 64])
    Ht_sb = t2_sb
    Hbf = take(BF16, [128, 8, 64])
    endM = cur[0]

    cur[0] = phase_base
    xt.append(take(F32, [128, 1024]))
    wo_sb = take(BF16, [128, 8, 1024])
    gpm = take(F32, [128, 1024])
    gpl = take(F32, [128, 1024])
    h1 = take(F32, [128, 4, 1024])
    hn = [take(BF16, [128, 1024]) for _ in range(2)]
    hnT = take(BF16, [128, 8, 512])
    aT = take(BF16, [128, 32, 512])
    wu_sb = [take(BF16, [128, 8, 512]) for _ in range(2)]
    wd_sb = [take(BF16, [128, 4, 1024]) for _ in range(2)]
    r32 = [take(F32, [128, 512]) for _ in range(2)]
    ot = [take(F32, [128, 1024]) for _ in range(2)]
    junkF = take(BF16, [128, 1024])
    endF = cur[0]
    print(f"[build] arena: base={phase_base} endM={endM} endF={endF} cap={AW * 4}")

    ps = [es.enter_context(nc.psum_tensor(f"ps{i}", [128, 512], F32)) for i in range(8)]

    dbg_out = {}

    def dump(name, src_ap, shape, reads=()):
        if dbg is None:
            return
        t = nc.dram_tensor("dbg_" + name, list(shape), src_ap.dtype, kind="ExternalOutput").ap()
        dbg_out[name] = (list(shape), src_ap.dtype)
        P.add("sp", lambda e, t=t, s=src_ap: e.dma_start(out=t, in_=s), reads=reads,
              writes=[("dbg", name)], dma_key="dbg_" + name)
        P.dma_tokens.append(("dbg", name))

    bar_n = [0]

    def barrier():
        n = bar_n[0]
        bar_n[0] += 1
        P.add("pe", lambda e: e.matmul(out=ps[7][0:1, 0:2], lhsT=ones_bf[0:1, 0:1], rhs=ones_bf[0:1, 0:2],
                                       start=True, stop=True),
              reads=["ones_bf"], writes=[("ps", 7), ("bar", n, "pe")])
        P.add("act", lambda e: e.activation(out=small[:, 156:157], in_=small[:, 156:157], func=AF.Copy),
              writes=[("bar", n, "act")])
        P.add("dve", lambda e: e.tensor_copy(out=small[:, 157:158], in_=small[:, 157:158]),
              writes=[("bar", n, "dve")])
        P.add("pool", lambda e: e.tensor_copy(out=small[:, 158:159], in_=small[:, 158:159]),
              writes=[("bar", n, "pool")])
        toks = list(P.dma_tokens)
        P.dma_tokens = []
        P.add("sp", lambda e: e.nop(), reads=toks, writes=[("bar", n, "sp")])
        allb = [("bar", n, e) for e in ("pe", "act", "dve", "pool", "sp")]
        P.add("pe", lambda e: e.matmul(out=ps[7][0:1, 0:2], lhsT=ones_bf[0:1, 0:1], rhs=ones_bf[0:1, 0:2],
                                       start=True, stop=True), reads=allb + ["ones_bf"], writes=[("ps", 7)])
        P.add("act", lambda e: e.activation(out=small[:, 156:157], in_=small[:, 156:157], func=AF.Copy), reads=allb)
        P.add("dve", lambda e: e.tensor_copy(out=small[:, 157:158], in_=small[:, 157:158]), reads=allb)
        P.add("pool", lambda e: e.tensor_copy(out=small[:, 158:159], in_=small[:, 158:159]), reads=allb)
        P.add("sp", lambda e: e.nop(), reads=allb)

    def dma(eng, out, in_, reads, writes, key):
        P.add(eng, lambda e: e.dma_start(out=out, in_=in_), reads=reads, writes=writes, dma_key=key)
        P.dma_tokens.extend(writes)

    P.dma_tokens = []

    dma("sp", consts, consts_d, [], ["consts"], "c0")
    dma("sp", pp, pp_d, [], ["pp"], "c1")
    dma("sp", rows, rows_d.partition_broadcast(128), [], ["rows"], "c2")
    dma("sp", rb_sb, relb_d, [], ["rb_sb"], "c3")
    dma("sp", r31, relb_d[31:32, :], [], ["r31"], "c4")
    dma("sp", convb32, convb_d, [], ["convb32"], "c5")
    P.add("dve", lambda e: e.memset(small, 0.0), writes=["small0"])
    P.add("dve", lambda e: e.tensor_copy(out=ident_bf, in_=consts[:, CO_IDENT:CO_IDENT + 128]),
          reads=["consts"], writes=["ident_bf"])
    P.add("dve", lambda e: e.tensor_copy(out=ones_bf, in_=consts[0:1, CO_ONES:CO_ONES + 128]),
          reads=["consts"], writes=["ones_bf"])
    P.add("dve", lambda e: e.tensor_copy(out=convb_bf, in_=convb32[0:1, 0:768]),
          reads=["convb32"], writes=["convb_bf"])
    for r4 in range(4):
        P.add("dve", lambda e, r4=r4: e.tensor_copy(out=I4[:, r4, :], in_=consts[:, CO_IDENT:CO_IDENT + 128]),
              reads=["consts"], writes=["I4"])
        P.add("dve", lambda e, r4=r4: e.tensor_copy(out=negl4_bf[:, r4, :], in_=consts[:, CO_NEGL:CO_NEGL + 128]),
              reads=["consts"], writes=["negl4"])
    for h in range(8):
        k = h % 2
        P.add("dve", lambda e, h=h, k=k: e.tensor_scalar(out=lhs_h[k], in0=consts[0:32, CO_ONES:CO_ONES + 128],
                                                         scalar1=rb_sb[:, h:h + 1], scalar2=None, op0=ALU.mult),
              reads=["consts", "rb_sb"], writes=[("lhs_h", k)])
        P.add("pe", lambda e, k=k: e.matmul(out=ps[k][:, 0:384], lhsT=lhs_h[k],
                                            rhs=consts[0:32, CO_OH:CO_OH + 384], start=True, stop=True),
              reads=[("lhs_h", k), "consts"], writes=[("ps", k)])
        P.add("act", lambda e, k=k: e.activation(out=Rsb[k], in_=ps[k][:, 0:384], func=AF.Copy),
              reads=[("ps", k)], writes=[("Rsb", k)])
        dma("sp", E_d.ap()[h, :].rearrange("(p m) -> p m", p=128), Rsb[k], [("Rsb", k)], [("E_d", h)], f"Rsb{k}")
        for dl in range(2):
            src = bass.AP(E_d, h * 128 * 384 + 127 + 128 * dl, [[383, 128], [1, 128]])
            dma("sp", Bt32[:, dl, h, :], src, [("E_d", h)], [("Bt32", dl, h)], "Bt32")
        P.add("dve", lambda e, h=h: e.tensor_scalar(out=c8row[0:1, h, :], in0=consts[0:1, CO_ONES:CO_ONES + 128],
                                                    scalar1=r31[0:1, h:h + 1], scalar2=8.0, op0=ALU.mult, op1=ALU.mult),
              reads=["consts", "r31"], writes=["c8row"])
    P.add("dve", lambda e: e.tensor_scalar(out=flat(B8), in0=flat(Bt32), scalar1=8.0, scalar2=None, op0=ALU.mult),
          reads=[("Bt32", dl, h) for dl in range(2) for h in range(8)], writes=["B8"])
    for cc in range(8):
        for k in range(4):
            P.add("dve", lambda e, cc=cc, k=k: e.tensor_scalar(
                out=Dg[:, cc, k, :], in0=consts[:, CO_IDENT:CO_IDENT + 128],
                scalar1=pp[:, PP_CONVW + cc * 4 + k:PP_CONVW + cc * 4 + k + 1], scalar2=None, op0=ALU.mult),
                reads=["consts", "pp"], writes=["Dg"])
    P.add("act", lambda e: e.activation(out=A_b, in_=rows[:, R_ALOG:R_ALOG + 8], func=AF.Exp),
          reads=["rows"], writes=["A_b"])
    P.add("dve", lambda e: e.tensor_scalar(out=A_b, in0=A_b, scalar1=-1.0, scalar2=None, op0=ALU.mult),
          reads=["A_b"], writes=["A_b"])

    prep_i = [0]

    def prep_chunk(src_ap, dst_ap, shape_free, scale_ap, cast_eng):
        k = prep_i[0] % NST
        prep_i[0] += 1
        n = prep_i[0]
        nfree = int(np.prod(shape_free))
        s32 = stage32[k][:, 0:nfree]
        s16 = stage16[k][:, 0:nfree]
        if len(shape_free) == 2:
            s32v = s32.rearrange("p (a b) -> p a b", a=shape_free[0])
            s16v = s16.rearrange("p (a b) -> p a b", a=shape_free[0])
        else:
            s32v, s16v = s32, s16
        dma("sp", s32v, src_ap, [], [("st32", k)], f"st32_{k}")
        if scale_ap is not None:
            P.add("act", lambda e: e.activation(out=s16, in_=s32, func=AF.Copy, scale=scale_ap),
                  reads=[("st32", k), "pp"], writes=[("st16", k)])
        else:
            P.add(cast_eng, lambda e: e.tensor_copy(out=s16, in_=s32), reads=[("st32", k)], writes=[("st16", k)])
        dma("sp", dst_ap, s16v, [("st16", k)], [("wscr", n)], f"st16_{k}")

    win_v = win_d.rearrange("(kc p) n -> p kc n", p=128)
    for kc in range(8):
        prep_chunk(win_v[:, kc, :], wi_bf[:, kc, :], [W_IN_COLS], pp[:, PP_GMIX + kc:PP_GMIX + kc + 1], None)
    wout_v = wout_d.rearrange("(cc p) n -> p cc n", p=128)
    for c2 in range(2):
        prep_chunk(wout_v[:, 4 * c2:4 * c2 + 4, :], wo_bf[:, 4 * c2:4 * c2 + 4, :], [4, 1024], None, "pool")
    wup_v = wup_d.rearrange("(kc p) n -> p kc n", p=128)
    wdn_v = wdn_d.rearrange("(fc p) n -> p fc n", p=128)
    for kc in range(8):
        prep_chunk(wup_v[:, kc, :], wu_bf[:, kc, :], [4096], pp[:, PP_GMLP + kc:PP_GMLP + kc + 1], None)
        prep_chunk(wdn_v[:, 4 * kc:4 * kc + 4, :], wd_bf[:, 4 * kc:4 * kc + 4, :], [4, 1024], None,
                   "dve" if kc % 2 else "pool")
    barrier()

    for s in range(nseq):
        dma("sp", w_in_sb, wi_bf, [], ["w_in_sb"], "w_in_sb")
        P.add("pool", lambda e: e.memset(v_aug[:, :, :, 64:65], 1.0), writes=["v_ones"])
        for kq in range(2):
            P.add("pool", lambda e, kq=kq: e.memset(flat(qTz[kq]), 0.0), writes=[("qT", kq)])
        P.add("pool", lambda e: e.memset(flat(H_sb), 0.0), writes=["H"])
        P.add("pool", lambda e: e.memset(flat(Hbf), 0.0), writes=["Hbf"])
        pend_attn = []
        finalizers = []
        for i in range(nt):
            k2 = i % 2
            r0 = s * L + i * 128
            dma("sp", xt[0], x_d[r0:r0 + 128, :], [], [("xt", 0)], "xt0")
            P.add("act", lambda e, k2=k2: e.activation(out=xb[k2], in_=xt[0], func=AF.Square, accum_out=ss[k2]),
                  reads=[("xt", 0)], writes=[("ss", k2), ("xb", k2)])
            P.add("act", lambda e, k2=k2: e.activation(out=sd[k2], in_=ss[k2], func=AF.Ln, scale=1.0 / 1024,
                                                       bias=pp[:, PP_EPS:PP_EPS + 1]),
                  reads=[("ss", k2), "pp"], writes=[("sd", k2)])
            P.add("act", lambda e, k2=k2: e.activation(out=rstd[k2], in_=sd[k2], func=AF.Exp, scale=-0.5),
                  reads=[("sd", k2)], writes=[("rstd", k2)])
            P.add("act", lambda e, k2=k2: e.activation(out=xb[k2], in_=xt[0], func=AF.Copy, scale=rstd[k2]),
                  reads=[("xt", 0), ("rstd", k2)], writes=[("xb", k2)])
            pT = ps[0][:].bitcast(BF16)
            for kc in range(8):
                P.add("pe", lambda e, k2=k2, kc=kc: e.transpose(out=pT[:, kc * 128:(kc + 1) * 128],
                                                                 in_=xb[k2][:, kc * 128:(kc + 1) * 128],
                                                                 identity=ident_bf),
                      reads=[("xb", k2), "ident_bf"], writes=[("ps", 0)])
            P.add("act", lambda e, k2=k2: e.activation(out=flat(uT[k2]), in_=pT, func=AF.Copy),
                  reads=[("ps", 0)], writes=[("uT", k2)])

            def fm_group(bank, slot, g, k2=k2):
                for kc in range(8):
                    P.add("pe", lambda e, kc=kc: e.matmul(out=ps[bank][:, slot * 128:(slot + 1) * 128],
                                                          lhsT=w_in_sb[:, kc, g * 128:(g + 1) * 128],
                                                          rhs=uT[k2][:, kc, :], start=(kc == 0), stop=(kc == 7)),
                          reads=["w_in_sb", ("uT", k2)], writes=[("ps", bank)])
            for g in range(4):
                fm_group(1, g, g)
            for half in range(2):
                P.add("act", lambda e, k2=k2, half=half: e.activation(
                    out=qTz[k2][half * 64:(half + 1) * 64, half, :, :],
                    in_=ps[1][half * 64:(half + 1) * 64, :].rearrange("p (a b) -> p a b", a=4), func=AF.Copy),
                    reads=[("ps", 1)], writes=[("qT", k2)])
            for g in range(4):
                fm_group(2, g, 4 + g)
            P.add("act", lambda e, i=i: e.activation(out=kT[:, :, i * 128:(i + 1) * 128],
                                                     in_=ps[2][:, 0:256].rearrange("p (a b) -> p a b", a=2),
                                                     func=AF.Copy),
                  reads=[("ps", 2)], writes=[("kT", i)])
            P.add("act", lambda e, k2=k2: e.activation(out=flat(qiT[k2]), in_=ps[2][:, 256:512], func=AF.Copy),
                  reads=[("ps", 2)], writes=[("qiT", k2)])
            fm_group(3, 0, 8)
            P.add("act", lambda e: e.activation(out=kiraw, in_=ps[3][:, 0:128], func=AF.Copy),
                  reads=[("ps", 3)], writes=["kiraw"])
            bd = consts[:, CO_BD64:CO_BD64 + 128]
            imbd = consts[:, CO_NU:CO_NU + 128]
            P.add("pe", lambda e: e.matmul(out=ps[3][:, 128:256], lhsT=imbd, rhs=kiraw, start=True, stop=True),
                  reads=["consts", "kiraw"], writes=[("ps", 3)])
            P.add("act", lambda e: e.activation(out=kisq, in_=ps[3][:, 128:256], func=AF.Square),
                  reads=[("ps", 3)], writes=["kisq"])
            P.add("act", lambda e: e.activation(out=kicen, in_=ps[3][:, 128:256], func=AF.Copy),
                  reads=[("ps", 3)], writes=["kicen"])
            P.add("pe", lambda e: e.matmul(out=ps[3][:, 256:384], lhsT=bd, rhs=kisq, start=True, stop=True),
                  reads=["consts", "kisq"], writes=[("ps", 3)])
            P.add("act", lambda e: e.activation(out=kisd, in_=ps[3][:, 256:384], func=AF.Ln,
                                                bias=pp[:, PP_EPS:PP_EPS + 1]),
                  reads=[("ps", 3), "pp"], writes=["kisd"])
            P.add("act", lambda e: e.activation(out=kirs, in_=kisd, func=AF.Exp, scale=-0.5),
                  reads=["kisd"], writes=["kirs"])
            P.add("pool", lambda e: e.tensor_tensor(out=kicen, in0=kicen, in1=kirs, op=ALU.mult),
                  reads=["kicen", "kirs"], writes=["kicen"])
            P.add("act", lambda e, i=i: e.activation(out=kiT[:, i * 128:(i + 1) * 128], in_=kicen,
                                                     func=AF.Identity, scale=pp[:, PP_LNW:PP_LNW + 1],
                                                     bias=pp[:, PP_LNB:PP_LNB + 1]),
                  reads=["kicen", "pp"], writes=[("kiT", i)])
            for cc in range(8):
                fm_group(4 + cc // 4, cc % 4, 9 + cc)
            if i == 0:
                P.add("pool", lambda e, k2=k2: e.memset(xbcT[k2][:, :, 0:3], 0.0), writes=[("xbcT", k2)])
            else:
                P.add("pool", lambda e, k2=k2: e.tensor_copy(out=xbcT[k2][:, :, 0:3], in_=xbcT[1 - k2][:, :, 128:131]),
                      reads=[("xbcT", 1 - k2)], writes=[("xbcT", k2)])
            for hb in range(2):
                P.add("act", lambda e, k2=k2, hb=hb: e.activation(
                    out=xbcT[k2][:, 4 * hb:4 * hb + 4, 3:131],
                    in_=ps[4 + hb][:].rearrange("p (a b) -> p a b", a=4), func=AF.Copy),
                    reads=[("ps", 4 + hb)], writes=[("xbcT", k2)])
            for kc in range(8):
                P.add("pe", lambda e, kc=kc, k2=k2: e.matmul(out=ps[6][:, 0:140], lhsT=uT[k2][:, kc, :],
                                                             rhs=w_in_sb[:, kc, C_TM1:C_TM1 + 140],
                                                             start=(kc == 0), stop=(kc == 7)),
                      reads=["w_in_sb", ("uT", k2)], writes=[("ps", 6)])
            for kc in range(8):
                P.add("pe", lambda e, kc=kc, k2=k2: e.matmul(out=ps[7][:], lhsT=uT[k2][:, kc, :],
                                                             rhs=w_in_sb[:, kc, C_Z:C_Z + 512],
                                                             start=(kc == 0), stop=(kc == 7)),
                      reads=["w_in_sb", ("uT", k2)], writes=[("ps", 7)])
            P.add("act", lambda e, i=i: e.activation(out=v_aug[:, i, :, 0:64],
                                                     in_=ps[6][:, 0:128].rearrange("p (a b) -> p a b", a=2),
                                                     func=AF.Copy),
                  reads=[("ps", 6)], writes=[("v", i)])
            P.add("act", lambda e, k2=k2: e.activation(out=wI[k2], in_=ps[6][:, 128:132], func=AF.Copy, scale=1.0 / 16),
                  reads=[("ps", 6)], writes=[("wI", k2)])
            P.add("act", lambda e, k2=k2: e.activation(out=dtraw[k2], in_=ps[6][:, 132:140], func=AF.Copy),
                  reads=[("ps", 6)], writes=[("dtraw", k2)])
            P.add("act", lambda e, k2=k2: e.activation(out=sz[k2], in_=ps[7][:], func=AF.Silu),
                  reads=[("ps", 7)], writes=[("sz", k2)])

            n = (i + 1) * 128
            Sb = S2[k2]
            cidx = 0
            nchunk = (n + 511) // 512
            for c in range(nchunk):
                wd = min(512, n - c * 512)
                for h in range(4):
                    pair, half = h // 2, h % 2
                    bk = cidx % 2
                    cidx += 1
                    P.add("pe", lambda e, k2=k2, pair=pair, half=half, bk=bk, c=c, wd=wd: e.matmul(
                        out=ps[bk][:, 0:wd], lhsT=qiT[k2][half * 64:(half + 1) * 64, pair, :],
                        rhs=kiT[half * 64:(half + 1) * 64, c * 512:c * 512 + wd], start=True, stop=True),
                        reads=[("qiT", k2)] + [("kiT", jj) for jj in range(4 * c, min(4 * c + 4, i + 1))],
                        writes=[("ps", bk)])
                    P.add("act", lambda e, bk=bk, wd=wd: e.activation(out=rrelu[bk][:, 0:wd], in_=ps[bk][:, 0:wd],
                                                                       func=AF.Relu),
                          reads=[("ps", bk)], writes=[("rrelu", bk)])
                    prev = (consts[:, CO_PERT + c * 512:CO_PERT + c * 512 + wd] if h == 0
                            else Sb[:, c * 512:c * 512 + wd])
                    P.add("dve", lambda e, k2=k2, h=h, bk=bk, c=c, wd=wd, prev=prev, Sb=Sb: e.scalar_tensor_tensor(
                        out=Sb[:, c * 512:c * 512 + wd], in0=rrelu[bk][:, 0:wd], scalar=wI[k2][:, h:h + 1],
                        in1=prev, op0=ALU.mult, op1=ALU.add),
                        reads=[("rrelu", bk), ("wI", k2), "consts", ("S", k2, c)], writes=[("S", k2, c)])
            cl = i // 4
            P.add("pool", lambda e, i=i, Sb=Sb: e.tensor_tensor(out=Sb[:, i * 128:(i + 1) * 128],
                                                                in0=Sb[:, i * 128:(i + 1) * 128],
                                                                in1=consts[:, CO_NEGTRI:CO_NEGTRI + 128], op=ALU.add),
                  reads=[("S", k2, cl), "consts"], writes=[("S", k2, cl)])
            allS = [("S", k2, c) for c in range(nchunk)]

            def queue_topk(i=i, n=n, Sb=Sb, allS=allS):
                if n > topk and i >= ACT_FROM:
                    K = 28
                    for k in range(K):
                        dk = 8.0 / (2 ** k)
                        if k == 0:
                            P.bg_push("act", lambda e: e.activation(out=junk8[:, 0:n], in_=Sb[:, 0:n], func=AF.Sign,
                                                                    accum_out=sgn),
                                      reads=allS, writes=["sgn", "junk8"])
                        else:
                            P.bg_push("act", lambda e: e.activation(out=junk8[:, 0:n], in_=Sb[:, 0:n], func=AF.Sign,
                                                                    bias=negth, accum_out=sgn),
                                      reads=allS + ["negth"], writes=["sgn", "junk8"])
                        P.bg_push("act", lambda e: e.activation(out=sg2, in_=sgn, func=AF.Sign,
                                                                bias=float(n - (2 * topk - 1))),
                                  reads=["sgn"], writes=["sg2"])
                        if k == 0:
                            P.bg_push("act", lambda e, dk=dk: e.activation(out=negth, in_=sg2, func=AF.Identity,
                                                                          scale=-dk / 2),
                                      reads=["sg2"], writes=["negth"])
                        else:
                            P.bg_push("act", lambda e, dk=dk: e.activation(out=negth, in_=sg2, func=AF.Identity,
                                                                          scale=-dk / 2, bias=negth),
                                      reads=["sg2", "negth"], writes=["negth"])
                    dK = 8.0 / (2 ** K)
                    P.bg_push("act", lambda e: e.activation(out=thf, in_=negth, func=AF.Identity, scale=-1.0, bias=-dK),
                              reads=["negth"], writes=["thf"])
                    finalizers.append(lambda: P.add("dve", lambda e: e.tensor_scalar(
                        out=negm[:, 0:n], in0=Sb[:, 0:n], scalar1=thf, scalar2=-30000.0,
                        op0=ALU.is_lt, op1=ALU.mult),
                        reads=allS + ["thf"], writes=["negm"]))
                elif n > topk:
                    for r in range(topk // 8):
                        P.add("dve", lambda e, r=r: e.max(out=m8[r % 2], in_=Sb[:, 0:n]),
                              reads=allS, writes=[("m8", r % 2)])
                        P.add("dve", lambda e, r=r: e.match_replace(
                            out=Sb[:, 0:n], in_to_replace=m8[r % 2], in_values=Sb[:, 0:n], imm_value=-3.0e38),
                            reads=[("m8", r % 2)] + allS, writes=allS)
                    finalizers.append(lambda: P.add("dve", lambda e: e.tensor_scalar(
                        out=negm[:, 0:n], in0=Sb[:, 0:n], scalar1=-1.0e38, scalar2=-30000.0,
                        op0=ALU.is_gt, op1=ALU.mult),
                        reads=allS, writes=["negm"]))
                else:
                    finalizers.append(lambda: P.add("dve", lambda e: e.tensor_scalar(
                        out=negm[:, 0:n], in0=Sb[:, 0:n], scalar1=-1.0e29, scalar2=-30000.0,
                        op0=ALU.is_lt, op1=ALU.mult),
                        reads=allS, writes=["negm"]))

            def attention(i=i, k2=k2):
                for j in range(i + 1):
                    ek = j % 2
                    for g in range(2):
                        bank = 2 + g
                        for half in range(2):
                            P.add("pe", lambda e, g=g, half=half, j=j, bank=bank: e.matmul(
                                out=ps[bank][:, half * 256:(half + 1) * 256],
                                lhsT=kT[:, g, j * 128:(j + 1) * 128],
                                rhs=qTz[k2][:, half, 2 * g:2 * g + 2, :],
                                start=(half == 0), stop=False),
                                reads=[("kT", j), ("qT", k2)], writes=[("ps", bank)])
                        P.add("pe", lambda e, j=j, bank=bank: e.matmul(
                            out=ps[bank][:], lhsT=negm[:, j * 128:(j + 1) * 128], rhs=flat(I4), start=False, stop=False),
                            reads=["negm", "I4"], writes=[("ps", bank)])
                        if i - j <= 1:
                            P.add("pe", lambda e, g=g, dl=i - j, bank=bank: e.matmul(
                                out=ps[bank][:], lhsT=ident_bf, rhs=B8[:, dl, 4 * g:4 * g + 4, :], start=False, stop=True),
                                reads=["ident_bf", "B8"], writes=[("ps", bank)])
                        else:
                            P.add("pe", lambda e, g=g, bank=bank: e.matmul(
                                out=ps[bank][:], lhsT=ones_bf[0:1, :], rhs=c8row[0:1, 4 * g:4 * g + 4, :],
                                start=False, stop=True),
                                reads=["ones_bf", "c8row"], writes=[("ps", bank)])
                        P.add("act", lambda e, g=g, ek=ek, bank=bank: e.activation(
                            out=E_sb[ek][:, g * 512:(g + 1) * 512], in_=ps[bank][:], func=AF.Exp, scale=0.125),
                            reads=[("ps", bank)], writes=[("E", ek, g)])
                    for h in range(8):
                        bpv = 4 + h // 4
                        hh = h % 4
                        P.add("pe", lambda e, h=h, hh=hh, bpv=bpv, ek=ek, j=j: e.matmul(
                            out=ps[bpv][:, hh * 65:hh * 65 + 65], lhsT=E_sb[ek][:, h * 128:(h + 1) * 128],
                            rhs=v_aug[:, j, h // 4, :], start=(j == 0 and hh == 0), stop=(j == i), skip_group_check=True),
                            reads=[("E", ek, h // 4), ("v", j), "v_ones"], writes=[("ps", bpv)])
                for b2 in range(2):
                    psv = ps[4 + b2][:, 0:260].rearrange("p (h c) -> p h c", c=65)
                    P.add("act", lambda e, b2=b2, psv=psv: e.activation(out=lnd[:, 4 * b2:4 * b2 + 4], in_=psv[:, :, 64],
                                                                       func=AF.Ln),
                          reads=[("ps", 4 + b2)], writes=[("lnd", b2)])
                    P.add("act", lambda e, b2=b2: e.activation(out=rec[:, 4 * b2:4 * b2 + 4], in_=lnd[:, 4 * b2:4 * b2 + 4],
                                                               func=AF.Exp, scale=-1.0),
                          reads=[("lnd", b2)], writes=[("rec", b2)])
                    for hh in range(4):
                        h = 4 * b2 + hh
                        P.add("act", lambda e, h=h, hh=hh, psv=psv: e.activation(
                            out=attn_tok[:, h * 64:(h + 1) * 64], in_=psv[:, hh, 0:64], func=AF.Copy,
                            scale=rec[:, h:h + 1]),
                            reads=[("ps", 4 + b2), ("rec", b2)], writes=["attn_tok"])
                pT6 = ps[6][:].bitcast(BF16)
                for c4 in range(4):
                    P.add("pe", lambda e, c4=c4: e.transpose(out=pT6[:, c4 * 128:(c4 + 1) * 128],
                                                             in_=attn_tok[:, c4 * 128:(c4 + 1) * 128], identity=ident_bf),
                          reads=["attn_tok", "ident_bf"], writes=[("ps", 6)])
                P.add("act", lambda e: e.activation(out=mixT[:, 0:4, i * 128:(i + 1) * 128],
                                                    in_=pT6[:, 0:512].rearrange("p (a b) -> p a b", a=4),
                                                    func=AF.Copy),
                      reads=[("ps", 6)], writes=[("mixT_a", i)])

            pend_attn.append(attention)
            queue_topk()

            xk = xbcT[k2]
            for cc in range(6):
                bank = 0 if cc < 4 else 1
                col = (cc % 4) * 128
                for k in range(4):
                    P.add("pe", lambda e, cc=cc, k=k, bank=bank, col=col, xk=xk: e.matmul(
                        out=ps[bank][:, col:col + 128], lhsT=xk[:, cc, k:k + 128], rhs=Dg[:, cc, k, :],
                        start=(k == 0), stop=False),
                        reads=[("xbcT", k2), "Dg"], writes=[("ps", bank)])
                P.add("pe", lambda e, cc=cc, bank=bank, col=col: e.matmul(
                    out=ps[bank][:, col:col + 128], lhsT=ones_bf[0:1, :], rhs=convb_bf[0:1, cc * 128:(cc + 1) * 128],
                    start=False, stop=True),
                    reads=["ones_bf", "convb_bf"], writes=[("ps", bank)])
            P.add("act", lambda e: e.activation(out=flat(xs_tok), in_=ps[0][:], func=AF.Silu),
                  reads=[("ps", 0)], writes=["xs_tok"])
            P.add("act", lambda e: e.activation(out=B_tok, in_=ps[1][:, 0:256], func=AF.Silu),
                  reads=[("ps", 1)], writes=["B_tok"])
            for c4 in range(4):
                cc = 4 + c4
                for k in range(4):
                    P.add("pe", lambda e, cc=cc, c4=c4, k=k, xk=xk: e.matmul(
                        out=ps[2][:, c4 * 128:(c4 + 1) * 128], lhsT=Dg[:, cc, k, :], rhs=xk[:, cc, k:k + 128],
                        start=(k == 0), stop=(k == 3)),
                        reads=[("xbcT", k2), "Dg"], writes=[("ps", 2)])
                P.add("act", lambda e, cc=cc, c4=c4: e.activation(
                    out=BCT[:, c4, :], in_=ps[2][:, c4 * 128:(c4 + 1) * 128], func=AF.Silu,
                    bias=pp[:, PP_CONVB + cc:PP_CONVB + cc + 1]),
                    reads=[("ps", 2), "pp"], writes=["BCT"])
            P.add("pool", lambda e, k2=k2: e.tensor_tensor(out=dt_sb, in0=dtraw[k2], in1=rows[:, R_DTB:R_DTB + 8],
                                                           op=ALU.add),
                  reads=[("dtraw", k2), "rows"], writes=["dt_sb"])
            P.add("act", lambda e: e.activation(out=dt_sb, in_=dt_sb, func=AF.Exp), reads=["dt_sb"], writes=["dt_sb"])
            P.add("act", lambda e: e.activation(out=dt_sb, in_=dt_sb, func=AF.Ln, bias=1.0),
                  reads=["dt_sb"], writes=["dt_sb"])
            P.add("pool", lambda e: e.tensor_tensor(out=a_sb, in0=dt_sb, in1=A_b, op=ALU.mult),
                  reads=["dt_sb", "A_b"], writes=["a_sb"])
            for g in range(2):
                P.add("pe", lambda e, g=g: e.matmul(out=ps[3][:, g * 128:(g + 1) * 128], lhsT=BCT[:, g, :],
                                                    rhs=BCT[:, 2 + g, :], start=True, stop=True),
                      reads=["BCT"], writes=[("ps", 3)])
            P.add("pe", lambda e: e.matmul(out=ps[3][:, 256:264], lhsT=consts[:, CO_U:CO_U + 128], rhs=a_sb,
                                           start=True, stop=True),
                  reads=["consts", "a_sb"], writes=[("ps", 3)])
            P.add("pe", lambda e: e.matmul(out=ps[3][:, 264:272], lhsT=consts[:, CO_ONES:CO_ONES + 128], rhs=a_sb,
                                           start=True, stop=True),
                  reads=["consts", "a_sb"], writes=[("ps", 3)])
            P.add("act", lambda e: e.activation(out=flat(GT_sb), in_=ps[3][:, 0:256], func=AF.Copy),
                  reads=[("ps", 3)], writes=["GT_sb"])
            P.add("act", lambda e: e.activation(out=cst, in_=ps[3][:, 256:272], func=AF.Copy),
                  reads=[("ps", 3)], writes=["cst"])
            P.add("act", lambda e: e.activation(out=ecs, in_=cst[:, 0:8], func=AF.Exp), reads=["cst"], writes=["ecs"])
            P.add("act", lambda e: e.activation(out=dtot, in_=cst[:, 8:16], func=AF.Exp), reads=["cst"], writes=["dtot"])
            P.add("pool", lambda e: e.tensor_tensor(out=dte, in0=cst[:, 8:16], in1=cst[:, 0:8], op=ALU.subtract),
                  reads=["cst"], writes=["dte"])
            P.add("act", lambda e: e.activation(out=dte, in_=dte, func=AF.Exp), reads=["dte"], writes=["dte"])
            P.add("pool", lambda e: e.tensor_scalar(out=negcs, in0=cst[:, 0:8], scalar1=-1.0, scalar2=1.0, op0=ALU.mult,
                                                   op1=ALU.mult),
                  reads=["cst"], writes=["negcs"])
            Ub = consts[:, CO_U:CO_U + 128].unsqueeze(1).broadcast_to([128, 8, 128])
            ab = a_sb.unsqueeze(2).broadcast_to([128, 8, 128])
            P.add("pool", lambda e, Ub=Ub, ab=ab: e.tensor_tensor(out=rL, in0=Ub, in1=ab, op=ALU.mult),
                  reads=["consts", "a_sb"], writes=["rL"])
            for b2 in range(2):
                P.add("pe", lambda e, b2=b2: e.matmul(out=ps[4 + b2][:], lhsT=consts[:, CO_ONES:CO_ONES + 128],
                                                      rhs=rL[:, 4 * b2:4 * b2 + 4, :], start=True, stop=False),
                      reads=["consts", "rL"], writes=[("ps", 4 + b2)])
                P.add("pe", lambda e, b2=b2: e.matmul(out=ps[4 + b2][:], lhsT=ident_bf, rhs=flat(negl4_bf),
                                                      start=False, stop=True),
                      reads=["ident_bf", "negl4"], writes=[("ps", 4 + b2)])
            for h in range(8):
                b2, hh = h // 4, h % 4
                P.add("act", lambda e, h=h, b2=b2, hh=hh: e.activation(
                    out=rL[:, h, :], in_=ps[4 + b2][:, hh * 128:(hh + 1) * 128], func=AF.Exp,
                    bias=negcs[:, h:h + 1]),
                    reads=[("ps", 4 + b2), "negcs"], writes=["rL"])
            for b2 in range(2):
                gtb = GT_sb[:, b2, :].unsqueeze(1).broadcast_to([128, 4, 128])
                P.add("pool", lambda e, b2=b2, gtb=gtb: e.tensor_tensor(out=WT[:, 4 * b2:4 * b2 + 4, :],
                                                                       in0=rL[:, 4 * b2:4 * b2 + 4, :], in1=gtb,
                                                                       op=ALU.mult),
                      reads=["rL", "GT_sb"], writes=[("WT", b2)])
            dtb = dt_sb.unsqueeze(2).broadcast_to([128, 8, 64])
            dteb = dte.unsqueeze(2).broadcast_to([128, 8, 64])
            P.add("pool", lambda e, dtb=dtb: e.tensor_tensor(out=X_sb, in0=xs_tok, in1=dtb, op=ALU.mult),
                  reads=["xs_tok", "dt_sb"], writes=["X_sb"])
            P.add("pool", lambda e, dteb=dteb: e.tensor_tensor(out=Xd_sb, in0=X_sb, in1=dteb, op=ALU.mult),
                  reads=["X_sb", "dte"], writes=["Xd_sb"])
            for h in range(8):
                P.add("pe", lambda e, h=h: e.matmul(out=ps[6][:, h * 64:(h + 1) * 64], lhsT=WT[:, h, :], rhs=X_sb[:, h, :],
                                                    start=True, stop=True),
                      reads=[("WT", h // 4), "X_sb"], writes=[("ps", 6)])
            for g in range(2):
                P.add("pe", lambda e, g=g: e.matmul(out=ps[7][:, g * 256:(g + 1) * 256], lhsT=BCT[:, 2 + g, :],
                                                    rhs=Hbf[:, 4 * g:4 * g + 4, :], start=True, stop=True),
                      reads=["BCT", "Hbf"], writes=[("ps", 7)])
            P.add("act", lambda e: e.activation(out=flat(yoff_sb), in_=ps[7][:], func=AF.Copy),
                  reads=[("ps", 7)], writes=["yoff_sb"])
            ecsb = ecs.unsqueeze(2).broadcast_to([128, 8, 64])
            dskb = rows[:, R_DSKIP:R_DSKIP + 8].unsqueeze(2).broadcast_to([128, 8, 64])
            P.add("pool", lambda e, ecsb=ecsb: e.tensor_tensor(out=yoff_sb, in0=yoff_sb, in1=ecsb, op=ALU.mult),
                  reads=["yoff_sb", "ecs"], writes=["yoff_sb"])
            P.add("pool", lambda e, dskb=dskb: e.tensor_tensor(out=t2_sb, in0=xs_tok, in1=dskb, op=ALU.mult),
                  reads=["xs_tok", "rows"], writes=["t2_sb"])
            P.add("pool", lambda e: e.tensor_tensor(out=yoff_sb, in0=yoff_sb, in1=t2_sb, op=ALU.add),
                  reads=["yoff_sb", "t2_sb"], writes=["yoff_sb"])
            P.add("act", lambda e: e.activation(out=y_sb, in_=ps[6][:], func=AF.Copy),
                  reads=[("ps", 6)], writes=["y_sb"])
            P.add("pool", lambda e: e.tensor_tensor(out=y_sb, in0=y_sb, in1=flat(yoff_sb), op=ALU.add),
                  reads=["y_sb", "yoff_sb"], writes=["y_sb"])
            P.add("pool", lambda e, k2=k2: e.tensor_tensor(out=y_sb, in0=y_sb, in1=sz[k2], op=ALU.mult),
                  reads=["y_sb", ("sz", k2)], writes=["y_sb"])
            for g in range(2):
                P.add("act", lambda e, g=g: e.activation(out=flat(t2_sb)[:, g * 256:(g + 1) * 256],
                                                         in_=y_sb[:, g * 256:(g + 1) * 256],
                                                         func=AF.Square, accum_out=ssg[:, g:g + 1]),
                      reads=["y_sb"], writes=[("ssg", g), "t2_sb"])
            P.add("act", lambda e: e.activation(out=sdg, in_=ssg, func=AF.Ln, scale=1.0 / 256,
                                                bias=pp[:, PP_EPS:PP_EPS + 1]),
                  reads=[("ssg", 0), ("ssg", 1), "pp"], writes=["sdg"])
            P.add("act", lambda e: e.activation(out=rsg, in_=sdg, func=AF.Exp, scale=-0.5), reads=["sdg"], writes=["rsg"])
            for g in range(2):
                P.add("act", lambda e, g=g: e.activation(out=flat(t2_sb)[:, g * 256:(g + 1) * 256],
                                                         in_=y_sb[:, g * 256:(g + 1) * 256], func=AF.Copy,
                                                         scale=rsg[:, g:g + 1]),
                      reads=["y_sb", "rsg"], writes=["t2_sb"])
                P.add("pool", lambda e, g=g: e.tensor_tensor(
                    out=ssd_tok[:, g * 256:(g + 1) * 256], in0=flat(t2_sb)[:, g * 256:(g + 1) * 256],
                    in1=rows[:, R_SSDNW + g * 256:R_SSDNW + (g + 1) * 256], op=ALU.mult),
                    reads=["t2_sb", "rows"], writes=["ssd_tok"])
            pT7 = ps[7][:].bitcast(BF16)
            for c4 in range(4):
                P.add("pe", lambda e, c4=c4: e.transpose(out=pT7[:, c4 * 128:(c4 + 1) * 128],
                                                         in_=ssd_tok[:, c4 * 128:(c4 + 1) * 128], identity=ident_bf),
                      reads=["ssd_tok", "ident_bf"], writes=[("ps", 7)])
            P.add("act", lambda e, i=i: e.activation(out=mixT[:, 4:8, i * 128:(i + 1) * 128],
                                                     in_=pT7[:, 0:512].rearrange("p (a b) -> p a b", a=4), func=AF.Copy),
                  reads=[("ps", 7)], writes=[("mixT_s", i)])
            for g in range(2):
                P.add("pe", lambda e, g=g: e.matmul(out=ps[0][:, g * 256:(g + 1) * 256], lhsT=B_tok[:, g * 128:(g + 1) * 128],
                                                    rhs=Xd_sb[:, 4 * g:4 * g + 4, :], start=True, stop=True),
                      reads=["B_tok", "Xd_sb"], writes=[("ps", 0)])
            dtotb = dtot.unsqueeze(2).broadcast_to([128, 8, 64])
            P.add("pool", lambda e, dtotb=dtotb: e.tensor_tensor(out=Ht_sb, in0=H_sb, in1=dtotb, op=ALU.mult),
                  reads=["H", "dtot"], writes=["t2_sb"])
            P.add("act", lambda e: e.activation(out=flat(yoff_sb), in_=ps[0][:], func=AF.Copy),
                  reads=[("ps", 0)], writes=["yoff_sb"])
            P.add("pool", lambda e: e.tensor_tensor(out=flat(H_sb), in0=flat(yoff_sb), in1=flat(Ht_sb), op=ALU.add),
                  reads=["yoff_sb", "t2_sb"], writes=["H"])
            P.add("act", lambda e: e.activation(out=flat(Hbf), in_=flat(H_sb), func=AF.Copy),
                  reads=["H"], writes=["Hbf"])
            if len(pend_attn) == 2:
                pend_attn.pop(0)()
            P.bg_flush()
            while finalizers:
                finalizers.pop(0)()
        P.bg_flush()
        while finalizers:
            finalizers.pop(0)()
        if dbg is not None and s == 0:
            dump("negm", negm, [128, L], reads=["negm"])
            dump("S", S2[(nt - 1) % 2], [128, L], reads=[("S", (nt - 1) % 2, c) for c in range((L + 511) // 512)])
            dump("thf", small[:, 160:164], [128, 4], reads=["thf", "negth", "sgn", "sg2"])
        while pend_attn:
            pend_attn.pop(0)()
        if dbg is not None and s == 0:
            dump("mixT", mixT, [128, 8, L], reads=[("mixT_a", jj) for jj in range(nt)] + [("mixT_s", jj) for jj in range(nt)])
        barrier()
        if stop_after == "M":
            continue

        dma("sp", wo_sb, wo_bf, [], ["wo_sb"], "wo_sb")
        dma("sp", gpm, gpost_d[0:1, :].partition_broadcast(128), [], ["gpm"], "gpm")
        dma("sp", gpl, gpost_d[1:2, :].partition_broadcast(128), [], ["gpl"], "gpl")
        for c in range(nt // 4):
            for tt in range(4):
                i = 4 * c + tt
                k2 = i % 2
                r0 = s * L + i * 128
                dma("sp", xt[k2], x_d[r0:r0 + 128, :], [], [("xt", k2)], f"xt{k2}")
                for nh in range(2):
                    for cc in range(8):
                        P.add("pe", lambda e, nh=nh, cc=cc, i=i: e.matmul(
                            out=ps[nh][:], lhsT=mixT[:, cc, i * 128:(i + 1) * 128],
                            rhs=wo_sb[:, cc, nh * 512:(nh + 1) * 512], start=(cc == 0), stop=(cc == 7)),
                            reads=[("mixT_a", i), ("mixT_s", i), "wo_sb"], writes=[("ps", nh)])
                    P.add("act", lambda e, nh=nh, k2=k2: e.activation(out=junkF[:, nh * 512:(nh + 1) * 512], in_=ps[nh][:], func=AF.Square,
                                                                      accum_out=ssa[k2][:, nh:nh + 1]),
                          reads=[("ps", nh)], writes=[("ssa", k2, nh), ("junkF", nh)])
                P.add("dve", lambda e, k2=k2: e.tensor_tensor(out=ssq[k2], in0=ssa[k2][:, 0:1], in1=ssa[k2][:, 1:2],
                                                              op=ALU.add),
                      reads=[("ssa", k2, 0), ("ssa", k2, 1)], writes=[("ssq", k2)])
                P.add("act", lambda e, k2=k2: e.activation(out=sq2[k2], in_=ssq[k2], func=AF.Sqrt, scale=1.0 / 1024,
                                                           bias=pp[:, PP_EPS:PP_EPS + 1]),
                      reads=[("ssq", k2), "pp"], writes=[("sq2", k2)])
                P.add("dve", lambda e, k2=k2: e.reciprocal(out=r1[k2], in_=sq2[k2]), reads=[("sq2", k2)],
                      writes=[("r1", k2)])
                for nh in range(2):
                    P.add("dve", lambda e, nh=nh, k2=k2, tt=tt: e.scalar_tensor_tensor(
                        out=h1[:, tt, nh * 512:(nh + 1) * 512], in0=ps[nh][:], scalar=r1[k2],
                        in1=gpm[:, nh * 512:(nh + 1) * 512], op0=ALU.mult, op1=ALU.mult),
                        reads=[("ps", nh), ("r1", k2), "gpm"], writes=[("h1", tt)])
                P.add("pool", lambda e, k2=k2, tt=tt: e.tensor_tensor(out=h1[:, tt, :], in0=h1[:, tt, :], in1=xt[k2],
                                                                      op=ALU.add),
                      reads=[("h1", tt), ("xt", k2)], writes=[("h1", tt)])
                P.add("act", lambda e, k2=k2, tt=tt: e.activation(out=hn[k2], in_=h1[:, tt, :], func=AF.Square,
                                                                  accum_out=ss2[k2]),
                      reads=[("h1", tt)], writes=[("ss2", k2), ("hn", k2)])
                P.add("act", lambda e, k2=k2: e.activation(out=sd2[k2], in_=ss2[k2], func=AF.Sqrt, scale=1.0 / 1024,
                                                           bias=pp[:, PP_EPS:PP_EPS + 1]),
                      reads=[("ss2", k2), "pp"], writes=[("sd2", k2)])
                P.add("dve", lambda e, k2=k2: e.reciprocal(out=r2[k2], in_=sd2[k2]), reads=[("sd2", k2)],
                      writes=[("r2", k2)])
                P.add("act", lambda e, k2=k2, tt=tt: e.activation(out=hn[k2], in_=h1[:, tt, :], func=AF.Copy,
                                                                  scale=r2[k2]),
                      reads=[("h1", tt), ("r2", k2)], writes=[("hn", k2)])
                pT2 = ps[2][:].bitcast(BF16)
                for kc in range(8):
                    P.add("pe", lambda e, k2=k2, kc=kc: e.transpose(out=pT2[:, kc * 128:(kc + 1) * 128],
                                                                     in_=hn[k2][:, kc * 128:(kc + 1) * 128],
                                                                     identity=ident_bf),
                          reads=[("hn", k2), "ident_bf"], writes=[("ps", 2)])
                P.add("act", lambda e, tt=tt: e.activation(out=hnT[:, :, tt * 128:(tt + 1) * 128],
                                                           in_=pT2.rearrange("p (a b) -> p a b", a=8), func=AF.Copy),
                      reads=[("ps", 2)], writes=[("hnT", tt)])
            ub = 0
            for fg in range(8):
                wk = fg % 2
                dma("sp", wu_sb[wk], wu_bf[:, :, fg * 512:(fg + 1) * 512], [], [("wu", wk)], f"wu{wk}")
                for f4 in range(4):
                    fc = fg * 4 + f4
                    bank = 3 + (ub % 4)
                    rk = ub % 2
                    ub += 1
                    for kc in range(8):
                        P.add("pe", lambda e, wk=wk, f4=f4, kc=kc, bank=bank: e.matmul(
                            out=ps[bank][:], lhsT=wu_sb[wk][:, kc, f4 * 128:(f4 + 1) * 128], rhs=hnT[:, kc, :],
                            start=(kc == 0), stop=(kc == 7)),
                            reads=[("wu", wk)] + [("hnT", t4) for t4 in range(4)], writes=[("ps", bank)])
                    P.add("act", lambda e, bank=bank, rk=rk: e.activation(out=r32[rk], in_=ps[bank][:], func=AF.Relu),
                          reads=[("ps", bank)], writes=[("r32", rk)])
                    P.add("pool", lambda e, fc=fc, rk=rk: e.tensor_tensor(out=aT[:, fc, :], in0=r32[rk], in1=r32[rk],
                                                                          op=ALU.mult),
                          reads=[("r32", rk)], writes=[("aT", fc)])
            for dg in range(8):
                wk = dg % 2
                dma("sp", wd_sb[wk], wd_bf[:, dg * 4:(dg + 1) * 4, :], [], [("wd", wk)], f"wd{wk}")
                for tt in range(4):
                    for nh in range(2):
                        for f4 in range(4):
                            fc = dg * 4 + f4
                            P.add("pe", lambda e, wk=wk, tt=tt, nh=nh, f4=f4, fc=fc, dg=dg: e.matmul(
                                out=ps[2 * tt + nh][:], lhsT=aT[:, fc, tt * 128:(tt + 1) * 128],
                                rhs=wd_sb[wk][:, f4, nh * 512:(nh + 1) * 512],
                                start=(dg == 0 and f4 == 0), stop=(dg == 7 and f4 == 3)),
                                reads=[("wd", wk), ("aT", fc)], writes=[("ps", 2 * tt + nh)])
            for tt in range(4):
                i = 4 * c + tt
                k2 = i % 2
                r0 = s * L + i * 128
                for nh in range(2):
                    P.add("act", lambda e, nh=nh, k2=k2, tt=tt: e.activation(
                        out=junkF[:, nh * 512:(nh + 1) * 512], in_=ps[2 * tt + nh][:], func=AF.Square,
                        accum_out=ssc[k2][:, nh:nh + 1]),
                        reads=[("ps", 2 * tt + nh)], writes=[("ssc", k2, nh), ("junkF", nh)])
                P.add("dve", lambda e, k2=k2: e.tensor_tensor(out=ss3[k2], in0=ssc[k2][:, 0:1], in1=ssc[k2][:, 1:2],
                                                              op=ALU.add),
                      reads=[("ssc", k2, 0), ("ssc", k2, 1)], writes=[("ss3", k2)])
                P.add("act", lambda e, k2=k2: e.activation(out=sd3[k2], in_=ss3[k2], func=AF.Sqrt, scale=1.0 / 1024,
                                                           bias=pp[:, PP_EPS:PP_EPS + 1]),
                      reads=[("ss3", k2), "pp"], writes=[("sd3", k2)])
                P.add("dve", lambda e, k2=k2: e.reciprocal(out=r3[k2], in_=sd3[k2]), reads=[("sd3", k2)],
                      writes=[("r3", k2)])
                for nh in range(2):
                    P.add("dve", lambda e, nh=nh, k2=k2, tt=tt: e.scalar_tensor_tensor(
                        out=ot[k2][:, nh * 512:(nh + 1) * 512], in0=ps[2 * tt + nh][:], scalar=r3[k2],
                        in1=gpl[:, nh * 512:(nh + 1) * 512], op0=ALU.mult, op1=ALU.mult),
                        reads=[("ps", 2 * tt + nh), ("r3", k2), "gpl"], writes=[("ot", k2)])
                P.add("pool", lambda e, k2=k2, tt=tt: e.tensor_tensor(out=ot[k2], in0=ot[k2], in1=h1[:, tt, :], op=ALU.add),
                      reads=[("ot", k2), ("h1", tt)], writes=[("ot", k2)])
                dma("sp", out_d[r0:r0 + 128, :], ot[k2], [("ot", k2)], [("ot", k2)], f"ot{k2}")
        barrier()

    final_keys = ["dbg_" + n for n in dbg_out] + ["ot0", "ot1"]
    P.emit(es, final_wait_keys=final_keys)
    es.close()
    if dbg is not None:
        dbg.update(dbg_out)
    return nc


PP_GMIX = 0
PP_GMLP = 8
PP_EPS = 16
PP_LNW = 17
PP_LNB = 18
PP_CONVW = 19
PP_CONVB = 51
PP_N = 59
R_DTB = 0
R_ALOG = 8
R_DSKIP = 16
R_SSDNW = 24
ROWS_N = 536


def _host_inputs(inputs, core, nseq, L):
    f = lambda a: np.ascontiguousarray(np.asarray(a, dtype=np.float32))
    x = f(inputs["x"])
    xs = x[core * nseq:(core + 1) * nseq, :L].reshape(nseq * L, D_MODEL)
    w_in = f(inputs["w_in"])[0][:, _w_in_perm()]
    pp = np.zeros((128, PP_N), np.float32)
    pp[:, PP_GMIX:PP_GMIX + 8] = f(inputs["norm_pre_mix"])[0].reshape(8, 128).T
    pp[:, PP_GMLP:PP_GMLP + 8] = f(inputs["norm_pre_mlp"])[0].reshape(8, 128).T
    pp[:, PP_EPS] = EPS
    pp[:, PP_LNW] = np.tile(f(inputs["k_idx_ln_w"])[0], 2)
    pp[:, PP_LNB] = np.tile(f(inputs["k_idx_ln_b"])[0], 2)
    cw = f(inputs["conv_w"])[0]
    pp[:, PP_CONVW:PP_CONVW + 32] = cw.reshape(4, 8, 128).transpose(2, 1, 0).reshape(128, 32)
    pp[:, PP_CONVB:PP_CONVB + 8] = f(inputs["conv_b"])[0].reshape(8, 128).T
    rows = np.zeros((1, ROWS_N), np.float32)
    rows[0, R_DTB:R_DTB + 8] = f(inputs["dt_bias"])[0]
    rows[0, R_ALOG:R_ALOG + 8] = f(inputs["a_log"])[0]
    rows[0, R_DSKIP:R_DSKIP + 8] = f(inputs["d_skip"])[0]
    rows[0, R_SSDNW:R_SSDNW + 512] = f(inputs["ssd_norm_w"])[0]
    return {
        "x": np.ascontiguousarray(xs),
        "w_in": np.ascontiguousarray(w_in),
        "w_out": f(inputs["w_out"])[0],
        "w_up": f(inputs["w_mlp_up"])[0],
        "w_down": f(inputs["w_mlp_down"])[0],
        "consts": _consts(),
        "pp": pp,
        "rows": rows,
        "gpost": np.ascontiguousarray(np.stack([f(inputs["norm_post_mix"])[0], f(inputs["norm_post_mlp"])[0]], 0)),
        "convb": f(inputs["conv_b"])[0].reshape(1, 1024),
        "rel_bias": f(inputs["rel_bias"]),
    }


def kernel(**inputs):
    nseq, nt = 2, 16
    nc = build(nseq=nseq, nt=nt)
    in_maps = [_host_inputs(inputs, c, nseq, nt * 128) for c in range(N_CORES)]
    res = run_bass_kernel_spmd(nc, in_maps, core_ids=list(range(N_CORES)))
    outs = [np.asarray(r["out"]).reshape(nseq, nt * 128, D_MODEL) for r in res.results]
    return np.concatenate(outs, axis=0).astype(np.float32)
```

```python
import numpy as np
from contextlib import ExitStack
import concourse.bass as bass
import concourse.mybir as mybir
from concourse.bass_utils import run_bass_kernel_spmd

F32 = mybir.dt.float32
BF16 = mybir.dt.bfloat16
AF = mybir.ActivationFunctionType
ALU = mybir.AluOpType
AX = mybir.AxisListType

D_MODEL = 1024
L_FULL = 2048
N_CORES = 8
EPS = 1e-6
NEG_BIG = -1.0e30

OFF_Q, OFF_K, OFF_V, OFF_QI, OFF_KI, OFF_WI, OFF_Z, OFF_XBC, OFF_DT = (
    0, 512, 640, 768, 1024, 1088, 1092, 1604, 2628)
N_FM = 17
C_TM1 = N_FM * 128
C_Z = C_TM1 + 140
W_IN_COLS = C_Z + 512


def _w_in_perm():
    cols = []
    for p in range(4):
        g, j = p // 2, p % 2
        for h in (4 * g + j, 4 * g + 2 + j):
            cols += list(range(OFF_Q + 64 * h, OFF_Q + 64 * h + 64))
    for g in range(2):
        for _ in range(2):
            cols += list(range(OFF_K + 64 * g, OFF_K + 64 * g + 64))
    for p in range(2):
        for h in (2 * p, 2 * p + 1):
            cols += list(range(OFF_QI + 64 * h, OFF_QI + 64 * h + 64))
    for _ in range(2):
        cols += list(range(OFF_KI, OFF_KI + 64))
    cols += list(range(OFF_XBC, OFF_XBC + 1024))
    cols += list(range(OFF_V, OFF_V + 128))
    cols += list(range(OFF_WI, OFF_WI + 4))
    cols += list(range(OFF_DT, OFF_DT + 8))
    cols += list(range(OFF_Z, OFF_Z + 512))
    assert len(cols) == W_IN_COLS
    return np.array(cols, dtype=np.int64)


def _t5_bucket_np(d):
    d = np.asarray(d)
    max_exact = 16
    df = np.maximum(d, 1).astype(np.float32)
    large = max_exact + (np.log(df / max_exact) / np.float32(np.log(128 / max_exact))
                         * (32 - max_exact)).astype(np.int32)
    large = np.minimum(large, 31)
    return np.where(d < max_exact, d, large)


CO_IDENT = 0
CO_U = 128
CO_NEGTRI = 256
CO_NEGL = 384
CO_BD64 = 512
CO_PERT = 640
CO_OH = 640 + 2048
CO_ONES = CO_OH + 384
CO_NU = CO_ONES + 128
CO_N = CO_NU + 128


def _consts():
    c = np.zeros((128, CO_N), np.float32)
    idx = np.arange(128)
    c[:, CO_IDENT:CO_IDENT + 128] = np.eye(128, dtype=np.float32)
    c[:, CO_U:CO_U + 128] = (idx[:, None] <= idx[None, :]).astype(np.float32)
    c[:, CO_NEGTRI:CO_NEGTRI + 128] = np.where(idx[None, :] > idx[:, None], NEG_BIG, 0.0)
    c[:, CO_NEGL:CO_NEGL + 128] = np.where(idx[None, :] < idx[:, None], -30000.0, 0.0)
    bd = np.zeros((128, 128), np.float32)
    bd[:64, :64] = 1.0 / 64
    bd[64:, 64:] = 1.0 / 64
    c[:, CO_BD64:CO_BD64 + 128] = bd
    c[:, CO_PERT:CO_PERT + 2048] = (-(2.0 ** -23) * (np.arange(2048) + 1)).astype(np.float32)[None, :]
    m = np.arange(383)
    b = _t5_bucket_np(np.maximum(m - 127, 0))
    oh = np.zeros((32, 384), np.float32)
    oh[b, m] = 1.0
    c[:32, CO_OH:CO_OH + 384] = oh
    c[:, CO_ONES:CO_ONES + 128] = 1.0
    c[:, CO_NU:CO_NU + 128] = np.eye(128, dtype=np.float32) - bd
    return c


class _Op:
    __slots__ = ("eng", "fn", "deps", "flag", "is_dma", "key", "tick")

    def __init__(self, eng, fn, is_dma, key):
        self.eng = eng
        self.fn = fn
        self.deps = []
        self.flag = False
        self.is_dma = is_dma
        self.key = key
        self.tick = 0


class Prog:
    ENGS = ("pe", "act", "dve", "pool", "sp")
    EPOCH = 12000

    def __init__(self, nc):
        self.nc = nc
        self.q = {e: [] for e in self.ENGS}
        self.last_w = {}
        self.readers = {}
        self.dma_ops = []
        self.bg = {}
        self.bg_rate = {"dve": 6, "act": 3}

    def bg_push(self, eng, fn, reads=(), writes=()):
        self.bg.setdefault(eng, []).append((fn, reads, writes))

    def bg_flush(self, eng=None, n=None):
        for e in ([eng] if eng else list(self.bg.keys())):
            q = self.bg.get(e, [])
            k = 0
            while q and (n is None or k < n):
                fn, reads, writes = q.pop(0)
                self._add(e, fn, reads, writes, None)
                k += 1

    def add(self, eng, fn, reads=(), writes=(), dma_key=None):
        if self.bg.get(eng):
            self.bg_flush(eng, self.bg_rate.get(eng, 2))
        return self._add(eng, fn, reads, writes, dma_key)

    def _add(self, eng, fn, reads=(), writes=(), dma_key=None):
        is_dma = dma_key is not None
        op = _Op(eng, fn, is_dma, dma_key)
        cand = {}
        for t in reads:
            w = self.last_w.get(t)
            if w is not None:
                cand[id(w)] = (w, True)
        for t in writes:
            w = self.last_w.get(t)
            if w is not None and id(w) not in cand:
                cand[id(w)] = (w, False)
            for r in self.readers.get(t, {}).values():
                if id(r) not in cand:
                    cand[id(r)] = (r, False)
        for d, raw in cand.values():
            if d is op:
                continue
            keep = True
            if not d.is_dma and d.eng == eng:
                keep = (eng in ("act", "dve", "pool")) or (is_dma and eng != "sp")
            if keep:
                d.flag = True
                op.deps.append(d)
        rk = ("dma", id(op)) if is_dma else eng
        for t in reads:
            self.readers.setdefault(t, {})[rk] = op
        for t in writes:
            self.last_w[t] = op
            self.readers[t] = {}
        self.q[eng].append(op)
        if is_dma:
            self.dma_ops.append(op)
        return op

    def emit(self, es, final_wait_keys=()):
        nc = self.nc
        n_epochs = {}
        for e in self.ENGS:
            cnt = 0
            for op in self.q[e]:
                if op.is_dma:
                    continue
                if op.flag:
                    cnt += 1
                    op.tick = cnt
            n_epochs[e] = (cnt + self.EPOCH - 1) // self.EPOCH
        dma_cnt = {}
        for e in self.ENGS:
            for op in self.q[e]:
                if op.is_dma:
                    dma_cnt[op.key] = dma_cnt.get(op.key, 0) + 1
                    op.tick = dma_cnt[op.key] * 16
        sems = {}
        for e in self.ENGS:
            for k in range(n_epochs[e]):
                sems[(e, k)] = es.enter_context(nc.semaphore(f"s_{e}_{k}"))
        for k in dma_cnt:
            sems[("dma", k)] = es.enter_context(nc.semaphore(f"d_{k}"))

        def ev(op):
            if op.is_dma:
                return sems[("dma", op.key)], op.tick
            k = (op.tick - 1) // self.EPOCH
            return sems[(op.eng, k)], op.tick - k * self.EPOCH

        block = es.enter_context(nc.Block())

        def run(eng_name, eng):
            waited = {}
            for op in self.q[eng_name]:
                need = {}
                for d in op.deps:
                    s, v = ev(d)
                    if need.get(s.num, (None, 0))[1] < v:
                        need[s.num] = (s, v)
                for sn, (s, v) in need.items():
                    if waited.get(sn, 0) < v:
                        eng.wait_ge(s, v)
                        waited[sn] = v
                ins = op.fn(eng)
                if op.is_dma:
                    s, _ = ev(op)
                    ins.then_inc(s, 16)
                elif op.flag:
                    s, _ = ev(op)
                    ins.then_inc(s, 1)
            if eng_name == "sp":
                for k in final_wait_keys:
                    if k in dma_cnt:
                        eng.wait_ge(sems[("dma", k)], dma_cnt[k] * 16)

        @block.tensor
        def _(e):
            run("pe", e)

        @block.scalar
        def _(e):
            run("act", e)

        @block.vector
        def _(e):
            run("dve", e)

        @block.gpsimd
        def _(e):
            run("pool", e)

        @block.sync
        def _(e):
            run("sp", e)


I8 = mybir.dt.int8
_DT_SIZE = {F32: 4, BF16: 2, I8: 1}


def build(nseq=2, nt=16, dbg=None, stop_after=None, act_from=99, bis_from=4):
    ACT_FROM = act_from
    BIS_FROM = bis_from
    L = nt * 128
    assert nt % 4 == 0
    topk = min(256, L // 4)
    ntok = nseq * L
    nc = bass.Bass("TRN2", target_bir_lowering=False)
    es = ExitStack()
    P = Prog(nc)

    def dram_in(name, shape, dt=F32):
        return nc.dram_tensor(name, list(shape), dt, kind="ExternalInput").ap()

    x_d = dram_in("x", [ntok, D_MODEL])
    win_d = dram_in("w_in", [D_MODEL, W_IN_COLS])
    wout_d = dram_in("w_out", [1024, 1024])
    wup_d = dram_in("w_up", [1024, 4096])
    wdn_d = dram_in("w_down", [4096, 1024])
    consts_d = dram_in("consts", [128, CO_N])
    pp_d = dram_in("pp", [128, PP_N])
    rows_d = dram_in("rows", [1, ROWS_N])
    gpost_d = dram_in("gpost", [2, 1024])
    convb_d = dram_in("convb", [1, 1024])
    relb_d = dram_in("rel_bias", [32, 8])
    out_d = nc.dram_tensor("out", [ntok, D_MODEL], F32, kind="ExternalOutput").ap()

    wi_bf = nc.dram_tensor("wi_bf", [128, 8, W_IN_COLS], BF16).ap()
    wo_bf = nc.dram_tensor("wo_bf", [128, 8, 1024], BF16).ap()
    wu_bf = nc.dram_tensor("wu_bf", [128, 8, 4096], BF16).ap()
    wd_bf = nc.dram_tensor("wd_bf", [128, 32, 1024], BF16).ap()
    E_d = nc.dram_tensor("E_d", [8, 128 * 384], F32)

    AW = 53200
    arena = es.enter_context(nc.sbuf_tensor("arena", [128, AW], F32))
    cur = [0]

    def take(dt, shape):
        nfree = int(np.prod(shape[1:]))
        nbytes = (nfree * _DT_SIZE[dt] + 63) // 64 * 64
        off = cur[0]
        cur[0] += nbytes
        assert cur[0] <= AW * 4, f"arena overflow {cur[0]} > {AW * 4}"
        v = arena[:, off // 4:(off + nbytes) // 4]
        if dt != F32:
            v = v.bitcast(dt)
        v = v[:, 0:nfree]
        if len(shape) == 3:
            v = v.rearrange("p (a b) -> p a b", a=shape[1])
        elif len(shape) == 4:
            v = v.rearrange("p (a b c) -> p a b c", a=shape[1], b=shape[2])
        if shape[0] < 128:
            v = v[0:shape[0]]
        return v

    def flat(v):
        if len(v.shape) == 3:
            return v.rearrange("p a b -> p (a b)")
        if len(v.shape) == 4:
            return v.rearrange("p a b c -> p (a b c)")
        return v

    consts = take(F32, [128, CO_N])
    pp = take(F32, [128, PP_N])
    rows = take(F32, [128, ROWS_N])
    ident_bf = take(BF16, [128, 128])
    I4 = take(BF16, [128, 4, 128])
    negl4_bf = take(BF16, [128, 4, 128])
    row0 = take(BF16, [1, 1024 + 768 + 128])
    c8row = row0[0:1, 0:1024].rearrange("p (a b) -> p a b", a=8)
    convb_bf = row0[0:1, 1024:1792]
    ones_bf = row0[0:1, 1792:1920]
    B8 = take(BF16, [128, 2, 8, 128])
    Dg = take(BF16, [128, 8, 4, 128])
    A_b = take(F32, [128, 8])
    rb_sb = take(F32, [32, 8])
    r31 = take(F32, [1, 8])
    small = take(F32, [128, 176])

    def sm(a, n):
        return small[:, a:a + n]
    ss = [sm(0, 1), sm(1, 1)]
    sd = [sm(2, 1), sm(3, 1)]
    rstd = [sm(4, 1), sm(5, 1)]
    wI = [sm(8, 4), sm(12, 4)]
    dtraw = [sm(16, 8), sm(24, 8)]
    m8 = [sm(32, 8), sm(40, 8)]
    rec = sm(48, 8)
    dt_sb = sm(56, 8)
    a_sb = sm(64, 8)
    cst = sm(72, 16)
    ecs = sm(88, 8)
    dte = sm(96, 8)
    dtot = sm(104, 8)
    negcs = sm(112, 8)
    ssg = sm(120, 2)
    sdg = sm(122, 2)
    rsg = sm(124, 2)
    ssa = [sm(128, 2), sm(130, 2)]
    ssq = [sm(132, 1), sm(133, 1)]
    sq2 = [sm(134, 1), sm(135, 1)]
    r1 = [sm(136, 1), sm(137, 1)]
    ss2 = [sm(138, 1), sm(139, 1)]
    sd2 = [sm(140, 1), sm(141, 1)]
    r2 = [sm(142, 1), sm(143, 1)]
    ssc = [sm(144, 2), sm(146, 2)]
    ss3 = [sm(148, 1), sm(149, 1)]
    sd3 = [sm(150, 1), sm(151, 1)]
    r3 = [sm(152, 1), sm(153, 1)]
    negth = sm(160, 1)
    sgn = sm(161, 1)
    sg2 = sm(162, 1)
    thf = sm(163, 1)
    lnd = sm(164, 8)
    thb = sm(172, 1)
    cntb = sm(173, 1)
    ubis = sm(174, 1)

    mixT = take(BF16, [128, 8, L])
    xt = [take(F32, [128, 1024])]
    phase_base = cur[0]

    NST = 4
    stage32 = [take(F32, [128, 4096]) for _ in range(NST)]
    stage16 = [take(BF16, [128, 4096]) for _ in range(NST)]
    lhs_h = [take(F32, [32, 128]) for _ in range(2)]
    Rsb = [take(F32, [128, 384]) for _ in range(2)]
    Bt32 = take(F32, [128, 2, 8, 128])
    convb32 = take(F32, [1, 1024])
    cur[0] = phase_base
    w_in_sb = take(BF16, [128, 8, W_IN_COLS])
    kT = take(BF16, [128, 2, L])
    kiT = take(BF16, [128, L])
    v_aug = take(BF16, [128, nt, 2, 65])
    S2 = [take(F32, [128, L]) for _ in range(2)]
    negm = take(BF16, [128, L])
    junk8 = take(I8, [128, L])
    xb = [take(BF16, [128, 1024]) for _ in range(2)]
    uT = [take(BF16, [128, 8, 128]) for _ in range(2)]
    qTz = [take(BF16, [128, 2, 4, 128]) for _ in range(2)]
    qiT = [take(BF16, [128, 2, 128]) for _ in range(2)]
    kiraw = take(F32, [128, 128])
    kicen = take(F32, [128, 128])
    kisq = take(F32, [128, 128])
    kisd = take(F32, [128, 128])
    kirs = take(F32, [128, 128])
    xbcT = [take(BF16, [128, 8, 131]) for _ in range(2)]
    sz = [take(F32, [128, 512]) for _ in range(2)]
    rrelu = [take(F32, [128, 512]) for _ in range(2)]
    E_sb = [take(BF16, [128, 1024]) for _ in range(2)]
    attn_tok = take(BF16, [128, 512])
    xs_tok = take(F32, [128, 8, 64])
    B_tok = take(BF16, [128, 256])
    BCT = take(BF16, [128, 4, 128])
    rL = take(F32, [128, 8, 128])
    GT_sb = take(F32, [128, 2, 128])
    WT = take(BF16, [128, 8, 128])
    X_sb = take(BF16, [128, 8, 64])
    Xd_sb = take(BF16, [128, 8, 64])
    yoff_sb = take(F32, [128, 8, 64])
    t2_sb = take(F32, [128, 8, 64])
    y_sb = take(F32, [128, 512])
    ssd_tok = take(BF16, [128, 512])
    H_sb = take(F32, [128, 8, 64])
    Ht_sb = t2_sb
    Hbf = take(BF16, [128, 8, 64])
    endM = cur[0]

    cur[0] = phase_base
    xt.append(take(F32, [128, 1024]))
    wo_sb = take(BF16, [128, 8, 1024])
    gpm = take(F32, [128, 1024])
    gpl = take(F32, [128, 1024])
    h1 = take(F32, [128, 4, 1024])
    hn = [take(BF16, [128, 1024]) for _ in range(2)]
    hnT = take(BF16, [128, 8, 512])
    aT = take(BF16, [128, 32, 512])
    wu_sb = [take(BF16, [128, 8, 512]) for _ in range(2)]
    wd_sb = [take(BF16, [128, 4, 1024]) for _ in range(2)]
    r32 = [take(F32, [128, 512]) for _ in range(2)]
    ot = [take(F32, [128, 1024]) for _ in range(2)]
    junkF = take(BF16, [128, 1024])
    endF = cur[0]
    print(f"[build] arena: base={phase_base} endM={endM} endF={endF} cap={AW * 4}")

    ps = [es.enter_context(nc.psum_tensor(f"ps{i}", [128, 512], F32)) for i in range(8)]

    dbg_out = {}

    def dump(name, src_ap, shape, reads=()):
        if dbg is None:
            return
        t = nc.dram_tensor("dbg_" + name, list(shape), src_ap.dtype, kind="ExternalOutput").ap()
        dbg_out[name] = (list(shape), src_ap.dtype)
        P.add("sp", lambda e, t=t, s=src_ap: e.dma_start(out=t, in_=s), reads=reads,
              writes=[("dbg", name)], dma_key="dbg_" + name)
        P.dma_tokens.append(("dbg", name))

    bar_n = [0]

    def barrier():
        n = bar_n[0]
        bar_n[0] += 1
        P.add("pe", lambda e: e.matmul(out=ps[7][0:1, 0:2], lhsT=ones_bf[0:1, 0:1], rhs=ones_bf[0:1, 0:2],
                                       start=True, stop=True),
              reads=["ones_bf"], writes=[("ps", 7), ("bar", n, "pe")])
        P.add("act", lambda e: e.activation(out=small[:, 156:157], in_=small[:, 156:157], func=AF.Copy),
              writes=[("bar", n, "act")])
        P.add("dve", lambda e: e.tensor_copy(out=small[:, 157:158], in_=small[:, 157:158]),
              writes=[("bar", n, "dve")])
        P.add("pool", lambda e: e.tensor_copy(out=small[:, 158:159], in_=small[:, 158:159]),
              writes=[("bar", n, "pool")])
        toks = list(P.dma_tokens)
        P.dma_tokens = []
        P.add("sp", lambda e: e.nop(), reads=toks, writes=[("bar", n, "sp")])
        allb = [("bar", n, e) for e in ("pe", "act", "dve", "pool", "sp")]
        P.add("pe", lambda e: e.matmul(out=ps[7][0:1, 0:2], lhsT=ones_bf[0:1, 0:1], rhs=ones_bf[0:1, 0:2],
                                       start=True, stop=True), reads=allb + ["ones_bf"], writes=[("ps", 7)])
        P.add("act", lambda e: e.activation(out=small[:, 156:157], in_=small[:, 156:157], func=AF.Copy), reads=allb)
        P.add("dve", lambda e: e.tensor_copy(out=small[:, 157:158], in_=small[:, 157:158]), reads=allb)
        P.add("pool", lambda e: e.tensor_copy(out=small[:, 158:159], in_=small[:, 158:159]), reads=allb)
        P.add("sp", lambda e: e.nop(), reads=allb)

    def dma(eng, out, in_, reads, writes, key):
        P.add(eng, lambda e: e.dma_start(out=out, in_=in_), reads=reads, writes=writes, dma_key=key)
        P.dma_tokens.extend(writes)

    P.dma_tokens = []

    dma("sp", consts, consts_d, [], ["consts"], "c0")
    dma("sp", pp, pp_d, [], ["pp"], "c1")
    dma("sp", rows, rows_d.partition_broadcast(128), [], ["rows"], "c2")
    dma("sp", rb_sb, relb_d, [], ["rb_sb"], "c3")
    dma("sp", r31, relb_d[31:32, :], [], ["r31"], "c4")
    dma("sp", convb32, convb_d, [], ["convb32"], "c5")
    P.add("dve", lambda e: e.memset(small, 0.0), writes=["small0"])
    P.add("dve", lambda e: e.tensor_copy(out=ident_bf, in_=consts[:, CO_IDENT:CO_IDENT + 128]),
          reads=["consts"], writes=["ident_bf"])
    P.add("dve", lambda e: e.tensor_copy(out=ones_bf, in_=consts[0:1, CO_ONES:CO_ONES + 128]),
          reads=["consts"], writes=["ones_bf"])
    P.add("dve", lambda e: e.tensor_copy(out=convb_bf, in_=convb32[0:1, 0:768]),
          reads=["convb32"], writes=["convb_bf"])
    for r4 in range(4):
        P.add("dve", lambda e, r4=r4: e.tensor_copy(out=I4[:, r4, :], in_=consts[:, CO_IDENT:CO_IDENT + 128]),
              reads=["consts"], writes=["I4"])
        P.add("dve", lambda e, r4=r4: e.tensor_copy(out=negl4_bf[:, r4, :], in_=consts[:, CO_NEGL:CO_NEGL + 128]),
              reads=["consts"], writes=["negl4"])
    for h in range(8):
        k = h % 2
        P.add("dve", lambda e, h=h, k=k: e.tensor_scalar(out=lhs_h[k], in0=consts[0:32, CO_ONES:CO_ONES + 128],
                                                         scalar1=rb_sb[:, h:h + 1], scalar2=None, op0=ALU.mult),
              reads=["consts", "rb_sb"], writes=[("lhs_h", k)])
        P.add("pe", lambda e, k=k: e.matmul(out=ps[k][:, 0:384], lhsT=lhs_h[k],
                                            rhs=consts[0:32, CO_OH:CO_OH + 384], start=True, stop=True),
              reads=[("lhs_h", k), "consts"], writes=[("ps", k)])
        P.add("act", lambda e, k=k: e.activation(out=Rsb[k], in_=ps[k][:, 0:384], func=AF.Copy),
              reads=[("ps", k)], writes=[("Rsb", k)])
        dma("sp", E_d.ap()[h, :].rearrange("(p m) -> p m", p=128), Rsb[k], [("Rsb", k)], [("E_d", h)], f"Rsb{k}")
        for dl in range(2):
            src = bass.AP(E_d, h * 128 * 384 + 127 + 128 * dl, [[383, 128], [1, 128]])
            dma("sp", Bt32[:, dl, h, :], src, [("E_d", h)], [("Bt32", dl, h)], "Bt32")
        P.add("dve", lambda e, h=h: e.tensor_scalar(out=c8row[0:1, h, :], in0=consts[0:1, CO_ONES:CO_ONES + 128],
                                                    scalar1=r31[0:1, h:h + 1], scalar2=8.0, op0=ALU.mult, op1=ALU.mult),
              reads=["consts", "r31"], writes=["c8row"])
    P.add("dve", lambda e: e.tensor_scalar(out=flat(B8), in0=flat(Bt32), scalar1=8.0, scalar2=None, op0=ALU.mult),
          reads=[("Bt32", dl, h) for dl in range(2) for h in range(8)], writes=["B8"])
    for cc in range(8):
        for k in range(4):
            P.add("dve", lambda e, cc=cc, k=k: e.tensor_scalar(
                out=Dg[:, cc, k, :], in0=consts[:, CO_IDENT:CO_IDENT + 128],
                scalar1=pp[:, PP_CONVW + cc * 4 + k:PP_CONVW + cc * 4 + k + 1], scalar2=None, op0=ALU.mult),
                reads=["consts", "pp"], writes=["Dg"])
    P.add("act", lambda e: e.activation(out=A_b, in_=rows[:, R_ALOG:R_ALOG + 8], func=AF.Exp),
          reads=["rows"], writes=["A_b"])
    P.add("dve", lambda e: e.tensor_scalar(out=A_b, in0=A_b, scalar1=-1.0, scalar2=None, op0=ALU.mult),
          reads=["A_b"], writes=["A_b"])

    win_v = win_d.rearrange("(kc p) n -> p kc n", p=128)
    wout_v = wout_d.rearrange("(cc p) n -> p cc n", p=128)
    wup_v = wup_d.rearrange("(kc p) n -> p kc n", p=128)
    wdn_v = wdn_d.rearrange("(fc p) n -> p fc n", p=128)
    chunks = []
    for kc in range(8):
        chunks.append((win_v[:, kc, :], wi_bf[:, kc, :], [W_IN_COLS], pp[:, PP_GMIX + kc:PP_GMIX + kc + 1], "act"))
    for c2 in range(2):
        chunks.append((wout_v[:, 4 * c2:4 * c2 + 4, :], wo_bf[:, 4 * c2:4 * c2 + 4, :], [4, 1024], None, "pool"))
    for kc in range(8):
        chunks.append((wup_v[:, kc, :], wu_bf[:, kc, :], [4096], pp[:, PP_GMLP + kc:PP_GMLP + kc + 1], "act"))
        chunks.append((wdn_v[:, 4 * kc:4 * kc + 4, :], wd_bf[:, 4 * kc:4 * kc + 4, :], [4, 1024], None,
                       "dve" if kc % 2 else "pool"))

    def stage_views(idx):
        src, dst, shp, sc, ce = chunks[idx]
        k = idx % NST
        nfree = int(np.prod(shp))
        s32 = stage32[k][:, 0:nfree]
        s16 = stage16[k][:, 0:nfree]
        if len(shp) == 2:
            return k, s32, s16, s32.rearrange("p (a b) -> p a b", a=shp[0]), s16.rearrange("p (a b) -> p a b", a=shp[0])
        return k, s32, s16, s32, s16

    def prep_load(idx):
        k, s32, s16, s32v, s16v = stage_views(idx)
        dma("sp", s32v, chunks[idx][0], [], [("st32", k)], f"st32_{k}")

    for idx in range(min(NST, len(chunks))):
        prep_load(idx)
    for idx in range(len(chunks)):
        src, dst, shp, sc, ce = chunks[idx]
        k, s32, s16, s32v, s16v = stage_views(idx)
        if sc is not None:
            P.add("act", lambda e, s16=s16, s32=s32, sc=sc: e.activation(out=s16, in_=s32, func=AF.Copy, scale=sc),
                  reads=[("st32", k), "pp"], writes=[("st16", k)])
        else:
            P.add(ce, lambda e, s16=s16, s32=s32: e.tensor_copy(out=s16, in_=s32), reads=[("st32", k)],
                  writes=[("st16", k)])
        dma("pool", dst, s16v, [("st16", k)], [("wscr", idx)], f"st16_{k}")
        if idx + NST < len(chunks):
            prep_load(idx + NST)
    barrier()

    for s in range(nseq):
        dma("sp", w_in_sb, wi_bf, [], ["w_in_sb"], "w_in_sb")
        P.add("pool", lambda e: e.memset(v_aug[:, :, :, 64:65], 1.0), writes=["v_ones"])
        for kq in range(2):
            P.add("pool", lambda e, kq=kq: e.memset(flat(qTz[kq]), 0.0), writes=[("qT", kq)])
        P.add("pool", lambda e: e.memset(flat(H_sb), 0.0), writes=["H"])
        P.add("pool", lambda e: e.memset(flat(Hbf), 0.0), writes=["Hbf"])
        pend_attn = []
        finalizers = []
        for i in range(nt):
            k2 = i % 2
            r0 = s * L + i * 128
            dma("sp", xt[0], x_d[r0:r0 + 128, :], [], [("xt", 0)], "xt0")
            P.add("act", lambda e, k2=k2: e.activation(out=xb[k2], in_=xt[0], func=AF.Square, accum_out=ss[k2]),
                  reads=[("xt", 0)], writes=[("ss", k2), ("xb", k2)])
            P.add("act", lambda e, k2=k2: e.activation(out=sd[k2], in_=ss[k2], func=AF.Ln, scale=1.0 / 1024,
                                                       bias=pp[:, PP_EPS:PP_EPS + 1]),
                  reads=[("ss", k2), "pp"], writes=[("sd", k2)])
            P.add("act", lambda e, k2=k2: e.activation(out=rstd[k2], in_=sd[k2], func=AF.Exp, scale=-0.5),
                  reads=[("sd", k2)], writes=[("rstd", k2)])
            P.add("act", lambda e, k2=k2: e.activation(out=xb[k2], in_=xt[0], func=AF.Copy, scale=rstd[k2]),
                  reads=[("xt", 0), ("rstd", k2)], writes=[("xb", k2)])
            pT = ps[0][:].bitcast(BF16)
            for kc in range(8):
                P.add("pe", lambda e, k2=k2, kc=kc: e.transpose(out=pT[:, kc * 128:(kc + 1) * 128],
                                                                 in_=xb[k2][:, kc * 128:(kc + 1) * 128],
                                                                 identity=ident_bf),
                      reads=[("xb", k2), "ident_bf"], writes=[("ps", 0)])
            P.add("act", lambda e, k2=k2: e.activation(out=flat(uT[k2]), in_=pT, func=AF.Copy),
                  reads=[("ps", 0)], writes=[("uT", k2)])

            def fm_group(bank, slot, g, k2=k2):
                for kc in range(8):
                    P.add("pe", lambda e, kc=kc: e.matmul(out=ps[bank][:, slot * 128:(slot + 1) * 128],
                                                          lhsT=w_in_sb[:, kc, g * 128:(g + 1) * 128],
                                                          rhs=uT[k2][:, kc, :], start=(kc == 0), stop=(kc == 7)),
                          reads=["w_in_sb", ("uT", k2)], writes=[("ps", bank)])
            for g in range(4):
                fm_group(1, g, g)
            for half in range(2):
                P.add("act", lambda e, k2=k2, half=half: e.activation(
                    out=qTz[k2][half * 64:(half + 1) * 64, half, :, :],
                    in_=ps[1][half * 64:(half + 1) * 64, :].rearrange("p (a b) -> p a b", a=4), func=AF.Copy),
                    reads=[("ps", 1)], writes=[("qT", k2)])
            for g in range(4):
                fm_group(2, g, 4 + g)
            P.add("act", lambda e, i=i: e.activation(out=kT[:, :, i * 128:(i + 1) * 128],
                                                     in_=ps[2][:, 0:256].rearrange("p (a b) -> p a b", a=2),
                                                     func=AF.Copy),
                  reads=[("ps", 2)], writes=[("kT", i)])
            P.add("act", lambda e, k2=k2: e.activation(out=flat(qiT[k2]), in_=ps[2][:, 256:512], func=AF.Copy),
                  reads=[("ps", 2)], writes=[("qiT", k2)])
            fm_group(3, 0, 8)
            P.add("act", lambda e: e.activation(out=kiraw, in_=ps[3][:, 0:128], func=AF.Copy),
                  reads=[("ps", 3)], writes=["kiraw"])
            bd = consts[:, CO_BD64:CO_BD64 + 128]
            imbd = consts[:, CO_NU:CO_NU + 128]
            P.add("pe", lambda e: e.matmul(out=ps[3][:, 128:256], lhsT=imbd, rhs=kiraw, start=True, stop=True),
                  reads=["consts", "kiraw"], writes=[("ps", 3)])
            P.add("act", lambda e: e.activation(out=kisq, in_=ps[3][:, 128:256], func=AF.Square),
                  reads=[("ps", 3)], writes=["kisq"])
            P.add("act", lambda e: e.activation(out=kicen, in_=ps[3][:, 128:256], func=AF.Copy),
                  reads=[("ps", 3)], writes=["kicen"])
            P.add("pe", lambda e: e.matmul(out=ps[3][:, 256:384], lhsT=bd, rhs=kisq, start=True, stop=True),
                  reads=["consts", "kisq"], writes=[("ps", 3)])
            P.add("act", lambda e: e.activation(out=kisd, in_=ps[3][:, 256:384], func=AF.Ln,
                                                bias=pp[:, PP_EPS:PP_EPS + 1]),
                  reads=[("ps", 3), "pp"], writes=["kisd"])
            P.add("act", lambda e: e.activation(out=kirs, in_=kisd, func=AF.Exp, scale=-0.5),
                  reads=["kisd"], writes=["kirs"])
            P.add("pool", lambda e: e.tensor_tensor(out=kicen, in0=kicen, in1=kirs, op=ALU.mult),
                  reads=["kicen", "kirs"], writes=["kicen"])
            P.add("act", lambda e, i=i: e.activation(out=kiT[:, i * 128:(i + 1) * 128], in_=kicen,
                                                     func=AF.Identity, scale=pp[:, PP_LNW:PP_LNW + 1],
                                                     bias=pp[:, PP_LNB:PP_LNB + 1]),
                  reads=["kicen", "pp"], writes=[("kiT", i)])
            for cc in range(8):
                fm_group(4 + cc // 4, cc % 4, 9 + cc)
            if i == 0:
                P.add("pool", lambda e, k2=k2: e.memset(xbcT[k2][:, :, 0:3], 0.0), writes=[("xbcT", k2)])
            else:
                P.add("pool", lambda e, k2=k2: e.tensor_copy(out=xbcT[k2][:, :, 0:3], in_=xbcT[1 - k2][:, :, 128:131]),
                      reads=[("xbcT", 1 - k2)], writes=[("xbcT", k2)])
            for hb in range(2):
                P.add("act", lambda e, k2=k2, hb=hb: e.activation(
                    out=xbcT[k2][:, 4 * hb:4 * hb + 4, 3:131],
                    in_=ps[4 + hb][:].rearrange("p (a b) -> p a b", a=4), func=AF.Copy),
                    reads=[("ps", 4 + hb)], writes=[("xbcT", k2)])
            for kc in range(8):
                P.add("pe", lambda e, kc=kc, k2=k2: e.matmul(out=ps[6][:, 0:140], lhsT=uT[k2][:, kc, :],
                                                             rhs=w_in_sb[:, kc, C_TM1:C_TM1 + 140],
                                                             start=(kc == 0), stop=(kc == 7)),
                      reads=["w_in_sb", ("uT", k2)], writes=[("ps", 6)])
            for kc in range(8):
                P.add("pe", lambda e, kc=kc, k2=k2: e.matmul(out=ps[7][:], lhsT=uT[k2][:, kc, :],
                                                             rhs=w_in_sb[:, kc, C_Z:C_Z + 512],
                                                             start=(kc == 0), stop=(kc == 7)),
                      reads=["w_in_sb", ("uT", k2)], writes=[("ps", 7)])
            P.add("act", lambda e, i=i: e.activation(out=v_aug[:, i, :, 0:64],
                                                     in_=ps[6][:, 0:128].rearrange("p (a b) -> p a b", a=2),
                                                     func=AF.Copy),
                  reads=[("ps", 6)], writes=[("v", i)])
            P.add("act", lambda e, k2=k2: e.activation(out=wI[k2], in_=ps[6][:, 128:132], func=AF.Copy, scale=1.0 / 16),
                  reads=[("ps", 6)], writes=[("wI", k2)])
            P.add("act", lambda e, k2=k2: e.activation(out=dtraw[k2], in_=ps[6][:, 132:140], func=AF.Copy),
                  reads=[("ps", 6)], writes=[("dtraw", k2)])
            P.add("act", lambda e, k2=k2: e.activation(out=sz[k2], in_=ps[7][:], func=AF.Silu),
                  reads=[("ps", 7)], writes=[("sz", k2)])

            n = (i + 1) * 128
            Sb = S2[k2]
            cidx = 0
            nchunk = (n + 511) // 512
            for c in range(nchunk):
                wd = min(512, n - c * 512)
                for h in range(4):
                    pair, half = h // 2, h % 2
                    bk = cidx % 2
                    cidx += 1
                    P.add("pe", lambda e, k2=k2, pair=pair, half=half, bk=bk, c=c, wd=wd: e.matmul(
                        out=ps[bk][:, 0:wd], lhsT=qiT[k2][half * 64:(half + 1) * 64, pair, :],
                        rhs=kiT[half * 64:(half + 1) * 64, c * 512:c * 512 + wd], start=True, stop=True),
                        reads=[("qiT", k2)] + [("kiT", jj) for jj in range(4 * c, min(4 * c + 4, i + 1))],
                        writes=[("ps", bk)])
                    P.add("act", lambda e, bk=bk, wd=wd: e.activation(out=rrelu[bk][:, 0:wd], in_=ps[bk][:, 0:wd],
                                                                       func=AF.Relu),
                          reads=[("ps", bk)], writes=[("rrelu", bk)])
                    prev = (consts[:, CO_PERT + c * 512:CO_PERT + c * 512 + wd] if h == 0
                            else Sb[:, c * 512:c * 512 + wd])
                    P.add("dve", lambda e, k2=k2, h=h, bk=bk, c=c, wd=wd, prev=prev, Sb=Sb: e.scalar_tensor_tensor(
                        out=Sb[:, c * 512:c * 512 + wd], in0=rrelu[bk][:, 0:wd], scalar=wI[k2][:, h:h + 1],
                        in1=prev, op0=ALU.mult, op1=ALU.add),
                        reads=[("rrelu", bk), ("wI", k2), "consts", ("S", k2, c)], writes=[("S", k2, c)])
            cl = i // 4
            P.add("pool", lambda e, i=i, Sb=Sb: e.tensor_tensor(out=Sb[:, i * 128:(i + 1) * 128],
                                                                in0=Sb[:, i * 128:(i + 1) * 128],
                                                                in1=consts[:, CO_NEGTRI:CO_NEGTRI + 128], op=ALU.add),
                  reads=[("S", k2, cl), "consts"], writes=[("S", k2, cl)])
            allS = [("S", k2, c) for c in range(nchunk)]

            def queue_topk(i=i, n=n, Sb=Sb, allS=allS):
                if n > topk and i >= ACT_FROM:
                    K = 28
                    for k in range(K):
                        dk = 8.0 / (2 ** k)
                        if k == 0:
                            P.bg_push("act", lambda e: e.activation(out=junk8[:, 0:n], in_=Sb[:, 0:n], func=AF.Sign,
                                                                    accum_out=sgn),
                                      reads=allS, writes=["sgn", "junk8"])
                        else:
                            P.bg_push("act", lambda e: e.activation(out=junk8[:, 0:n], in_=Sb[:, 0:n], func=AF.Sign,
                                                                    bias=negth, accum_out=sgn),
                                      reads=allS + ["negth"], writes=["sgn", "junk8"])
                        P.bg_push("act", lambda e: e.activation(out=sg2, in_=sgn, func=AF.Sign,
                                                                bias=float(n - (2 * topk - 1))),
                                  reads=["sgn"], writes=["sg2"])
                        if k == 0:
                            P.bg_push("act", lambda e, dk=dk: e.activation(out=negth, in_=sg2, func=AF.Identity,
                                                                          scale=-dk / 2),
                                      reads=["sg2"], writes=["negth"])
                        else:
                            P.bg_push("act", lambda e, dk=dk: e.activation(out=negth, in_=sg2, func=AF.Identity,
                                                                          scale=-dk / 2, bias=negth),
                                      reads=["sg2", "negth"], writes=["negth"])
                    dK = 8.0 / (2 ** K)
                    P.bg_push("act", lambda e: e.activation(out=thf, in_=negth, func=AF.Identity, scale=-1.0, bias=-dK),
                              reads=["negth"], writes=["thf"])
                    finalizers.append(lambda: P.add("dve", lambda e: e.tensor_scalar(
                        out=negm[:, 0:n], in0=Sb[:, 0:n], scalar1=thf, scalar2=-30000.0,
                        op0=ALU.is_lt, op1=ALU.mult),
                        reads=allS + ["thf"], writes=["negm"]))
                elif n > topk and i >= BIS_FROM:
                    K = 28
                    P.add("dve", lambda e: e.memset(thb, 0.0), writes=["thb"])
                    for k in range(K):
                        dk = 8.0 / (2 ** k)
                        P.add("dve", lambda e: e.tensor_scalar(out=junk8[:, 0:n], in0=Sb[:, 0:n], scalar1=thb, scalar2=None,
                                                               op0=ALU.is_ge, op1=ALU.add, accum_out=cntb),
                              reads=allS + ["thb"], writes=["cntb", "junk8"])
                        P.add("dve", lambda e, dk=dk: e.tensor_scalar(out=ubis, in0=cntb, scalar1=float(topk) - 0.5, scalar2=dk,
                                                                      op0=ALU.is_ge, op1=ALU.mult),
                              reads=["cntb"], writes=["ub"])
                        P.add("dve", lambda e, dk=dk: e.scalar_tensor_tensor(out=thb, in0=thb, scalar=-dk / 2, in1=ubis,
                                                                             op0=ALU.add, op1=ALU.add),
                              reads=["thb", "ub"], writes=["thb"])
                    dK = 8.0 / (2 ** K)
                    P.add("dve", lambda e: e.scalar_tensor_tensor(out=ubis, in0=thb, scalar=-1.0, in1=thb,
                                                                  op0=ALU.mult, op1=ALU.max),
                          reads=["thb"], writes=["ub"])
                    P.add("dve", lambda e: e.tensor_scalar(out=ubis, in0=ubis, scalar1=2.4e-7, scalar2=None, op0=ALU.mult),
                          reads=["ub"], writes=["ub"])
                    P.add("dve", lambda e: e.scalar_tensor_tensor(out=thf, in0=thb, scalar=-dK, in1=ubis,
                                                                  op0=ALU.add, op1=ALU.subtract),
                          reads=["thb", "ub"], writes=["thf"])
                    finalizers.append(lambda: P.add("dve", lambda e: e.tensor_scalar(
                        out=negm[:, 0:n], in0=Sb[:, 0:n], scalar1=thf, scalar2=-30000.0,
                        op0=ALU.is_lt, op1=ALU.mult),
                        reads=allS + ["thf"], writes=["negm"]))
                elif n > topk:
                    for r in range(topk // 8):
                        P.add("dve", lambda e, r=r: e.max(out=m8[r % 2], in_=Sb[:, 0:n]),
                              reads=allS, writes=[("m8", r % 2)])
                        P.add("dve", lambda e, r=r: e.match_replace(
                            out=Sb[:, 0:n], in_to_replace=m8[r % 2], in_values=Sb[:, 0:n], imm_value=-3.0e38),
                            reads=[("m8", r % 2)] + allS, writes=allS)
                    finalizers.append(lambda: P.add("dve", lambda e: e.tensor_scalar(
                        out=negm[:, 0:n], in0=Sb[:, 0:n], scalar1=-1.0e38, scalar2=-30000.0,
                        op0=ALU.is_gt, op1=ALU.mult),
                        reads=allS, writes=["negm"]))
                else:
                    finalizers.append(lambda: P.add("dve", lambda e: e.tensor_scalar(
                        out=negm[:, 0:n], in0=Sb[:, 0:n], scalar1=-1.0e29, scalar2=-30000.0,
                        op0=ALU.is_lt, op1=ALU.mult),
                        reads=allS, writes=["negm"]))

            def attention(i=i, k2=k2):
                for j in range(i + 1):
                    ek = j % 2
                    for g in range(2):
                        bank = 2 + g
                        for half in range(2):
                            P.add("pe", lambda e, g=g, half=half, j=j, bank=bank: e.matmul(
                                out=ps[bank][:, half * 256:(half + 1) * 256],
                                lhsT=kT[:, g, j * 128:(j + 1) * 128],
                                rhs=qTz[k2][:, half, 2 * g:2 * g + 2, :],
                                start=(half == 0), stop=False),
                                reads=[("kT", j), ("qT", k2)], writes=[("ps", bank)])
                        P.add("pe", lambda e, j=j, bank=bank: e.matmul(
                            out=ps[bank][:], lhsT=negm[:, j * 128:(j + 1) * 128], rhs=flat(I4), start=False, stop=False),
                            reads=["negm", "I4"], writes=[("ps", bank)])
                        if i - j <= 1:
                            P.add("pe", lambda e, g=g, dl=i - j, bank=bank: e.matmul(
                                out=ps[bank][:], lhsT=ident_bf, rhs=B8[:, dl, 4 * g:4 * g + 4, :], start=False, stop=True),
                                reads=["ident_bf", "B8"], writes=[("ps", bank)])
                        else:
                            P.add("pe", lambda e, g=g, bank=bank: e.matmul(
                                out=ps[bank][:], lhsT=ones_bf[0:1, :], rhs=c8row[0:1, 4 * g:4 * g + 4, :],
                                start=False, stop=True),
                                reads=["ones_bf", "c8row"], writes=[("ps", bank)])
                        P.add("act", lambda e, g=g, ek=ek, bank=bank: e.activation(
                            out=E_sb[ek][:, g * 512:(g + 1) * 512], in_=ps[bank][:], func=AF.Exp, scale=0.125),
                            reads=[("ps", bank)], writes=[("E", ek, g)])
                    for h in range(8):
                        bpv = 4 + h // 4
                        hh = h % 4
                        P.add("pe", lambda e, h=h, hh=hh, bpv=bpv, ek=ek, j=j: e.matmul(
                            out=ps[bpv][:, hh * 65:hh * 65 + 65], lhsT=E_sb[ek][:, h * 128:(h + 1) * 128],
                            rhs=v_aug[:, j, h // 4, :], start=(j == 0 and hh == 0), stop=(j == i), skip_group_check=True),
                            reads=[("E", ek, h // 4), ("v", j), "v_ones"], writes=[("ps", bpv)])
                for b2 in range(2):
                    psv = ps[4 + b2][:, 0:260].rearrange("p (h c) -> p h c", c=65)
                    P.add("act", lambda e, b2=b2, psv=psv: e.activation(out=lnd[:, 4 * b2:4 * b2 + 4], in_=psv[:, :, 64],
                                                                       func=AF.Ln),
                          reads=[("ps", 4 + b2)], writes=[("lnd", b2)])
                    P.add("act", lambda e, b2=b2: e.activation(out=rec[:, 4 * b2:4 * b2 + 4], in_=lnd[:, 4 * b2:4 * b2 + 4],
                                                               func=AF.Exp, scale=-1.0),
                          reads=[("lnd", b2)], writes=[("rec", b2)])
                    for hh in range(4):
                        h = 4 * b2 + hh
                        P.add("act", lambda e, h=h, hh=hh, psv=psv: e.activation(
                            out=attn_tok[:, h * 64:(h + 1) * 64], in_=psv[:, hh, 0:64], func=AF.Copy,
                            scale=rec[:, h:h + 1]),
                            reads=[("ps", 4 + b2), ("rec", b2)], writes=["attn_tok"])
                pT6 = ps[6][:].bitcast(BF16)
                for c4 in range(4):
                    P.add("pe", lambda e, c4=c4: e.transpose(out=pT6[:, c4 * 128:(c4 + 1) * 128],
                                                             in_=attn_tok[:, c4 * 128:(c4 + 1) * 128], identity=ident_bf),
                          reads=["attn_tok", "ident_bf"], writes=[("ps", 6)])
                P.add("act", lambda e: e.activation(out=mixT[:, 0:4, i * 128:(i + 1) * 128],
                                                    in_=pT6[:, 0:512].rearrange("p (a b) -> p a b", a=4),
                                                    func=AF.Copy),
                      reads=[("ps", 6)], writes=[("mixT_a", i)])

            pend_attn.append(attention)
            queue_topk()

            xk = xbcT[k2]
            for cc in range(6):
                bank = 0 if cc < 4 else 1
                col = (cc % 4) * 128
                for k in range(4):
                    P.add("pe", lambda e, cc=cc, k=k, bank=bank, col=col, xk=xk: e.matmul(
                        out=ps[bank][:, col:col + 128], lhsT=xk[:, cc, k:k + 128], rhs=Dg[:, cc, k, :],
                        start=(k == 0), stop=False),
                        reads=[("xbcT", k2), "Dg"], writes=[("ps", bank)])
                P.add("pe", lambda e, cc=cc, bank=bank, col=col: e.matmul(
                    out=ps[bank][:, col:col + 128], lhsT=ones_bf[0:1, :], rhs=convb_bf[0:1, cc * 128:(cc + 1) * 128],
                    start=False, stop=True),
                    reads=["ones_bf", "convb_bf"], writes=[("ps", bank)])
            P.add("act", lambda e: e.activation(out=flat(xs_tok), in_=ps[0][:], func=AF.Silu),
                  reads=[("ps", 0)], writes=["xs_tok"])
            P.add("act", lambda e: e.activation(out=B_tok, in_=ps[1][:, 0:256], func=AF.Silu),
                  reads=[("ps", 1)], writes=["B_tok"])
            for c4 in range(4):
                cc = 4 + c4
                for k in range(4):
                    P.add("pe", lambda e, cc=cc, c4=c4, k=k, xk=xk: e.matmul(
                        out=ps[2][:, c4 * 128:(c4 + 1) * 128], lhsT=Dg[:, cc, k, :], rhs=xk[:, cc, k:k + 128],
                        start=(k == 0), stop=(k == 3)),
                        reads=[("xbcT", k2), "Dg"], writes=[("ps", 2)])
                P.add("act", lambda e, cc=cc, c4=c4: e.activation(
                    out=BCT[:, c4, :], in_=ps[2][:, c4 * 128:(c4 + 1) * 128], func=AF.Silu,
                    bias=pp[:, PP_CONVB + cc:PP_CONVB + cc + 1]),
                    reads=[("ps", 2), "pp"], writes=["BCT"])
            P.add("pool", lambda e, k2=k2: e.tensor_tensor(out=dt_sb, in0=dtraw[k2], in1=rows[:, R_DTB:R_DTB + 8],
                                                           op=ALU.add),
                  reads=[("dtraw", k2), "rows"], writes=["dt_sb"])
            P.add("act", lambda e: e.activation(out=dt_sb, in_=dt_sb, func=AF.Exp), reads=["dt_sb"], writes=["dt_sb"])
            P.add("act", lambda e: e.activation(out=dt_sb, in_=dt_sb, func=AF.Ln, bias=1.0),
                  reads=["dt_sb"], writes=["dt_sb"])
            P.add("pool", lambda e: e.tensor_tensor(out=a_sb, in0=dt_sb, in1=A_b, op=ALU.mult),
                  reads=["dt_sb", "A_b"], writes=["a_sb"])
            for g in range(2):
                P.add("pe", lambda e, g=g: e.matmul(out=ps[3][:, g * 128:(g + 1) * 128], lhsT=BCT[:, g, :],
                                                    rhs=BCT[:, 2 + g, :], start=True, stop=True),
                      reads=["BCT"], writes=[("ps", 3)])
            P.add("pe", lambda e: e.matmul(out=ps[3][:, 256:264], lhsT=consts[:, CO_U:CO_U + 128], rhs=a_sb,
                                           start=True, stop=True),
                  reads=["consts", "a_sb"], writes=[("ps", 3)])
            P.add("pe", lambda e: e.matmul(out=ps[3][:, 264:272], lhsT=consts[:, CO_ONES:CO_ONES + 128], rhs=a_sb,
                                           start=True, stop=True),
                  reads=["consts", "a_sb"], writes=[("ps", 3)])
            P.add("act", lambda e: e.activation(out=flat(GT_sb), in_=ps[3][:, 0:256], func=AF.Copy),
                  reads=[("ps", 3)], writes=["GT_sb"])
            P.add("act", lambda e: e.activation(out=cst, in_=ps[3][:, 256:272], func=AF.Copy),
                  reads=[("ps", 3)], writes=["cst"])
            P.add("act", lambda e: e.activation(out=ecs, in_=cst[:, 0:8], func=AF.Exp), reads=["cst"], writes=["ecs"])
            P.add("act", lambda e: e.activation(out=dtot, in_=cst[:, 8:16], func=AF.Exp), reads=["cst"], writes=["dtot"])
            P.add("pool", lambda e: e.tensor_tensor(out=dte, in0=cst[:, 8:16], in1=cst[:, 0:8], op=ALU.subtract),
                  reads=["cst"], writes=["dte"])
            P.add("act", lambda e: e.activation(out=dte, in_=dte, func=AF.Exp), reads=["dte"], writes=["dte"])
            P.add("pool", lambda e: e.tensor_scalar(out=negcs, in0=cst[:, 0:8], scalar1=-1.0, scalar2=1.0, op0=ALU.mult,
                                                   op1=ALU.mult),
                  reads=["cst"], writes=["negcs"])
            Ub = consts[:, CO_U:CO_U + 128].unsqueeze(1).broadcast_to([128, 8, 128])
            ab = a_sb.unsqueeze(2).broadcast_to([128, 8, 128])
            P.add("pool", lambda e, Ub=Ub, ab=ab: e.tensor_tensor(out=rL, in0=Ub, in1=ab, op=ALU.mult),
                  reads=["consts", "a_sb"], writes=["rL"])
            for b2 in range(2):
                P.add("pe", lambda e, b2=b2: e.matmul(out=ps[4 + b2][:], lhsT=consts[:, CO_ONES:CO_ONES + 128],
                                                      rhs=rL[:, 4 * b2:4 * b2 + 4, :], start=True, stop=False),
                      reads=["consts", "rL"], writes=[("ps", 4 + b2)])
                P.add("pe", lambda e, b2=b2: e.matmul(out=ps[4 + b2][:], lhsT=ident_bf, rhs=flat(negl4_bf),
                                                      start=False, stop=True),
                      reads=["ident_bf", "negl4"], writes=[("ps", 4 + b2)])
            for h in range(8):
                b2, hh = h // 4, h % 4
                P.add("act", lambda e, h=h, b2=b2, hh=hh: e.activation(
                    out=rL[:, h, :], in_=ps[4 + b2][:, hh * 128:(hh + 1) * 128], func=AF.Exp,
                    bias=negcs[:, h:h + 1]),
                    reads=[("ps", 4 + b2), "negcs"], writes=["rL"])
            for b2 in range(2):
                gtb = GT_sb[:, b2, :].unsqueeze(1).broadcast_to([128, 4, 128])
                P.add("pool", lambda e, b2=b2, gtb=gtb: e.tensor_tensor(out=WT[:, 4 * b2:4 * b2 + 4, :],
                                                                       in0=rL[:, 4 * b2:4 * b2 + 4, :], in1=gtb,
                                                                       op=ALU.mult),
                      reads=["rL", "GT_sb"], writes=[("WT", b2)])
            dtb = dt_sb.unsqueeze(2).broadcast_to([128, 8, 64])
            dteb = dte.unsqueeze(2).broadcast_to([128, 8, 64])
            P.add("pool", lambda e, dtb=dtb: e.tensor_tensor(out=X_sb, in0=xs_tok, in1=dtb, op=ALU.mult),
                  reads=["xs_tok", "dt_sb"], writes=["X_sb"])
            P.add("pool", lambda e, dteb=dteb: e.tensor_tensor(out=Xd_sb, in0=X_sb, in1=dteb, op=ALU.mult),
                  reads=["X_sb", "dte"], writes=["Xd_sb"])
            for h in range(8):
                P.add("pe", lambda e, h=h: e.matmul(out=ps[6][:, h * 64:(h + 1) * 64], lhsT=WT[:, h, :], rhs=X_sb[:, h, :],
                                                    start=True, stop=True),
                      reads=[("WT", h // 4), "X_sb"], writes=[("ps", 6)])
            for g in range(2):
                P.add("pe", lambda e, g=g: e.matmul(out=ps[7][:, g * 256:(g + 1) * 256], lhsT=BCT[:, 2 + g, :],
                                                    rhs=Hbf[:, 4 * g:4 * g + 4, :], start=True, stop=True),
                      reads=["BCT", "Hbf"], writes=[("ps", 7)])
            P.add("act", lambda e: e.activation(out=flat(yoff_sb), in_=ps[7][:], func=AF.Copy),
                  reads=[("ps", 7)], writes=["yoff_sb"])
            ecsb = ecs.unsqueeze(2).broadcast_to([128, 8, 64])
            dskb = rows[:, R_DSKIP:R_DSKIP + 8].unsqueeze(2).broadcast_to([128, 8, 64])
            P.add("pool", lambda e, ecsb=ecsb: e.tensor_tensor(out=yoff_sb, in0=yoff_sb, in1=ecsb, op=ALU.mult),
                  reads=["yoff_sb", "ecs"], writes=["yoff_sb"])
            P.add("pool", lambda e, dskb=dskb: e.tensor_tensor(out=t2_sb, in0=xs_tok, in1=dskb, op=ALU.mult),
                  reads=["xs_tok", "rows"], writes=["t2_sb"])
            P.add("pool", lambda e: e.tensor_tensor(out=yoff_sb, in0=yoff_sb, in1=t2_sb, op=ALU.add),
                  reads=["yoff_sb", "t2_sb"], writes=["yoff_sb"])
            P.add("act", lambda e: e.activation(out=y_sb, in_=ps[6][:], func=AF.Copy),
                  reads=[("ps", 6)], writes=["y_sb"])
            P.add("pool", lambda e: e.tensor_tensor(out=y_sb, in0=y_sb, in1=flat(yoff_sb), op=ALU.add),
                  reads=["y_sb", "yoff_sb"], writes=["y_sb"])
            P.add("pool", lambda e, k2=k2: e.tensor_tensor(out=y_sb, in0=y_sb, in1=sz[k2], op=ALU.mult),
                  reads=["y_sb", ("sz", k2)], writes=["y_sb"])
            for g in range(2):
                P.add("act", lambda e, g=g: e.activation(out=flat(t2_sb)[:, g * 256:(g + 1) * 256],
                                                         in_=y_sb[:, g * 256:(g + 1) * 256],
                                                         func=AF.Square, accum_out=ssg[:, g:g + 1]),
                      reads=["y_sb"], writes=[("ssg", g), "t2_sb"])
            P.add("act", lambda e: e.activation(out=sdg, in_=ssg, func=AF.Ln, scale=1.0 / 256,
                                                bias=pp[:, PP_EPS:PP_EPS + 1]),
                  reads=[("ssg", 0), ("ssg", 1), "pp"], writes=["sdg"])
            P.add("act", lambda e: e.activation(out=rsg, in_=sdg, func=AF.Exp, scale=-0.5), reads=["sdg"], writes=["rsg"])
            for g in range(2):
                P.add("act", lambda e, g=g: e.activation(out=flat(t2_sb)[:, g * 256:(g + 1) * 256],
                                                         in_=y_sb[:, g * 256:(g + 1) * 256], func=AF.Copy,
                                                         scale=rsg[:, g:g + 1]),
                      reads=["y_sb", "rsg"], writes=["t2_sb"])
                P.add("pool", lambda e, g=g: e.tensor_tensor(
                    out=ssd_tok[:, g * 256:(g + 1) * 256], in0=flat(t2_sb)[:, g * 256:(g + 1) * 256],
                    in1=rows[:, R_SSDNW + g * 256:R_SSDNW + (g + 1) * 256], op=ALU.mult),
                    reads=["t2_sb", "rows"], writes=["ssd_tok"])
            pT7 = ps[7][:].bitcast(BF16)
            for c4 in range(4):
                P.add("pe", lambda e, c4=c4: e.transpose(out=pT7[:, c4 * 128:(c4 + 1) * 128],
                                                         in_=ssd_tok[:, c4 * 128:(c4 + 1) * 128], identity=ident_bf),
                      reads=["ssd_tok", "ident_bf"], writes=[("ps", 7)])
            P.add("act", lambda e, i=i: e.activation(out=mixT[:, 4:8, i * 128:(i + 1) * 128],
                                                     in_=pT7[:, 0:512].rearrange("p (a b) -> p a b", a=4), func=AF.Copy),
                  reads=[("ps", 7)], writes=[("mixT_s", i)])
            for g in range(2):
                P.add("pe", lambda e, g=g: e.matmul(out=ps[0][:, g * 256:(g + 1) * 256], lhsT=B_tok[:, g * 128:(g + 1) * 128],
                                                    rhs=Xd_sb[:, 4 * g:4 * g + 4, :], start=True, stop=True),
                      reads=["B_tok", "Xd_sb"], writes=[("ps", 0)])
            dtotb = dtot.unsqueeze(2).broadcast_to([128, 8, 64])
            P.add("pool", lambda e, dtotb=dtotb: e.tensor_tensor(out=Ht_sb, in0=H_sb, in1=dtotb, op=ALU.mult),
                  reads=["H", "dtot"], writes=["t2_sb"])
            P.add("act", lambda e: e.activation(out=flat(yoff_sb), in_=ps[0][:], func=AF.Copy),
                  reads=[("ps", 0)], writes=["yoff_sb"])
            P.add("pool", lambda e: e.tensor_tensor(out=flat(H_sb), in0=flat(yoff_sb), in1=flat(Ht_sb), op=ALU.add),
                  reads=["yoff_sb", "t2_sb"], writes=["H"])
            P.add("act", lambda e: e.activation(out=flat(Hbf), in_=flat(H_sb), func=AF.Copy),
                  reads=["H"], writes=["Hbf"])
            if len(pend_attn) == 2:
                pend_attn.pop(0)()
            P.bg_flush()
            while finalizers:
                finalizers.pop(0)()
        P.bg_flush()
        while finalizers:
            finalizers.pop(0)()
        if dbg is not None and s == 0:
            dump("negm", negm, [128, L], reads=["negm"])
            dump("S", S2[(nt - 1) % 2], [128, L], reads=[("S", (nt - 1) % 2, c) for c in range((L + 511) // 512)])
            dump("thf", small[:, 160:164], [128, 4], reads=["thf", "negth", "sgn", "sg2"])
        while pend_attn:
            pend_attn.pop(0)()
        if dbg is not None and s == 0:
            dump("mixT", mixT, [128, 8, L], reads=[("mixT_a", jj) for jj in range(nt)] + [("mixT_s", jj) for jj in range(nt)])
        barrier()
        if stop_after == "M":
            continue

        dma("sp", wo_sb, wo_bf, [], ["wo_sb"], "wo_sb")
        dma("sp", gpm, gpost_d[0:1, :].partition_broadcast(128), [], ["gpm"], "gpm")
        dma("sp", gpl, gpost_d[1:2, :].partition_broadcast(128), [], ["gpl"], "gpl")
        for c in range(nt // 4):
            for tt in range(4):
                i = 4 * c + tt
                k2 = i % 2
                r0 = s * L + i * 128
                dma("sp", xt[k2], x_d[r0:r0 + 128, :], [], [("xt", k2)], f"xt{k2}")
                for nh in range(2):
                    for cc in range(8):
                        P.add("pe", lambda e, nh=nh, cc=cc, i=i: e.matmul(
                            out=ps[nh][:], lhsT=mixT[:, cc, i * 128:(i + 1) * 128],
                            rhs=wo_sb[:, cc, nh * 512:(nh + 1) * 512], start=(cc == 0), stop=(cc == 7)),
                            reads=[("mixT_a", i), ("mixT_s", i), "wo_sb"], writes=[("ps", nh)])
                    P.add("act", lambda e, nh=nh, k2=k2: e.activation(out=junkF[:, nh * 512:(nh + 1) * 512], in_=ps[nh][:], func=AF.Square,
                                                                      accum_out=ssa[k2][:, nh:nh + 1]),
                          reads=[("ps", nh)], writes=[("ssa", k2, nh), ("junkF", nh)])
                P.add("dve", lambda e, k2=k2: e.tensor_tensor(out=ssq[k2], in0=ssa[k2][:, 0:1], in1=ssa[k2][:, 1:2],
                                                              op=ALU.add),
                      reads=[("ssa", k2, 0), ("ssa", k2, 1)], writes=[("ssq", k2)])
                P.add("act", lambda e, k2=k2: e.activation(out=sq2[k2], in_=ssq[k2], func=AF.Sqrt, scale=1.0 / 1024,
                                                           bias=pp[:, PP_EPS:PP_EPS + 1]),
                      reads=[("ssq", k2), "pp"], writes=[("sq2", k2)])
                P.add("dve", lambda e, k2=k2: e.reciprocal(out=r1[k2], in_=sq2[k2]), reads=[("sq2", k2)],
                      writes=[("r1", k2)])
                for nh in range(2):
                    P.add("dve", lambda e, nh=nh, k2=k2, tt=tt: e.scalar_tensor_tensor(
                        out=h1[:, tt, nh * 512:(nh + 1) * 512], in0=ps[nh][:], scalar=r1[k2],
                        in1=gpm[:, nh * 512:(nh + 1) * 512], op0=ALU.mult, op1=ALU.mult),
                        reads=[("ps", nh), ("r1", k2), "gpm"], writes=[("h1", tt)])
                P.add("pool", lambda e, k2=k2, tt=tt: e.tensor_tensor(out=h1[:, tt, :], in0=h1[:, tt, :], in1=xt[k2],
                                                                      op=ALU.add),
                      reads=[("h1", tt), ("xt", k2)], writes=[("h1", tt)])
                P.add("act", lambda e, k2=k2, tt=tt: e.activation(out=hn[k2], in_=h1[:, tt, :], func=AF.Square,
                                                                  accum_out=ss2[k2]),
                      reads=[("h1", tt)], writes=[("ss2", k2), ("hn", k2)])
                P.add("act", lambda e, k2=k2: e.activation(out=sd2[k2], in_=ss2[k2], func=AF.Sqrt, scale=1.0 / 1024,
                                                           bias=pp[:, PP_EPS:PP_EPS + 1]),
                      reads=[("ss2", k2), "pp"], writes=[("sd2", k2)])
                P.add("dve", lambda e, k2=k2: e.reciprocal(out=r2[k2], in_=sd2[k2]), reads=[("sd2", k2)],
                      writes=[("r2", k2)])
                P.add("act", lambda e, k2=k2, tt=tt: e.activation(out=hn[k2], in_=h1[:, tt, :], func=AF.Copy,
                                                                  scale=r2[k2]),
                      reads=[("h1", tt), ("r2", k2)], writes=[("hn", k2)])
                pT2 = ps[2][:].bitcast(BF16)
                for kc in range(8):
                    P.add("pe", lambda e, k2=k2, kc=kc: e.transpose(out=pT2[:, kc * 128:(kc + 1) * 128],
                                                                     in_=hn[k2][:, kc * 128:(kc + 1) * 128],
                                                                     identity=ident_bf),
                          reads=[("hn", k2), "ident_bf"], writes=[("ps", 2)])
                P.add("act", lambda e, tt=tt: e.activation(out=hnT[:, :, tt * 128:(tt + 1) * 128],
                                                           in_=pT2.rearrange("p (a b) -> p a b", a=8), func=AF.Copy),
                      reads=[("ps", 2)], writes=[("hnT", tt)])
            ub = 0
            for fg in range(8):
                wk = fg % 2
                dma("sp", wu_sb[wk], wu_bf[:, :, fg * 512:(fg + 1) * 512], [], [("wu", wk)], f"wu{wk}")
                for f4 in range(4):
                    fc = fg * 4 + f4
                    bank = 3 + (ub % 4)
                    rk = ub % 2
                    ub += 1
                    for kc in range(8):
                        P.add("pe", lambda e, wk=wk, f4=f4, kc=kc, bank=bank: e.matmul(
                            out=ps[bank][:], lhsT=wu_sb[wk][:, kc, f4 * 128:(f4 + 1) * 128], rhs=hnT[:, kc, :],
                            start=(kc == 0), stop=(kc == 7)),
                            reads=[("wu", wk)] + [("hnT", t4) for t4 in range(4)], writes=[("ps", bank)])
                    P.add("act", lambda e, bank=bank, rk=rk: e.activation(out=r32[rk], in_=ps[bank][:], func=AF.Relu),
                          reads=[("ps", bank)], writes=[("r32", rk)])
                    P.add("pool", lambda e, fc=fc, rk=rk: e.tensor_tensor(out=aT[:, fc, :], in0=r32[rk], in1=r32[rk],
                                                                          op=ALU.mult),
                          reads=[("r32", rk)], writes=[("aT", fc)])
            for dg in range(8):
                wk = dg % 2
                dma("sp", wd_sb[wk], wd_bf[:, dg * 4:(dg + 1) * 4, :], [], [("wd", wk)], f"wd{wk}")
                for tt in range(4):
                    for nh in range(2):
                        for f4 in range(4):
                            fc = dg * 4 + f4
                            P.add("pe", lambda e, wk=wk, tt=tt, nh=nh, f4=f4, fc=fc, dg=dg: e.matmul(
                                out=ps[2 * tt + nh][:], lhsT=aT[:, fc, tt * 128:(tt + 1) * 128],
                                rhs=wd_sb[wk][:, f4, nh * 512:(nh + 1) * 512],
                                start=(dg == 0 and f4 == 0), stop=(dg == 7 and f4 == 3)),
                                reads=[("wd", wk), ("aT", fc)], writes=[("ps", 2 * tt + nh)])
            for tt in range(4):
                i = 4 * c + tt
                k2 = i % 2
                r0 = s * L + i * 128
                for nh in range(2):
                    P.add("act", lambda e, nh=nh, k2=k2, tt=tt: e.activation(
                        out=junkF[:, nh * 512:(nh + 1) * 512], in_=ps[2 * tt + nh][:], func=AF.Square,
                        accum_out=ssc[k2][:, nh:nh + 1]),
                        reads=[("ps", 2 * tt + nh)], writes=[("ssc", k2, nh), ("junkF", nh)])
                P.add("dve", lambda e, k2=k2: e.tensor_tensor(out=ss3[k2], in0=ssc[k2][:, 0:1], in1=ssc[k2][:, 1:2],
                                                              op=ALU.add),
                      reads=[("ssc", k2, 0), ("ssc", k2, 1)], writes=[("ss3", k2)])
                P.add("act", lambda e, k2=k2: e.activation(out=sd3[k2], in_=ss3[k2], func=AF.Sqrt, scale=1.0 / 1024,
                                                           bias=pp[:, PP_EPS:PP_EPS + 1]),
                      reads=[("ss3", k2), "pp"], writes=[("sd3", k2)])
                P.add("dve", lambda e, k2=k2: e.reciprocal(out=r3[k2], in_=sd3[k2]), reads=[("sd3", k2)],
                      writes=[("r3", k2)])
                for nh in range(2):
                    P.add("dve", lambda e, nh=nh, k2=k2, tt=tt: e.scalar_tensor_tensor(
                        out=ot[k2][:, nh * 512:(nh + 1) * 512], in0=ps[2 * tt + nh][:], scalar=r3[k2],
                        in1=gpl[:, nh * 512:(nh + 1) * 512], op0=ALU.mult, op1=ALU.mult),
                        reads=[("ps", 2 * tt + nh), ("r3", k2), "gpl"], writes=[("ot", k2)])
                P.add("pool", lambda e, k2=k2, tt=tt: e.tensor_tensor(out=ot[k2], in0=ot[k2], in1=h1[:, tt, :], op=ALU.add),
                      reads=[("ot", k2), ("h1", tt)], writes=[("ot", k2)])
                dma("sp", out_d[r0:r0 + 128, :], ot[k2], [("ot", k2)], [("ot", k2)], f"ot{k2}")
        barrier()

    final_keys = ["dbg_" + n for n in dbg_out] + ["ot0", "ot1"]
    P.emit(es, final_wait_keys=final_keys)
    es.close()
    if dbg is not None:
        dbg.update(dbg_out)
    return nc


PP_GMIX = 0
PP_GMLP = 8
PP_EPS = 16
PP_LNW = 17
PP_LNB = 18
PP_CONVW = 19
PP_CONVB = 51
PP_N = 59
R_DTB = 0
R_ALOG = 8
R_DSKIP = 16
R_SSDNW = 24
ROWS_N = 536


def _host_inputs(inputs, core, nseq, L):
    f = lambda a: np.ascontiguousarray(np.asarray(a, dtype=np.float32))
    x = f(inputs["x"])
    xs = x[core * nseq:(core + 1) * nseq, :L].reshape(nseq * L, D_MODEL)
    w_in = f(inputs["w_in"])[0][:, _w_in_perm()]
    pp = np.zeros((128, PP_N), np.float32)
    pp[:, PP_GMIX:PP_GMIX + 8] = f(inputs["norm_pre_mix"])[0].reshape(8, 128).T
    pp[:, PP_GMLP:PP_GMLP + 8] = f(inputs["norm_pre_mlp"])[0].reshape(8, 128).T
    pp[:, PP_EPS] = EPS
    pp[:, PP_LNW] = np.tile(f(inputs["k_idx_ln_w"])[0], 2)
    pp[:, PP_LNB] = np.tile(f(inputs["k_idx_ln_b"])[0], 2)
    cw = f(inputs["conv_w"])[0]
    pp[:, PP_CONVW:PP_CONVW + 32] = cw.reshape(4, 8, 128).transpose(2, 1, 0).reshape(128, 32)
    pp[:, PP_CONVB:PP_CONVB + 8] = f(inputs["conv_b"])[0].reshape(8, 128).T
    rows = np.zeros((1, ROWS_N), np.float32)
    rows[0, R_DTB:R_DTB + 8] = f(inputs["dt_bias"])[0]
    rows[0, R_ALOG:R_ALOG + 8] = f(inputs["a_log"])[0]
    rows[0, R_DSKIP:R_DSKIP + 8] = f(inputs["d_skip"])[0]
    rows[0, R_SSDNW:R_SSDNW + 512] = f(inputs["ssd_norm_w"])[0]
    return {
        "x": np.ascontiguousarray(xs),
        "w_in": np.ascontiguousarray(w_in),
        "w_out": f(inputs["w_out"])[0],
        "w_up": f(inputs["w_mlp_up"])[0],
        "w_down": f(inputs["w_mlp_down"])[0],
        "consts": _consts(),
        "pp": pp,
        "rows": rows,
        "gpost": np.ascontiguousarray(np.stack([f(inputs["norm_post_mix"])[0], f(inputs["norm_post_mlp"])[0]], 0)),
        "convb": f(inputs["conv_b"])[0].reshape(1, 1024),
        "rel_bias": f(inputs["rel_bias"]),
    }


def kernel(**inputs):
    nseq, nt = 2, 16
    nc = build(nseq=nseq, nt=nt)
    in_maps = [_host_inputs(inputs, c, nseq, nt * 128) for c in range(N_CORES)]
    res = run_bass_kernel_spmd(nc, in_maps, core_ids=list(range(N_CORES)))
    outs = [np.asarray(r["out"]).reshape(nseq, nt * 128, D_MODEL) for r in res.results]
    return np.concatenate(outs, axis=0).astype(np.float32)
```

```python
import numpy as np
from contextlib import ExitStack
import concourse.bass as bass
import concourse.mybir as mybir
from concourse.bass_utils import run_bass_kernel_spmd

F32 = mybir.dt.float32
BF16 = mybir.dt.bfloat16
AF = mybir.ActivationFunctionType
ALU = mybir.AluOpType
AX = mybir.AxisListType

D_MODEL = 1024
L_FULL = 2048
N_CORES = 8
EPS = 1e-6
NEG_BIG = -1.0e30

OFF_Q, OFF_K, OFF_V, OFF_QI, OFF_KI, OFF_WI, OFF_Z, OFF_XBC, OFF_DT = (
    0, 512, 640, 768, 1024, 1088, 1092, 1604, 2628)
N_FM = 17
C_TM1 = N_FM * 128
C_Z = C_TM1 + 140
W_IN_COLS = C_Z + 512


def _w_in_perm():
    cols = []
    for p in range(4):
        g, j = p // 2, p % 2
        for h in (4 * g + j, 4 * g + 2 + j):
            cols += list(range(OFF_Q + 64 * h, OFF_Q + 64 * h + 64))
    for g in range(2):
        for _ in range(2):
            cols += list(range(OFF_K + 64 * g, OFF_K + 64 * g + 64))
    for p in range(2):
        for h in (2 * p, 2 * p + 1):
            cols += list(range(OFF_QI + 64 * h, OFF_QI + 64 * h + 64))
    for _ in range(2):
        cols += list(range(OFF_KI, OFF_KI + 64))
    cols += list(range(OFF_XBC, OFF_XBC + 1024))
    cols += list(range(OFF_V, OFF_V + 128))
    cols += list(range(OFF_WI, OFF_WI + 4))
    cols += list(range(OFF_DT, OFF_DT + 8))
    cols += list(range(OFF_Z, OFF_Z + 512))
    assert len(cols) == W_IN_COLS
    return np.array(cols, dtype=np.int64)


def _t5_bucket_np(d):
    d = np.asarray(d)
    max_exact = 16
    df = np.maximum(d, 1).astype(np.float32)
    large = max_exact + (np.log(df / max_exact) / np.float32(np.log(128 / max_exact))
                         * (32 - max_exact)).astype(np.int32)
    large = np.minimum(large, 31)
    return np.where(d < max_exact, d, large)


CO_IDENT = 0
CO_U = 128
CO_NEGTRI = 256
CO_NEGL = 384
CO_BD64 = 512
CO_PERT = 640
CO_OH = 640 + 2048
CO_ONES = CO_OH + 384
CO_NU = CO_ONES + 128
CO_N = CO_NU + 128


def _consts():
    c = np.zeros((128, CO_N), np.float32)
    idx = np.arange(128)
    c[:, CO_IDENT:CO_IDENT + 128] = np.eye(128, dtype=np.float32)
    c[:, CO_U:CO_U + 128] = (idx[:, None] <= idx[None, :]).astype(np.float32)
    c[:, CO_NEGTRI:CO_NEGTRI + 128] = np.where(idx[None, :] > idx[:, None], NEG_BIG, 0.0)
    c[:, CO_NEGL:CO_NEGL + 128] = np.where(idx[None, :] < idx[:, None], -30000.0, 0.0)
    bd = np.zeros((128, 128), np.float32)
    bd[:64, :64] = 1.0 / 64
    bd[64:, 64:] = 1.0 / 64
    c[:, CO_BD64:CO_BD64 + 128] = bd
    c[:, CO_PERT:CO_PERT + 2048] = (-(2.0 ** -23) * (np.arange(2048) + 1)).astype(np.float32)[None, :]
    m = np.arange(383)
    b = _t5_bucket_np(np.maximum(m - 127, 0))
    oh = np.zeros((32, 384), np.float32)
    oh[b, m] = 1.0
    c[:32, CO_OH:CO_OH + 384] = oh
    c[:, CO_ONES:CO_ONES + 128] = 1.0
    c[:, CO_NU:CO_NU + 128] = np.eye(128, dtype=np.float32) - bd
    return c


class _Op:
    __slots__ = ("eng", "fn", "deps", "flag", "is_dma", "key", "tick")

    def __init__(self, eng, fn, is_dma, key):
        self.eng = eng
        self.fn = fn
        self.deps = []
        self.flag = False
        self.is_dma = is_dma
        self.key = key
        self.tick = 0


class Prog:
    ENGS = ("pe", "act", "dve", "pool", "sp")
    EPOCH = 12000

    def __init__(self, nc):
        self.nc = nc
        self.q = {e: [] for e in self.ENGS}
        self.last_w = {}
        self.readers = {}
        self.dma_ops = []
        self.bg = {}
        self.bg_rate = {"dve": 6, "act": 3}

    def bg_push(self, eng, fn, reads=(), writes=()):
        self.bg.setdefault(eng, []).append((fn, reads, writes))

    def bg_flush(self, eng=None, n=None):
        for e in ([eng] if eng else list(self.bg.keys())):
            q = self.bg.get(e, [])
            k = 0
            while q and (n is None or k < n):
                fn, reads, writes = q.pop(0)
                self._add(e, fn, reads, writes, None)
                k += 1

    def add(self, eng, fn, reads=(), writes=(), dma_key=None):
        if self.bg.get(eng):
            self.bg_flush(eng, self.bg_rate.get(eng, 2))
        return self._add(eng, fn, reads, writes, dma_key)

    def _add(self, eng, fn, reads=(), writes=(), dma_key=None):
        is_dma = dma_key is not None
        op = _Op(eng, fn, is_dma, dma_key)
        cand = {}
        for t in reads:
            w = self.last_w.get(t)
            if w is not None:
                cand[id(w)] = (w, True)
        for t in writes:
            w = self.last_w.get(t)
            if w is not None and id(w) not in cand:
                cand[id(w)] = (w, False)
            for r in self.readers.get(t, {}).values():
                if id(r) not in cand:
                    cand[id(r)] = (r, False)
        for d, raw in cand.values():
            if d is op:
                continue
            keep = True
            if not d.is_dma and d.eng == eng:
                keep = (eng in ("act", "dve", "pool")) or (is_dma and eng != "sp")
            if keep:
                d.flag = True
                op.deps.append(d)
        rk = ("dma", id(op)) if is_dma else eng
        for t in reads:
            self.readers.setdefault(t, {})[rk] = op
        for t in writes:
            self.last_w[t] = op
            self.readers[t] = {}
        self.q[eng].append(op)
        if is_dma:
            self.dma_ops.append(op)
        return op

    def emit(self, es, final_wait_keys=()):
        nc = self.nc
        n_epochs = {}
        for e in self.ENGS:
            cnt = 0
            for op in self.q[e]:
                if op.is_dma:
                    continue
                if op.flag:
                    cnt += 1
                    op.tick = cnt
            n_epochs[e] = (cnt + self.EPOCH - 1) // self.EPOCH
        dma_cnt = {}
        for e in self.ENGS:
            for op in self.q[e]:
                if op.is_dma:
                    dma_cnt[op.key] = dma_cnt.get(op.key, 0) + 1
                    op.tick = dma_cnt[op.key] * 16
        sems = {}
        for e in self.ENGS:
            for k in range(n_epochs[e]):
                sems[(e, k)] = es.enter_context(nc.semaphore(f"s_{e}_{k}"))
        for k in dma_cnt:
            sems[("dma", k)] = es.enter_context(nc.semaphore(f"d_{k}"))

        def ev(op):
            if op.is_dma:
                return sems[("dma", op.key)], op.tick
            k = (op.tick - 1) // self.EPOCH
            return sems[(op.eng, k)], op.tick - k * self.EPOCH

        block = es.enter_context(nc.Block())

        def run(eng_name, eng):
            waited = {}
            for op in self.q[eng_name]:
                need = {}
                for d in op.deps:
                    s, v = ev(d)
                    if need.get(s.num, (None, 0))[1] < v:
                        need[s.num] = (s, v)
                for sn, (s, v) in need.items():
                    if waited.get(sn, 0) < v:
                        eng.wait_ge(s, v)
                        waited[sn] = v
                ins = op.fn(eng)
                if op.is_dma:
                    s, _ = ev(op)
                    ins.then_inc(s, 16)
                elif op.flag:
                    s, _ = ev(op)
                    ins.then_inc(s, 1)
            if eng_name == "sp":
                for k in final_wait_keys:
                    if k in dma_cnt:
                        eng.wait_ge(sems[("dma", k)], dma_cnt[k] * 16)

        @block.tensor
        def _(e):
            run("pe", e)

        @block.scalar
        def _(e):
            run("act", e)

        @block.vector
        def _(e):
            run("dve", e)

        @block.gpsimd
        def _(e):
            run("pool", e)

        @block.sync
        def _(e):
            run("sp", e)


I8 = mybir.dt.int8
_DT_SIZE = {F32: 4, BF16: 2, I8: 1}


def build(nseq=2, nt=16, dbg=None, stop_after=None, act_from=99, bis_from=7):
    ACT_FROM = act_from
    BIS_FROM = bis_from
    L = nt * 128
    assert nt % 4 == 0
    topk = min(256, L // 4)
    ntok = nseq * L
    nc = bass.Bass("TRN2", target_bir_lowering=False)
    es = ExitStack()
    P = Prog(nc)

    def dram_in(name, shape, dt=F32):
        return nc.dram_tensor(name, list(shape), dt, kind="ExternalInput").ap()

    x_d = dram_in("x", [ntok, D_MODEL])
    win_d = dram_in("w_in", [D_MODEL, W_IN_COLS])
    wout_d = dram_in("w_out", [1024, 1024])
    wup_d = dram_in("w_up", [1024, 4096])
    wdn_d = dram_in("w_down", [4096, 1024])
    consts_d = dram_in("consts", [128, CO_N])
    pp_d = dram_in("pp", [128, PP_N])
    rows_d = dram_in("rows", [1, ROWS_N])
    gpost_d = dram_in("gpost", [2, 1024])
    convb_d = dram_in("convb", [1, 1024])
    relb_d = dram_in("rel_bias", [32, 8])
    out_d = nc.dram_tensor("out", [ntok, D_MODEL], F32, kind="ExternalOutput").ap()

    wi_bf = nc.dram_tensor("wi_bf", [128, 8, W_IN_COLS], BF16).ap()
    wo_bf = nc.dram_tensor("wo_bf", [128, 8, 1024], BF16).ap()
    wu_bf = nc.dram_tensor("wu_bf", [128, 8, 4096], BF16).ap()
    wd_bf = nc.dram_tensor("wd_bf", [128, 32, 1024], BF16).ap()
    E_d = nc.dram_tensor("E_d", [8, 128 * 384], F32)

    AW = 53200
    arena = es.enter_context(nc.sbuf_tensor("arena", [128, AW], F32))
    cur = [0]

    def take(dt, shape):
        nfree = int(np.prod(shape[1:]))
        nbytes = (nfree * _DT_SIZE[dt] + 63) // 64 * 64
        off = cur[0]
        cur[0] += nbytes
        assert cur[0] <= AW * 4, f"arena overflow {cur[0]} > {AW * 4}"
        v = arena[:, off // 4:(off + nbytes) // 4]
        if dt != F32:
            v = v.bitcast(dt)
        v = v[:, 0:nfree]
        if len(shape) == 3:
            v = v.rearrange("p (a b) -> p a b", a=shape[1])
        elif len(shape) == 4:
            v = v.rearrange("p (a b c) -> p a b c", a=shape[1], b=shape[2])
        if shape[0] < 128:
            v = v[0:shape[0]]
        return v

    def flat(v):
        if len(v.shape) == 3:
            return v.rearrange("p a b -> p (a b)")
        if len(v.shape) == 4:
            return v.rearrange("p a b c -> p (a b c)")
        return v

    consts = take(F32, [128, CO_N])
    pp = take(F32, [128, PP_N])
    rows = take(F32, [128, ROWS_N])
    ident_bf = take(BF16, [128, 128])
    I4 = take(BF16, [128, 4, 128])
    negl4_bf = take(BF16, [128, 4, 128])
    row0 = take(BF16, [1, 1024 + 768 + 128])
    c8row = row0[0:1, 0:1024].rearrange("p (a b) -> p a b", a=8)
    convb_bf = row0[0:1, 1024:1792]
    ones_bf = row0[0:1, 1792:1920]
    B8 = take(BF16, [128, 2, 8, 128])
    Dg = take(BF16, [128, 8, 4, 128])
    A_b = take(F32, [128, 8])
    rb_sb = take(F32, [32, 8])
    r31 = take(F32, [1, 8])
    small = take(F32, [128, 176])

    def sm(a, n):
        return small[:, a:a + n]
    ss = [sm(0, 1), sm(1, 1)]
    sd = [sm(2, 1), sm(3, 1)]
    rstd = [sm(4, 1), sm(5, 1)]
    wI = [sm(8, 4), sm(12, 4)]
    dtraw = [sm(16, 8), sm(24, 8)]
    m8 = [sm(32, 8), sm(40, 8)]
    rec = sm(48, 8)
    dt_sb = sm(56, 8)
    a_sb = sm(64, 8)
    cst = sm(72, 16)
    ecs = sm(88, 8)
    dte = sm(96, 8)
    dtot = sm(104, 8)
    negcs = sm(112, 8)
    ssg = sm(120, 2)
    sdg = sm(122, 2)
    rsg = sm(124, 2)
    ssa = [sm(128, 2), sm(130, 2)]
    ssq = [sm(132, 1), sm(133, 1)]
    sq2 = [sm(134, 1), sm(135, 1)]
    r1 = [sm(136, 1), sm(137, 1)]
    ss2 = [sm(138, 1), sm(139, 1)]
    sd2 = [sm(140, 1), sm(141, 1)]
    r2 = [sm(142, 1), sm(143, 1)]
    ssc = [sm(144, 2), sm(146, 2)]
    ss3 = [sm(148, 1), sm(149, 1)]
    sd3 = [sm(150, 1), sm(151, 1)]
    r3 = [sm(152, 1), sm(153, 1)]
    negth = sm(160, 1)
    sgn = sm(161, 1)
    sg2 = sm(162, 1)
    thf = sm(163, 1)
    lnd = sm(164, 8)
    thb = sm(172, 1)
    cntb = sm(173, 1)
    ubis = sm(174, 1)

    mixT = take(BF16, [128, 8, L])
    xt = [take(F32, [128, 1024])]
    phase_base = cur[0]

    NST = 4
    stage32 = [take(F32, [128, 4096]) for _ in range(NST)]
    stage16 = [take(BF16, [128, 4096]) for _ in range(NST)]
    lhs_h = [take(F32, [32, 128]) for _ in range(2)]
    Rsb = [take(F32, [128, 384]) for _ in range(2)]
    Bt32 = take(F32, [128, 2, 8, 128])
    convb32 = take(F32, [1, 1024])
    cur[0] = phase_base
    w_in_sb = take(BF16, [128, 8, W_IN_COLS])
    kT = take(BF16, [128, 2, L])
    kiT = take(BF16, [128, L])
    v_aug = take(BF16, [128, nt, 2, 65])
    S2 = [take(F32, [128, L]) for _ in range(2)]
    negm = take(BF16, [128, L])
    junk8 = take(I8, [128, L])
    xb = [take(BF16, [128, 1024]) for _ in range(2)]
    uT = [take(BF16, [128, 8, 128]) for _ in range(2)]
    qTz = [take(BF16, [128, 2, 4, 128]) for _ in range(2)]
    qiT = [take(BF16, [128, 2, 128]) for _ in range(2)]
    kiraw = take(F32, [128, 128])
    kicen = take(F32, [128, 128])
    kisq = take(F32, [128, 128])
    kisd = take(F32, [128, 128])
    kirs = take(F32, [128, 128])
    xbcT = [take(BF16, [128, 8, 131]) for _ in range(2)]
    sz = [take(F32, [128, 512]) for _ in range(2)]
    rrelu = [take(F32, [128, 512]) for _ in range(2)]
    E_sb = [take(BF16, [128, 1024]) for _ in range(2)]
    attn_tok = take(BF16, [128, 512])
    xs_tok = take(F32, [128, 8, 64])
    B_tok = take(BF16, [128, 256])
    BCT = take(BF16, [128, 4, 128])
    rL = take(F32, [128, 8, 128])
    GT_sb = take(F32, [128, 2, 128])
    WT = take(BF16, [128, 8, 128])
    X_sb = take(BF16, [128, 8, 64])
    Xd_sb = take(BF16, [128, 8, 64])
    yoff_sb = take(F32, [128, 8, 64])
    t2_sb = take(F32, [128, 8, 64])
    y_sb = take(F32, [128, 512])
    ssd_tok = take(BF16, [128, 512])
    H_sb = take(F32, [128, 8, 64])
    Ht_sb = t2_sb
    Hbf = take(BF16, [128, 8, 64])
    endM = cur[0]

    cur[0] = phase_base
    xt.append(take(F32, [128, 1024]))
    wo_sb = take(BF16, [128, 8, 1024])
    gpm = take(F32, [128, 1024])
    gpl = take(F32, [128, 1024])
    h1 = take(F32, [128, 4, 1024])
    hn = [take(BF16, [128, 1024]) for _ in range(2)]
    hnT = take(BF16, [128, 8, 512])
    aT = take(BF16, [128, 32, 512])
    wu_sb = [take(BF16, [128, 8, 512]) for _ in range(2)]
    wd_sb = [take(BF16, [128, 4, 1024]) for _ in range(2)]
    r32 = [take(F32, [128, 512]) for _ in range(2)]
    ot = [take(F32, [128, 1024]) for _ in range(2)]
    junkF = take(BF16, [128, 1024])
    endF = cur[0]
    print(f"[build] arena: base={phase_base} endM={endM} endF={endF} cap={AW * 4}")

    ps = [es.enter_context(nc.psum_tensor(f"ps{i}", [128, 512], F32)) for i in range(8)]

    dbg_out = {}

    def dump(name, src_ap, shape, reads=()):
        if dbg is None:
            return
        t = nc.dram_tensor("dbg_" + name, list(shape), src_ap.dtype, kind="ExternalOutput").ap()
        dbg_out[name] = (list(shape), src_ap.dtype)
        P.add("sp", lambda e, t=t, s=src_ap: e.dma_start(out=t, in_=s), reads=reads,
              writes=[("dbg", name)], dma_key="dbg_" + name)
        P.dma_tokens.append(("dbg", name))

    bar_n = [0]

    def barrier():
        n = bar_n[0]
        bar_n[0] += 1
        P.add("pe", lambda e: e.matmul(out=ps[7][0:1, 0:2], lhsT=ones_bf[0:1, 0:1], rhs=ones_bf[0:1, 0:2],
                                       start=True, stop=True),
              reads=["ones_bf"], writes=[("ps", 7), ("bar", n, "pe")])
        P.add("act", lambda e: e.activation(out=small[:, 156:157], in_=small[:, 156:157], func=AF.Copy),
              writes=[("bar", n, "act")])
        P.add("dve", lambda e: e.tensor_copy(out=small[:, 157:158], in_=small[:, 157:158]),
              writes=[("bar", n, "dve")])
        P.add("pool", lambda e: e.tensor_copy(out=small[:, 158:159], in_=small[:, 158:159]),
              writes=[("bar", n, "pool")])
        toks = list(P.dma_tokens)
        P.dma_tokens = []
        P.add("sp", lambda e: e.nop(), reads=toks, writes=[("bar", n, "sp")])
        allb = [("bar", n, e) for e in ("pe", "act", "dve", "pool", "sp")]
        P.add("pe", lambda e: e.matmul(out=ps[7][0:1, 0:2], lhsT=ones_bf[0:1, 0:1], rhs=ones_bf[0:1, 0:2],
                                       start=True, stop=True), reads=allb + ["ones_bf"], writes=[("ps", 7)])
        P.add("act", lambda e: e.activation(out=small[:, 156:157], in_=small[:, 156:157], func=AF.Copy), reads=allb)
        P.add("dve", lambda e: e.tensor_copy(out=small[:, 157:158], in_=small[:, 157:158]), reads=allb)
        P.add("pool", lambda e: e.tensor_copy(out=small[:, 158:159], in_=small[:, 158:159]), reads=allb)
        P.add("sp", lambda e: e.nop(), reads=allb)

    def dma(eng, out, in_, reads, writes, key):
        P.add(eng, lambda e: e.dma_start(out=out, in_=in_), reads=reads, writes=writes, dma_key=key)
        P.dma_tokens.extend(writes)

    P.dma_tokens = []

    dma("sp", consts, consts_d, [], ["consts"], "c0")
    dma("sp", pp, pp_d, [], ["pp"], "c1")
    dma("sp", rows, rows_d.partition_broadcast(128), [], ["rows"], "c2")
    dma("sp", rb_sb, relb_d, [], ["rb_sb"], "c3")
    dma("sp", r31, relb_d[31:32, :], [], ["r31"], "c4")
    dma("sp", convb32, convb_d, [], ["convb32"], "c5")
    P.add("dve", lambda e: e.memset(small, 0.0), writes=["small0"])
    P.add("dve", lambda e: e.tensor_copy(out=ident_bf, in_=consts[:, CO_IDENT:CO_IDENT + 128]),
          reads=["consts"], writes=["ident_bf"])
    P.add("dve", lambda e: e.tensor_copy(out=ones_bf, in_=consts[0:1, CO_ONES:CO_ONES + 128]),
          reads=["consts"], writes=["ones_bf"])
    P.add("dve", lambda e: e.tensor_copy(out=convb_bf, in_=convb32[0:1, 0:768]),
          reads=["convb32"], writes=["convb_bf"])
    for r4 in range(4):
        P.add("dve", lambda e, r4=r4: e.tensor_copy(out=I4[:, r4, :], in_=consts[:, CO_IDENT:CO_IDENT + 128]),
              reads=["consts"], writes=["I4"])
        P.add("dve", lambda e, r4=r4: e.tensor_copy(out=negl4_bf[:, r4, :], in_=consts[:, CO_NEGL:CO_NEGL + 128]),
              reads=["consts"], writes=["negl4"])
    for h in range(8):
        k = h % 2
        P.add("dve", lambda e, h=h, k=k: e.tensor_scalar(out=lhs_h[k], in0=consts[0:32, CO_ONES:CO_ONES + 128],
                                                         scalar1=rb_sb[:, h:h + 1], scalar2=None, op0=ALU.mult),
              reads=["consts", "rb_sb"], writes=[("lhs_h", k)])
        P.add("pe", lambda e, k=k: e.matmul(out=ps[k][:, 0:384], lhsT=lhs_h[k],
                                            rhs=consts[0:32, CO_OH:CO_OH + 384], start=True, stop=True),
              reads=[("lhs_h", k), "consts"], writes=[("ps", k)])
        P.add("act", lambda e, k=k: e.activation(out=Rsb[k], in_=ps[k][:, 0:384], func=AF.Copy),
              reads=[("ps", k)], writes=[("Rsb", k)])
        dma("sp", E_d.ap()[h, :].rearrange("(p m) -> p m", p=128), Rsb[k], [("Rsb", k)], [("E_d", h)], f"Rsb{k}")
        for dl in range(2):
            src = bass.AP(E_d, h * 128 * 384 + 127 + 128 * dl, [[383, 128], [1, 128]])
            dma("sp", Bt32[:, dl, h, :], src, [("E_d", h)], [("Bt32", dl, h)], "Bt32")
        P.add("dve", lambda e, h=h: e.tensor_scalar(out=c8row[0:1, h, :], in0=consts[0:1, CO_ONES:CO_ONES + 128],
                                                    scalar1=r31[0:1, h:h + 1], scalar2=8.0, op0=ALU.mult, op1=ALU.mult),
              reads=["consts", "r31"], writes=["c8row"])
    P.add("dve", lambda e: e.tensor_scalar(out=flat(B8), in0=flat(Bt32), scalar1=8.0, scalar2=None, op0=ALU.mult),
          reads=[("Bt32", dl, h) for dl in range(2) for h in range(8)], writes=["B8"])
    for cc in range(8):
        for k in range(4):
            P.add("dve", lambda e, cc=cc, k=k: e.tensor_scalar(
                out=Dg[:, cc, k, :], in0=consts[:, CO_IDENT:CO_IDENT + 128],
                scalar1=pp[:, PP_CONVW + cc * 4 + k:PP_CONVW + cc * 4 + k + 1], scalar2=None, op0=ALU.mult),
                reads=["consts", "pp"], writes=["Dg"])
    P.add("act", lambda e: e.activation(out=A_b, in_=rows[:, R_ALOG:R_ALOG + 8], func=AF.Exp),
          reads=["rows"], writes=["A_b"])
    P.add("dve", lambda e: e.tensor_scalar(out=A_b, in0=A_b, scalar1=-1.0, scalar2=None, op0=ALU.mult),
          reads=["A_b"], writes=["A_b"])

    win_v = win_d.rearrange("(kc p) n -> p kc n", p=128)
    wout_v = wout_d.rearrange("(cc p) n -> p cc n", p=128)
    wup_v = wup_d.rearrange("(kc p) n -> p kc n", p=128)
    wdn_v = wdn_d.rearrange("(fc p) n -> p fc n", p=128)
    chunks = []
    for kc in range(8):
        chunks.append((win_v[:, kc, :], wi_bf[:, kc, :], [W_IN_COLS], pp[:, PP_GMIX + kc:PP_GMIX + kc + 1], "act"))
    for c2 in range(2):
        chunks.append((wout_v[:, 4 * c2:4 * c2 + 4, :], wo_bf[:, 4 * c2:4 * c2 + 4, :], [4, 1024], None, "pool"))
    for kc in range(8):
        chunks.append((wup_v[:, kc, :], wu_bf[:, kc, :], [4096], pp[:, PP_GMLP + kc:PP_GMLP + kc + 1], "act"))
        chunks.append((wdn_v[:, 4 * kc:4 * kc + 4, :], wd_bf[:, 4 * kc:4 * kc + 4, :], [4, 1024], None,
                       "dve" if kc % 2 else "pool"))

    def stage_views(idx):
        src, dst, shp, sc, ce = chunks[idx]
        k = idx % NST
        nfree = int(np.prod(shp))
        s32 = stage32[k][:, 0:nfree]
        s16 = stage16[k][:, 0:nfree]
        if len(shp) == 2:
            return k, s32, s16, s32.rearrange("p (a b) -> p a b", a=shp[0]), s16.rearrange("p (a b) -> p a b", a=shp[0])
        return k, s32, s16, s32, s16

    def prep_load(idx):
        k, s32, s16, s32v, s16v = stage_views(idx)
        dma("sp", s32v, chunks[idx][0], [], [("st32", k)], f"st32_{k}")

    for idx in range(min(NST, len(chunks))):
        prep_load(idx)
    for idx in range(len(chunks)):
        src, dst, shp, sc, ce = chunks[idx]
        k, s32, s16, s32v, s16v = stage_views(idx)
        if sc is not None:
            P.add("act", lambda e, s16=s16, s32=s32, sc=sc: e.activation(out=s16, in_=s32, func=AF.Copy, scale=sc),
                  reads=[("st32", k), "pp"], writes=[("st16", k)])
        else:
            P.add(ce, lambda e, s16=s16, s32=s32: e.tensor_copy(out=s16, in_=s32), reads=[("st32", k)],
                  writes=[("st16", k)])
        dma("pool", dst, s16v, [("st16", k)], [("wscr", idx)], f"st16_{k}")
        if idx + NST < len(chunks):
            prep_load(idx + NST)
    barrier()

    for s in range(nseq):
        dma("sp", w_in_sb, wi_bf, [], ["w_in_sb"], "w_in_sb")
        P.add("pool", lambda e: e.memset(v_aug[:, :, :, 64:65], 1.0), writes=["v_ones"])
        for kq in range(2):
            P.add("pool", lambda e, kq=kq: e.memset(flat(qTz[kq]), 0.0), writes=[("qT", kq)])
        P.add("pool", lambda e: e.memset(flat(H_sb), 0.0), writes=["H"])
        P.add("pool", lambda e: e.memset(flat(Hbf), 0.0), writes=["Hbf"])
        pend_attn = []
        finalizers = []
        for i in range(nt):
            k2 = i % 2
            r0 = s * L + i * 128
            dma("sp", xt[0], x_d[r0:r0 + 128, :], [], [("xt", 0)], "xt0")
            P.add("act", lambda e, k2=k2: e.activation(out=xb[k2], in_=xt[0], func=AF.Square, accum_out=ss[k2]),
                  reads=[("xt", 0)], writes=[("ss", k2), ("xb", k2)])
            P.add("act", lambda e, k2=k2: e.activation(out=sd[k2], in_=ss[k2], func=AF.Ln, scale=1.0 / 1024,
                                                       bias=pp[:, PP_EPS:PP_EPS + 1]),
                  reads=[("ss", k2), "pp"], writes=[("sd", k2)])
            P.add("act", lambda e, k2=k2: e.activation(out=rstd[k2], in_=sd[k2], func=AF.Exp, scale=-0.5),
                  reads=[("sd", k2)], writes=[("rstd", k2)])
            P.add("act", lambda e, k2=k2: e.activation(out=xb[k2], in_=xt[0], func=AF.Copy, scale=rstd[k2]),
                  reads=[("xt", 0), ("rstd", k2)], writes=[("xb", k2)])
            pT = ps[0][:].bitcast(BF16)
            for kc in range(8):
                P.add("pe", lambda e, k2=k2, kc=kc: e.transpose(out=pT[:, kc * 128:(kc + 1) * 128],
                                                                 in_=xb[k2][:, kc * 128:(kc + 1) * 128],
                                                                 identity=ident_bf),
                      reads=[("xb", k2), "ident_bf"], writes=[("ps", 0)])
            P.add("act", lambda e, k2=k2: e.activation(out=flat(uT[k2]), in_=pT, func=AF.Copy),
                  reads=[("ps", 0)], writes=[("uT", k2)])

            def fm_group(bank, slot, g, k2=k2):
                for kc in range(8):
                    P.add("pe", lambda e, kc=kc: e.matmul(out=ps[bank][:, slot * 128:(slot + 1) * 128],
                                                          lhsT=w_in_sb[:, kc, g * 128:(g + 1) * 128],
                                                          rhs=uT[k2][:, kc, :], start=(kc == 0), stop=(kc == 7)),
                          reads=["w_in_sb", ("uT", k2)], writes=[("ps", bank)])
            for g in range(4):
                fm_group(1, g, g)
            for half in range(2):
                P.add("act", lambda e, k2=k2, half=half: e.activation(
                    out=qTz[k2][half * 64:(half + 1) * 64, half, :, :],
                    in_=ps[1][half * 64:(half + 1) * 64, :].rearrange("p (a b) -> p a b", a=4), func=AF.Copy),
                    reads=[("ps", 1)], writes=[("qT", k2)])
            for g in range(4):
                fm_group(2, g, 4 + g)
            P.add("act", lambda e, i=i: e.activation(out=kT[:, :, i * 128:(i + 1) * 128],
                                                     in_=ps[2][:, 0:256].rearrange("p (a b) -> p a b", a=2),
                                                     func=AF.Copy),
                  reads=[("ps", 2)], writes=[("kT", i)])
            P.add("act", lambda e, k2=k2: e.activation(out=flat(qiT[k2]), in_=ps[2][:, 256:512], func=AF.Copy),
                  reads=[("ps", 2)], writes=[("qiT", k2)])
            fm_group(3, 0, 8)
            P.add("act", lambda e: e.activation(out=kiraw, in_=ps[3][:, 0:128], func=AF.Copy),
                  reads=[("ps", 3)], writes=["kiraw"])
            bd = consts[:, CO_BD64:CO_BD64 + 128]
            imbd = consts[:, CO_NU:CO_NU + 128]
            P.add("pe", lambda e: e.matmul(out=ps[3][:, 128:256], lhsT=imbd, rhs=kiraw, start=True, stop=True),
                  reads=["consts", "kiraw"], writes=[("ps", 3)])
            P.add("act", lambda e: e.activation(out=kisq, in_=ps[3][:, 128:256], func=AF.Square),
                  reads=[("ps", 3)], writes=["kisq"])
            P.add("act", lambda e: e.activation(out=kicen, in_=ps[3][:, 128:256], func=AF.Copy),
                  reads=[("ps", 3)], writes=["kicen"])
            P.add("pe", lambda e: e.matmul(out=ps[3][:, 256:384], lhsT=bd, rhs=kisq, start=True, stop=True),
                  reads=["consts", "kisq"], writes=[("ps", 3)])
            P.add("act", lambda e: e.activation(out=kisd, in_=ps[3][:, 256:384], func=AF.Ln,
                                                bias=pp[:, PP_EPS:PP_EPS + 1]),
                  reads=[("ps", 3), "pp"], writes=["kisd"])
            P.add("act", lambda e: e.activation(out=kirs, in_=kisd, func=AF.Exp, scale=-0.5),
                  reads=["kisd"], writes=["kirs"])
            P.add("pool", lambda e: e.tensor_tensor(out=kicen, in0=kicen, in1=kirs, op=ALU.mult),
                  reads=["kicen", "kirs"], writes=["kicen"])
            P.add("act", lambda e, i=i: e.activation(out=kiT[:, i * 128:(i + 1) * 128], in_=kicen,
                                                     func=AF.Identity, scale=pp[:, PP_LNW:PP_LNW + 1],
                                                     bias=pp[:, PP_LNB:PP_LNB + 1]),
                  reads=["kicen", "pp"], writes=[("kiT", i)])
            for cc in range(8):
                fm_group(4 + cc // 4, cc % 4, 9 + cc)
            if i == 0:
                P.add("pool", lambda e, k2=k2: e.memset(xbcT[k2][:, :, 0:3], 0.0), writes=[("xbcT", k2)])
            else:
                P.add("pool", lambda e, k2=k2: e.tensor_copy(out=xbcT[k2][:, :, 0:3], in_=xbcT[1 - k2][:, :, 128:131]),
                      reads=[("xbcT", 1 - k2)], writes=[("xbcT", k2)])
            for hb in range(2):
                P.add("act", lambda e, k2=k2, hb=hb: e.activation(
                    out=xbcT[k2][:, 4 * hb:4 * hb + 4, 3:131],
                    in_=ps[4 + hb][:].rearrange("p (a b) -> p a b", a=4), func=AF.Copy),
                    reads=[("ps", 4 + hb)], writes=[("xbcT", k2)])
            for kc in range(8):
                P.add("pe", lambda e, kc=kc, k2=k2: e.matmul(out=ps[6][:, 0:140], lhsT=uT[k2][:, kc, :],
                                                             rhs=w_in_sb[:, kc, C_TM1:C_TM1 + 140],
                                                             start=(kc == 0), stop=(kc == 7)),
                      reads=["w_in_sb", ("uT", k2)], writes=[("ps", 6)])
            for kc in range(8):
                P.add("pe", lambda e, kc=kc, k2=k2: e.matmul(out=ps[7][:], lhsT=uT[k2][:, kc, :],
                                                             rhs=w_in_sb[:, kc, C_Z:C_Z + 512],
                                                             start=(kc == 0), stop=(kc == 7)),
                      reads=["w_in_sb", ("uT", k2)], writes=[("ps", 7)])
            P.add("act", lambda e, i=i: e.activation(out=v_aug[:, i, :, 0:64],
                                                     in_=ps[6][:, 0:128].rearrange("p (a b) -> p a b", a=2),
                                                     func=AF.Copy),
                  reads=[("ps", 6)], writes=[("v", i)])
            P.add("act", lambda e, k2=k2: e.activation(out=wI[k2], in_=ps[6][:, 128:132], func=AF.Copy, scale=1.0 / 16),
                  reads=[("ps", 6)], writes=[("wI", k2)])
            P.add("act", lambda e, k2=k2: e.activation(out=dtraw[k2], in_=ps[6][:, 132:140], func=AF.Copy),
                  reads=[("ps", 6)], writes=[("dtraw", k2)])
            P.add("act", lambda e, k2=k2: e.activation(out=sz[k2], in_=ps[7][:], func=AF.Silu),
                  reads=[("ps", 7)], writes=[("sz", k2)])

            n = (i + 1) * 128
            Sb = S2[k2]
            cidx = 0
            nchunk = (n + 511) // 512
            for c in range(nchunk):
                wd = min(512, n - c * 512)
                for h in range(4):
                    pair, half = h // 2, h % 2
                    bk = cidx % 2
                    cidx += 1
                    P.add("pe", lambda e, k2=k2, pair=pair, half=half, bk=bk, c=c, wd=wd: e.matmul(
                        out=ps[bk][:, 0:wd], lhsT=qiT[k2][half * 64:(half + 1) * 64, pair, :],
                        rhs=kiT[half * 64:(half + 1) * 64, c * 512:c * 512 + wd], start=True, stop=True),
                        reads=[("qiT", k2)] + [("kiT", jj) for jj in range(4 * c, min(4 * c + 4, i + 1))],
                        writes=[("ps", bk)])
                    P.add("act", lambda e, bk=bk, wd=wd: e.activation(out=rrelu[bk][:, 0:wd], in_=ps[bk][:, 0:wd],
                                                                       func=AF.Relu),
                          reads=[("ps", bk)], writes=[("rrelu", bk)])
                    prev = (consts[:, CO_PERT + c * 512:CO_PERT + c * 512 + wd] if h == 0
                            else Sb[:, c * 512:c * 512 + wd])
                    P.add("dve", lambda e, k2=k2, h=h, bk=bk, c=c, wd=wd, prev=prev, Sb=Sb: e.scalar_tensor_tensor(
                        out=Sb[:, c * 512:c * 512 + wd], in0=rrelu[bk][:, 0:wd], scalar=wI[k2][:, h:h + 1],
                        in1=prev, op0=ALU.mult, op1=ALU.add),
                        reads=[("rrelu", bk), ("wI", k2), "consts", ("S", k2, c)], writes=[("S", k2, c)])
            cl = i // 4
            P.add("pool", lambda e, i=i, Sb=Sb: e.tensor_tensor(out=Sb[:, i * 128:(i + 1) * 128],
                                                                in0=Sb[:, i * 128:(i + 1) * 128],
                                                                in1=consts[:, CO_NEGTRI:CO_NEGTRI + 128], op=ALU.add),
                  reads=[("S", k2, cl), "consts"], writes=[("S", k2, cl)])
            allS = [("S", k2, c) for c in range(nchunk)]

            def queue_topk(i=i, n=n, Sb=Sb, allS=allS):
                if n > topk and i >= ACT_FROM:
                    K = 28
                    for k in range(K):
                        dk = 8.0 / (2 ** k)
                        if k == 0:
                            P.bg_push("act", lambda e: e.activation(out=junk8[:, 0:n], in_=Sb[:, 0:n], func=AF.Sign,
                                                                    accum_out=sgn),
                                      reads=allS, writes=["sgn", "junk8"])
                        else:
                            P.bg_push("act", lambda e: e.activation(out=junk8[:, 0:n], in_=Sb[:, 0:n], func=AF.Sign,
                                                                    bias=negth, accum_out=sgn),
                                      reads=allS + ["negth"], writes=["sgn", "junk8"])
                        P.bg_push("act", lambda e: e.activation(out=sg2, in_=sgn, func=AF.Sign,
                                                                bias=float(n - (2 * topk - 1))),
                                  reads=["sgn"], writes=["sg2"])
                        if k == 0:
                            P.bg_push("act", lambda e, dk=dk: e.activation(out=negth, in_=sg2, func=AF.Identity,
                                                                          scale=-dk / 2),
                                      reads=["sg2"], writes=["negth"])
                        else:
                            P.bg_push("act", lambda e, dk=dk: e.activation(out=negth, in_=sg2, func=AF.Identity,
                                                                          scale=-dk / 2, bias=negth),
                                      reads=["sg2", "negth"], writes=["negth"])
                    dK = 8.0 / (2 ** K)
                    P.bg_push("act", lambda e: e.activation(out=thf, in_=negth, func=AF.Identity, scale=-1.0, bias=-dK),
                              reads=["negth"], writes=["thf"])
                    finalizers.append(lambda: P.add("dve", lambda e: e.tensor_scalar(
                        out=negm[:, 0:n], in0=Sb[:, 0:n], scalar1=thf, scalar2=-30000.0,
                        op0=ALU.is_lt, op1=ALU.mult),
                        reads=allS + ["thf"], writes=["negm"]))
                elif n > topk and i >= BIS_FROM:
                    K = 27
                    P.add("dve", lambda e: e.memset(thb, 0.0), writes=["thb"])
                    for k in range(K):
                        dk = 4.0 / (2 ** k)
                        P.add("dve", lambda e: e.tensor_scalar(out=junk8[:, 0:n], in0=Sb[:, 0:n], scalar1=thb, scalar2=None,
                                                               op0=ALU.is_ge, op1=ALU.add, accum_out=cntb),
                              reads=allS + ["thb"], writes=["cntb", "junk8"])
                        P.add("dve", lambda e, dk=dk: e.tensor_scalar(out=ubis, in0=cntb, scalar1=float(topk) - 0.5, scalar2=dk,
                                                                      op0=ALU.is_ge, op1=ALU.mult),
                              reads=["cntb"], writes=["ub"])
                        P.add("dve", lambda e, dk=dk: e.scalar_tensor_tensor(out=thb, in0=thb, scalar=-dk / 2, in1=ubis,
                                                                             op0=ALU.add, op1=ALU.add),
                              reads=["thb", "ub"], writes=["thb"])
                    dK = 4.0 / (2 ** K)
                    P.add("dve", lambda e: e.scalar_tensor_tensor(out=ubis, in0=thb, scalar=-1.0, in1=thb,
                                                                  op0=ALU.mult, op1=ALU.max),
                          reads=["thb"], writes=["ub"])
                    P.add("dve", lambda e: e.tensor_scalar(out=ubis, in0=ubis, scalar1=2.4e-7, scalar2=None, op0=ALU.mult),
                          reads=["ub"], writes=["ub"])
                    P.add("dve", lambda e: e.scalar_tensor_tensor(out=thf, in0=thb, scalar=-dK, in1=ubis,
                                                                  op0=ALU.add, op1=ALU.subtract),
                          reads=["thb", "ub"], writes=["thf"])
                    finalizers.append(lambda: P.add("dve", lambda e: e.tensor_scalar(
                        out=negm[:, 0:n], in0=Sb[:, 0:n], scalar1=thf, scalar2=-30000.0,
                        op0=ALU.is_lt, op1=ALU.mult),
                        reads=allS + ["thf"], writes=["negm"]))
                elif n > topk:
                    for r in range(topk // 8):
                        P.add("dve", lambda e, r=r: e.max(out=m8[r % 2], in_=Sb[:, 0:n]),
                              reads=allS, writes=[("m8", r % 2)])
                        P.add("dve", lambda e, r=r: e.match_replace(
                            out=Sb[:, 0:n], in_to_replace=m8[r % 2], in_values=Sb[:, 0:n], imm_value=-3.0e38),
                            reads=[("m8", r % 2)] + allS, writes=allS)
                    finalizers.append(lambda: P.add("dve", lambda e: e.tensor_scalar(
                        out=negm[:, 0:n], in0=Sb[:, 0:n], scalar1=-1.0e38, scalar2=-30000.0,
                        op0=ALU.is_gt, op1=ALU.mult),
                        reads=allS, writes=["negm"]))
                else:
                    finalizers.append(lambda: P.add("dve", lambda e: e.tensor_scalar(
                        out=negm[:, 0:n], in0=Sb[:, 0:n], scalar1=-1.0e29, scalar2=-30000.0,
                        op0=ALU.is_lt, op1=ALU.mult),
                        reads=allS, writes=["negm"]))

            def attention(i=i, k2=k2):
                for j in range(i + 1):
                    ek = j % 2
                    for g in range(2):
                        bank = 2 + g
                        for half in range(2):
                            P.add("pe", lambda e, g=g, half=half, j=j, bank=bank: e.matmul(
                                out=ps[bank][:, half * 256:(half + 1) * 256],
                                lhsT=kT[:, g, j * 128:(j + 1) * 128],
                                rhs=qTz[k2][:, half, 2 * g:2 * g + 2, :],
                                start=(half == 0), stop=False),
                                reads=[("kT", j), ("qT", k2)], writes=[("ps", bank)])
                        P.add("pe", lambda e, j=j, bank=bank: e.matmul(
                            out=ps[bank][:], lhsT=negm[:, j * 128:(j + 1) * 128], rhs=flat(I4), start=False, stop=False),
                            reads=["negm", "I4"], writes=[("ps", bank)])
                        if i - j <= 1:
                            P.add("pe", lambda e, g=g, dl=i - j, bank=bank: e.matmul(
                                out=ps[bank][:], lhsT=ident_bf, rhs=B8[:, dl, 4 * g:4 * g + 4, :], start=False, stop=True),
                                reads=["ident_bf", "B8"], writes=[("ps", bank)])
                        else:
                            P.add("pe", lambda e, g=g, bank=bank: e.matmul(
                                out=ps[bank][:], lhsT=ones_bf[0:1, :], rhs=c8row[0:1, 4 * g:4 * g + 4, :],
                                start=False, stop=True),
                                reads=["ones_bf", "c8row"], writes=[("ps", bank)])
                        P.add("act", lambda e, g=g, ek=ek, bank=bank: e.activation(
                            out=E_sb[ek][:, g * 512:(g + 1) * 512], in_=ps[bank][:], func=AF.Exp, scale=0.125),
                            reads=[("ps", bank)], writes=[("E", ek, g)])
                    for h in range(8):
                        bpv = 4 + h // 4
                        hh = h % 4
                        P.add("pe", lambda e, h=h, hh=hh, bpv=bpv, ek=ek, j=j: e.matmul(
                            out=ps[bpv][:, hh * 65:hh * 65 + 65], lhsT=E_sb[ek][:, h * 128:(h + 1) * 128],
                            rhs=v_aug[:, j, h // 4, :], start=(j == 0 and hh == 0), stop=(j == i), skip_group_check=True),
                            reads=[("E", ek, h // 4), ("v", j), "v_ones"], writes=[("ps", bpv)])
                for b2 in range(2):
                    psv = ps[4 + b2][:, 0:260].rearrange("p (h c) -> p h c", c=65)
                    P.add("act", lambda e, b2=b2, psv=psv: e.activation(out=lnd[:, 4 * b2:4 * b2 + 4], in_=psv[:, :, 64],
                                                                       func=AF.Ln),
                          reads=[("ps", 4 + b2)], writes=[("lnd", b2)])
                    P.add("act", lambda e, b2=b2: e.activation(out=rec[:, 4 * b2:4 * b2 + 4], in_=lnd[:, 4 * b2:4 * b2 + 4],
                                                               func=AF.Exp, scale=-1.0),
                          reads=[("lnd", b2)], writes=[("rec", b2)])
                    for hh in range(4):
                        h = 4 * b2 + hh
                        P.add("act", lambda e, h=h, hh=hh, psv=psv: e.activation(
                            out=attn_tok[:, h * 64:(h + 1) * 64], in_=psv[:, hh, 0:64], func=AF.Copy,
                            scale=rec[:, h:h + 1]),
                            reads=[("ps", 4 + b2), ("rec", b2)], writes=["attn_tok"])
                pT6 = ps[6][:].bitcast(BF16)
                for c4 in range(4):
                    P.add("pe", lambda e, c4=c4: e.transpose(out=pT6[:, c4 * 128:(c4 + 1) * 128],
                                                             in_=attn_tok[:, c4 * 128:(c4 + 1) * 128], identity=ident_bf),
                          reads=["attn_tok", "ident_bf"], writes=[("ps", 6)])
                P.add("act", lambda e: e.activation(out=mixT[:, 0:4, i * 128:(i + 1) * 128],
                                                    in_=pT6[:, 0:512].rearrange("p (a b) -> p a b", a=4),
                                                    func=AF.Copy),
                      reads=[("ps", 6)], writes=[("mixT_a", i)])

            pend_attn.append(attention)
            queue_topk()

            xk = xbcT[k2]
            for cc in range(6):
                bank = 0 if cc < 4 else 1
                col = (cc % 4) * 128
                for k in range(4):
                    P.add("pe", lambda e, cc=cc, k=k, bank=bank, col=col, xk=xk: e.matmul(
                        out=ps[bank][:, col:col + 128], lhsT=xk[:, cc, k:k + 128], rhs=Dg[:, cc, k, :],
                        start=(k == 0), stop=False),
                        reads=[("xbcT", k2), "Dg"], writes=[("ps", bank)])
                P.add("pe", lambda e, cc=cc, bank=bank, col=col: e.matmul(
                    out=ps[bank][:, col:col + 128], lhsT=ones_bf[0:1, :], rhs=convb_bf[0:1, cc * 128:(cc + 1) * 128],
                    start=False, stop=True),
                    reads=["ones_bf", "convb_bf"], writes=[("ps", bank)])
            P.add("act", lambda e: e.activation(out=flat(xs_tok), in_=ps[0][:], func=AF.Silu),
                  reads=[("ps", 0)], writes=["xs_tok"])
            P.add("act", lambda e: e.activation(out=B_tok, in_=ps[1][:, 0:256], func=AF.Silu),
                  reads=[("ps", 1)], writes=["B_tok"])
            for c4 in range(4):
                cc = 4 + c4
                for k in range(4):
                    P.add("pe", lambda e, cc=cc, c4=c4, k=k, xk=xk: e.matmul(
                        out=ps[2][:, c4 * 128:(c4 + 1) * 128], lhsT=Dg[:, cc, k, :], rhs=xk[:, cc, k:k + 128],
                        start=(k == 0), stop=(k == 3)),
                        reads=[("xbcT", k2), "Dg"], writes=[("ps", 2)])
                P.add("act", lambda e, cc=cc, c4=c4: e.activation(
                    out=BCT[:, c4, :], in_=ps[2][:, c4 * 128:(c4 + 1) * 128], func=AF.Silu,
                    bias=pp[:, PP_CONVB + cc:PP_CONVB + cc + 1]),
                    reads=[("ps", 2), "pp"], writes=["BCT"])
            P.add("pool", lambda e, k2=k2: e.tensor_tensor(out=dt_sb, in0=dtraw[k2], in1=rows[:, R_DTB:R_DTB + 8],
                                                           op=ALU.add),
                  reads=[("dtraw", k2), "rows"], writes=["dt_sb"])
            P.add("act", lambda e: e.activation(out=dt_sb, in_=dt_sb, func=AF.Exp), reads=["dt_sb"], writes=["dt_sb"])
            P.add("act", lambda e: e.activation(out=dt_sb, in_=dt_sb, func=AF.Ln, bias=1.0),
                  reads=["dt_sb"], writes=["dt_sb"])
            P.add("pool", lambda e: e.tensor_tensor(out=a_sb, in0=dt_sb, in1=A_b, op=ALU.mult),
                  reads=["dt_sb", "A_b"], writes=["a_sb"])
            for g in range(2):
                P.add("pe", lambda e, g=g: e.matmul(out=ps[3][:, g * 128:(g + 1) * 128], lhsT=BCT[:, g, :],
                                                    rhs=BCT[:, 2 + g, :], start=True, stop=True),
                      reads=["BCT"], writes=[("ps", 3)])
            P.add("pe", lambda e: e.matmul(out=ps[3][:, 256:264], lhsT=consts[:, CO_U:CO_U + 128], rhs=a_sb,
                                           start=True, stop=True),
                  reads=["consts", "a_sb"], writes=[("ps", 3)])
            P.add("pe", lambda e: e.matmul(out=ps[3][:, 264:272], lhsT=consts[:, CO_ONES:CO_ONES + 128], rhs=a_sb,
                                           start=True, stop=True),
                  reads=["consts", "a_sb"], writes=[("ps", 3)])
            P.add("act", lambda e: e.activation(out=flat(GT_sb), in_=ps[3][:, 0:256], func=AF.Copy),
                  reads=[("ps", 3)], writes=["GT_sb"])
            P.add("act", lambda e: e.activation(out=cst, in_=ps[3][:, 256:272], func=AF.Copy),
                  reads=[("ps", 3)], writes=["cst"])
            P.add("act", lambda e: e.activation(out=ecs, in_=cst[:, 0:8], func=AF.Exp), reads=["cst"], writes=["ecs"])
            P.add("act", lambda e: e.activation(out=dtot, in_=cst[:, 8:16], func=AF.Exp), reads=["cst"], writes=["dtot"])
            P.add("pool", lambda e: e.tensor_tensor(out=dte, in0=cst[:, 8:16], in1=cst[:, 0:8], op=ALU.subtract),
                  reads=["cst"], writes=["dte"])
            P.add("act", lambda e: e.activation(out=dte, in_=dte, func=AF.Exp), reads=["dte"], writes=["dte"])
            P.add("pool", lambda e: e.tensor_scalar(out=negcs, in0=cst[:, 0:8], scalar1=-1.0, scalar2=1.0, op0=ALU.mult,
                                                   op1=ALU.mult),
                  reads=["cst"], writes=["negcs"])
            Ub = consts[:, CO_U:CO_U + 128].unsqueeze(1).broadcast_to([128, 8, 128])
            ab = a_sb.unsqueeze(2).broadcast_to([128, 8, 128])
            P.add("pool", lambda e, Ub=Ub, ab=ab: e.tensor_tensor(out=rL, in0=Ub, in1=ab, op=ALU.mult),
                  reads=["consts", "a_sb"], writes=["rL"])
            for b2 in range(2):
                P.add("pe", lambda e, b2=b2: e.matmul(out=ps[4 + b2][:], lhsT=consts[:, CO_ONES:CO_ONES + 128],
                                                      rhs=rL[:, 4 * b2:4 * b2 + 4, :], start=True, stop=False),
                      reads=["consts", "rL"], writes=[("ps", 4 + b2)])
                P.add("pe", lambda e, b2=b2: e.matmul(out=ps[4 + b2][:], lhsT=ident_bf, rhs=flat(negl4_bf),
                                                      start=False, stop=True),
                      reads=["ident_bf", "negl4"], writes=[("ps", 4 + b2)])
            for h in range(8):
                b2, hh = h // 4, h % 4
                P.add("act", lambda e, h=h, b2=b2, hh=hh: e.activation(
                    out=rL[:, h, :], in_=ps[4 + b2][:, hh * 128:(hh + 1) * 128], func=AF.Exp,
                    bias=negcs[:, h:h + 1]),
                    reads=[("ps", 4 + b2), "negcs"], writes=["rL"])
            for b2 in range(2):
                gtb = GT_sb[:, b2, :].unsqueeze(1).broadcast_to([128, 4, 128])
                P.add("pool", lambda e, b2=b2, gtb=gtb: e.tensor_tensor(out=WT[:, 4 * b2:4 * b2 + 4, :],
                                                                       in0=rL[:, 4 * b2:4 * b2 + 4, :], in1=gtb,
                                                                       op=ALU.mult),
                      reads=["rL", "GT_sb"], writes=[("WT", b2)])
            dtb = dt_sb.unsqueeze(2).broadcast_to([128, 8, 64])
            dteb = dte.unsqueeze(2).broadcast_to([128, 8, 64])
            P.add("pool", lambda e, dtb=dtb: e.tensor_tensor(out=X_sb, in0=xs_tok, in1=dtb, op=ALU.mult),
                  reads=["xs_tok", "dt_sb"], writes=["X_sb"])
            P.add("pool", lambda e, dteb=dteb: e.tensor_tensor(out=Xd_sb, in0=X_sb, in1=dteb, op=ALU.mult),
                  reads=["X_sb", "dte"], writes=["Xd_sb"])
            for h in range(8):
                P.add("pe", lambda e, h=h: e.matmul(out=ps[6][:, h * 64:(h + 1) * 64], lhsT=WT[:, h, :], rhs=X_sb[:, h, :],
                                                    start=True, stop=True),
                      reads=[("WT", h // 4), "X_sb"], writes=[("ps", 6)])
            for g in range(2):
                P.add("pe", lambda e, g=g: e.matmul(out=ps[7][:, g * 256:(g + 1) * 256], lhsT=BCT[:, 2 + g, :],
                                                    rhs=Hbf[:, 4 * g:4 * g + 4, :], start=True, stop=True),
                      reads=["BCT", "Hbf"], writes=[("ps", 7)])
            P.add("act", lambda e: e.activation(out=flat(yoff_sb), in_=ps[7][:], func=AF.Copy),
                  reads=[("ps", 7)], writes=["yoff_sb"])
            ecsb = ecs.unsqueeze(2).broadcast_to([128, 8, 64])
            dskb = rows[:, R_DSKIP:R_DSKIP + 8].unsqueeze(2).broadcast_to([128, 8, 64])
            P.add("pool", lambda e, ecsb=ecsb: e.tensor_tensor(out=yoff_sb, in0=yoff_sb, in1=ecsb, op=ALU.mult),
                  reads=["yoff_sb", "ecs"], writes=["yoff_sb"])
            P.add("pool", lambda e, dskb=dskb: e.tensor_tensor(out=t2_sb, in0=xs_tok, in1=dskb, op=ALU.mult),
                  reads=["xs_tok", "rows"], writes=["t2_sb"])
            P.add("pool", lambda e: e.tensor_tensor(out=yoff_sb, in0=yoff_sb, in1=t2_sb, op=ALU.add),
                  reads=["yoff_sb", "t2_sb"], writes=["yoff_sb"])
            P.add("act", lambda e: e.activation(out=y_sb, in_=ps[6][:], func=AF.Copy),
                  reads=[("ps", 6)], writes=["y_sb"])
            P.add("pool", lambda e: e.tensor_tensor(out=y_sb, in0=y_sb, in1=flat(yoff_sb), op=ALU.add),
                  reads=["y_sb", "yoff_sb"], writes=["y_sb"])
            P.add("pool", lambda e, k2=k2: e.tensor_tensor(out=y_sb, in0=y_sb, in1=sz[k2], op=ALU.mult),
                  reads=["y_sb", ("sz", k2)], writes=["y_sb"])
            for g in range(2):
                P.add("act", lambda e, g=g: e.activation(out=flat(t2_sb)[:, g * 256:(g + 1) * 256],
                                                         in_=y_sb[:, g * 256:(g + 1) * 256],
                                                         func=AF.Square, accum_out=ssg[:, g:g + 1]),
                      reads=["y_sb"], writes=[("ssg", g), "t2_sb"])
            P.add("act", lambda e: e.activation(out=sdg, in_=ssg, func=AF.Ln, scale=1.0 / 256,
                                                bias=pp[:, PP_EPS:PP_EPS + 1]),
                  reads=[("ssg", 0), ("ssg", 1), "pp"], writes=["sdg"])
            P.add("act", lambda e: e.activation(out=rsg, in_=sdg, func=AF.Exp, scale=-0.5), reads=["sdg"], writes=["rsg"])
            for g in range(2):
                P.add("act", lambda e, g=g: e.activation(out=flat(t2_sb)[:, g * 256:(g + 1) * 256],
                                                         in_=y_sb[:, g * 256:(g + 1) * 256], func=AF.Copy,
                                                         scale=rsg[:, g:g + 1]),
                      reads=["y_sb", "rsg"], writes=["t2_sb"])
                P.add("pool", lambda e, g=g: e.tensor_tensor(
                    out=ssd_tok[:, g * 256:(g + 1) * 256], in0=flat(t2_sb)[:, g * 256:(g + 1) * 256],
                    in1=rows[:, R_SSDNW + g * 256:R_SSDNW + (g + 1) * 256], op=ALU.mult),
                    reads=["t2_sb", "rows"], writes=["ssd_tok"])
            pT7 = ps[7][:].bitcast(BF16)
            for c4 in range(4):
                P.add("pe", lambda e, c4=c4: e.transpose(out=pT7[:, c4 * 128:(c4 + 1) * 128],
                                                         in_=ssd_tok[:, c4 * 128:(c4 + 1) * 128], identity=ident_bf),
                      reads=["ssd_tok", "ident_bf"], writes=[("ps", 7)])
            P.add("act", lambda e, i=i: e.activation(out=mixT[:, 4:8, i * 128:(i + 1) * 128],
                                                     in_=pT7[:, 0:512].rearrange("p (a b) -> p a b", a=4), func=AF.Copy),
                  reads=[("ps", 7)], writes=[("mixT_s", i)])
            for g in range(2):
                P.add("pe", lambda e, g=g: e.matmul(out=ps[0][:, g * 256:(g + 1) * 256], lhsT=B_tok[:, g * 128:(g + 1) * 128],
                                                    rhs=Xd_sb[:, 4 * g:4 * g + 4, :], start=True, stop=True),
                      reads=["B_tok", "Xd_sb"], writes=[("ps", 0)])
            dtotb = dtot.unsqueeze(2).broadcast_to([128, 8, 64])
            P.add("pool", lambda e, dtotb=dtotb: e.tensor_tensor(out=Ht_sb, in0=H_sb, in1=dtotb, op=ALU.mult),
                  reads=["H", "dtot"], writes=["t2_sb"])
            P.add("act", lambda e: e.activation(out=flat(yoff_sb), in_=ps[0][:], func=AF.Copy),
                  reads=[("ps", 0)], writes=["yoff_sb"])
            P.add("pool", lambda e: e.tensor_tensor(out=flat(H_sb), in0=flat(yoff_sb), in1=flat(Ht_sb), op=ALU.add),
                  reads=["yoff_sb", "t2_sb"], writes=["H"])
            P.add("act", lambda e: e.activation(out=flat(Hbf), in_=flat(H_sb), func=AF.Copy),
                  reads=["H"], writes=["Hbf"])
            if len(pend_attn) == 2:
                pend_attn.pop(0)()
            P.bg_flush()
            while finalizers:
                finalizers.pop(0)()
        P.bg_flush()
        while finalizers:
            finalizers.pop(0)()
        if dbg is not None and s == 0:
            dump("negm", negm, [128, L], reads=["negm"])
            dump("S", S2[(nt - 1) % 2], [128, L], reads=[("S", (nt - 1) % 2, c) for c in range((L + 511) // 512)])
            dump("thf", small[:, 160:164], [128, 4], reads=["thf", "negth", "sgn", "sg2"])
        while pend_attn:
            pend_attn.pop(0)()
        if dbg is not None and s == 0:
            dump("mixT", mixT, [128, 8, L], reads=[("mixT_a", jj) for jj in range(nt)] + [("mixT_s", jj) for jj in range(nt)])
        barrier()
        if stop_after == "M":
            continue

        dma("sp", wo_sb, wo_bf, [], ["wo_sb"], "wo_sb")
        dma("sp", gpm, gpost_d[0:1, :].partition_broadcast(128), [], ["gpm"], "gpm")
        dma("sp", gpl, gpost_d[1:2, :].partition_broadcast(128), [], ["gpl"], "gpl")
        for c in range(nt // 4):
            for tt in range(4):
                i = 4 * c + tt
                k2 = i % 2
                r0 = s * L + i * 128
                dma("sp", xt[k2], x_d[r0:r0 + 128, :], [], [("xt", k2)], f"xt{k2}")
                for nh in range(2):
                    for cc in range(8):
                        P.add("pe", lambda e, nh=nh, cc=cc, i=i: e.matmul(
                            out=ps[nh][:], lhsT=mixT[:, cc, i * 128:(i + 1) * 128],
                            rhs=wo_sb[:, cc, nh * 512:(nh + 1) * 512], start=(cc == 0), stop=(cc == 7)),
                            reads=[("mixT_a", i), ("mixT_s", i), "wo_sb"], writes=[("ps", nh)])
                    P.add("act", lambda e, nh=nh, k2=k2: e.activation(out=junkF[:, nh * 512:(nh + 1) * 512], in_=ps[nh][:], func=AF.Square,
                                                                      accum_out=ssa[k2][:, nh:nh + 1]),
                          reads=[("ps", nh)], writes=[("ssa", k2, nh), ("junkF", nh)])
                P.add("dve", lambda e, k2=k2: e.tensor_tensor(out=ssq[k2], in0=ssa[k2][:, 0:1], in1=ssa[k2][:, 1:2],
                                                              op=ALU.add),
                      reads=[("ssa", k2, 0), ("ssa", k2, 1)], writes=[("ssq", k2)])
                P.add("act", lambda e, k2=k2: e.activation(out=sq2[k2], in_=ssq[k2], func=AF.Sqrt, scale=1.0 / 1024,
                                                           bias=pp[:, PP_EPS:PP_EPS + 1]),
                      reads=[("ssq", k2), "pp"], writes=[("sq2", k2)])
                P.add("dve", lambda e, k2=k2: e.reciprocal(out=r1[k2], in_=sq2[k2]), reads=[("sq2", k2)],
                      writes=[("r1", k2)])
                for nh in range(2):
                    P.add("dve", lambda e, nh=nh, k2=k2, tt=tt: e.scalar_tensor_tensor(
                        out=h1[:, tt, nh * 512:(nh + 1) * 512], in0=ps[nh][:], scalar=r1[k2],
                        in1=gpm[:, nh * 512:(nh + 1) * 512], op0=ALU.mult, op1=ALU.mult),
                        reads=[("ps", nh), ("r1", k2), "gpm"], writes=[("h1", tt)])
                P.add("pool", lambda e, k2=k2, tt=tt: e.tensor_tensor(out=h1[:, tt, :], in0=h1[:, tt, :], in1=xt[k2],
                                                                      op=ALU.add),
                      reads=[("h1", tt), ("xt", k2)], writes=[("h1", tt)])
                P.add("act", lambda e, k2=k2, tt=tt: e.activation(out=hn[k2], in_=h1[:, tt, :], func=AF.Square,
                                                                  accum_out=ss2[k2]),
                      reads=[("h1", tt)], writes=[("ss2", k2), ("hn", k2)])
                P.add("act", lambda e, k2=k2: e.activation(out=sd2[k2], in_=ss2[k2], func=AF.Sqrt, scale=1.0 / 1024,
                                                           bias=pp[:, PP_EPS:PP_EPS + 1]),
                      reads=[("ss2", k2), "pp"], writes=[("sd2", k2)])
                P.add("dve", lambda e, k2=k2: e.reciprocal(out=r2[k2], in_=sd2[k2]), reads=[("sd2", k2)],
                      writes=[("r2", k2)])
                P.add("act", lambda e, k2=k2, tt=tt: e.activation(out=hn[k2], in_=h1[:, tt, :], func=AF.Copy,
                                                                  scale=r2[k2]),
                      reads=[("h1", tt), ("r2", k2)], writes=[("hn", k2)])
                pT2 = ps[2][:].bitcast(BF16)
                for kc in range(8):
                    P.add("pe", lambda e, k2=k2, kc=kc: e.transpose(out=pT2[:, kc * 128:(kc + 1) * 128],
                                                                     in_=hn[k2][:, kc * 128:(kc + 1) * 128],
                                                                     identity=ident_bf),
                          reads=[("hn", k2), "ident_bf"], writes=[("ps", 2)])
                P.add("act", lambda e, tt=tt: e.activation(out=hnT[:, :, tt * 128:(tt + 1) * 128],
                                                           in_=pT2.rearrange("p (a b) -> p a b", a=8), func=AF.Copy),
                      reads=[("ps", 2)], writes=[("hnT", tt)])
            ub = 0
            for fg in range(8):
                wk = fg % 2
                dma("sp", wu_sb[wk], wu_bf[:, :, fg * 512:(fg + 1) * 512], [], [("wu", wk)], f"wu{wk}")
                for f4 in range(4):
                    fc = fg * 4 + f4
                    bank = 3 + (ub % 4)
                    rk = ub % 2
                    ub += 1
                    for kc in range(8):
                        P.add("pe", lambda e, wk=wk, f4=f4, kc=kc, bank=bank: e.matmul(
                            out=ps[bank][:], lhsT=wu_sb[wk][:, kc, f4 * 128:(f4 + 1) * 128], rhs=hnT[:, kc, :],
                            start=(kc == 0), stop=(kc == 7)),
                            reads=[("wu", wk)] + [("hnT", t4) for t4 in range(4)], writes=[("ps", bank)])
                    P.add("act", lambda e, bank=bank, rk=rk: e.activation(out=r32[rk], in_=ps[bank][:], func=AF.Relu),
                          reads=[("ps", bank)], writes=[("r32", rk)])
                    P.add("pool", lambda e, fc=fc, rk=rk: e.tensor_tensor(out=aT[:, fc, :], in0=r32[rk], in1=r32[rk],
                                                                          op=ALU.mult),
                          reads=[("r32", rk)], writes=[("aT", fc)])
            for dg in range(8):
                wk = dg % 2
                dma("sp", wd_sb[wk], wd_bf[:, dg * 4:(dg + 1) * 4, :], [], [("wd", wk)], f"wd{wk}")
                for tt in range(4):
                    for nh in range(2):
                        for f4 in range(4):
                            fc = dg * 4 + f4
                            P.add("pe", lambda e, wk=wk, tt=tt, nh=nh, f4=f4, fc=fc, dg=dg: e.matmul(
                                out=ps[2 * tt + nh][:], lhsT=aT[:, fc, tt * 128:(tt + 1) * 128],
                                rhs=wd_sb[wk][:, f4, nh * 512:(nh + 1) * 512],
                                start=(dg == 0 and f4 == 0), stop=(dg == 7 and f4 == 3)),
                                reads=[("wd", wk), ("aT", fc)], writes=[("ps", 2 * tt + nh)])
            for tt in range(4):
                i = 4 * c + tt
                k2 = i % 2
                r0 = s * L + i * 128
                for nh in range(2):
                    P.add("act", lambda e, nh=nh, k2=k2, tt=tt: e.activation(
                        out=junkF[:, nh * 512:(nh + 1) * 512], in_=ps[2 * tt + nh][:], func=AF.Square,
                        accum_out=ssc[k2][:, nh:nh + 1]),
                        reads=[("ps", 2 * tt + nh)], writes=[("ssc", k2, nh), ("junkF", nh)])
                P.add("dve", lambda e, k2=k2: e.tensor_tensor(out=ss3[k2], in0=ssc[k2][:, 0:1], in1=ssc[k2][:, 1:2],
                                                              op=ALU.add),
                      reads=[("ssc", k2, 0), ("ssc", k2, 1)], writes=[("ss3", k2)])
                P.add("act", lambda e, k2=k2: e.activation(out=sd3[k2], in_=ss3[k2], func=AF.Sqrt, scale=1.0 / 1024,
                                                           bias=pp[:, PP_EPS:PP_EPS + 1]),
                      reads=[("ss3", k2), "pp"], writes=[("sd3", k2)])
                P.add("dve", lambda e, k2=k2: e.reciprocal(out=r3[k2], in_=sd3[k2]), reads=[("sd3", k2)],
                      writes=[("r3", k2)])
                for nh in range(2):
                    P.add("dve", lambda e, nh=nh, k2=k2, tt=tt: e.scalar_tensor_tensor(
                        out=ot[k2][:, nh * 512:(nh + 1) * 512], in0=ps[2 * tt + nh][:], scalar=r3[k2],
                        in1=gpl[:, nh * 512:(nh + 1) * 512], op0=ALU.mult, op1=ALU.mult),
                        reads=[("ps", 2 * tt + nh), ("r3", k2), "gpl"], writes=[("ot", k2)])
                P.add("pool", lambda e, k2=k2, tt=tt: e.tensor_tensor(out=ot[k2], in0=ot[k2], in1=h1[:, tt, :], op=ALU.add),
                      reads=[("ot", k2), ("h1", tt)], writes=[("ot", k2)])
                dma("sp", out_d[r0:r0 + 128, :], ot[k2], [("ot", k2)], [("ot", k2)], f"ot{k2}")
        barrier()

    final_keys = ["dbg_" + n for n in dbg_out] + ["ot0", "ot1"]
    P.emit(es, final_wait_keys=final_keys)
    es.close()
    if dbg is not None:
        dbg.update(dbg_out)
    return nc


PP_GMIX = 0
PP_GMLP = 8
PP_EPS = 16
PP_LNW = 17
PP_LNB = 18
PP_CONVW = 19
PP_CONVB = 51
PP_N = 59
R_DTB = 0
R_ALOG = 8
R_DSKIP = 16
R_SSDNW = 24
ROWS_N = 536


def _host_inputs(inputs, core, nseq, L):
    f = lambda a: np.ascontiguousarray(np.asarray(a, dtype=np.float32))
    x = f(inputs["x"])
    xs = x[core * nseq:(core + 1) * nseq, :L].reshape(nseq * L, D_MODEL)
    w_in = f(inputs["w_in"])[0][:, _w_in_perm()]
    pp = np.zeros((128, PP_N), np.float32)
    pp[:, PP_GMIX:PP_GMIX + 8] = f(inputs["norm_pre_mix"])[0].reshape(8, 128).T
    pp[:, PP_GMLP:PP_GMLP + 8] = f(inputs["norm_pre_mlp"])[0].reshape(8, 128).T
    pp[:, PP_EPS] = EPS
    pp[:, PP_LNW] = np.tile(f(inputs["k_idx_ln_w"])[0], 2)
    pp[:, PP_LNB] = np.tile(f(inputs["k_idx_ln_b"])[0], 2)
    cw = f(inputs["conv_w"])[0]
    pp[:, PP_CONVW:PP_CONVW + 32] = cw.reshape(4, 8, 128).transpose(2, 1, 0).reshape(128, 32)
    pp[:, PP_CONVB:PP_CONVB + 8] = f(inputs["conv_b"])[0].reshape(8, 128).T
    rows = np.zeros((1, ROWS_N), np.float32)
    rows[0, R_DTB:R_DTB + 8] = f(inputs["dt_bias"])[0]
    rows[0, R_ALOG:R_ALOG + 8] = f(inputs["a_log"])[0]
    rows[0, R_DSKIP:R_DSKIP + 8] = f(inputs["d_skip"])[0]
    rows[0, R_SSDNW:R_SSDNW + 512] = f(inputs["ssd_norm_w"])[0]
    return {
        "x": np.ascontiguousarray(xs),
        "w_in": np.ascontiguousarray(w_in),
        "w_out": f(inputs["w_out"])[0],
        "w_up": f(inputs["w_mlp_up"])[0],
        "w_down": f(inputs["w_mlp_down"])[0],
        "consts": _consts(),
        "pp": pp,
        "rows": rows,
        "gpost": np.ascontiguousarray(np.stack([f(inputs["norm_post_mix"])[0], f(inputs["norm_post_mlp"])[0]], 0)),
        "convb": f(inputs["conv_b"])[0].reshape(1, 1024),
        "rel_bias": f(inputs["rel_bias"]),
    }


def kernel(**inputs):
    nseq, nt = 2, 16
    nc = build(nseq=nseq, nt=nt)
    in_maps = [_host_inputs(inputs, c, nseq, nt * 128) for c in range(N_CORES)]
    res = run_bass_kernel_spmd(nc, in_maps, core_ids=list(range(N_CORES)))
    outs = [np.asarray(r["out"]).reshape(nseq, nt * 128, D_MODEL) for r in res.results]
    return np.concatenate(outs, axis=0).astype(np.float32)
```

```python
import numpy as np
from contextlib import ExitStack
import concourse.bass as bass
import concourse.mybir as mybir
from concourse.bass_utils import run_bass_kernel_spmd

F32 = mybir.dt.float32
BF16 = mybir.dt.bfloat16
AF = mybir.ActivationFunctionType
ALU = mybir.AluOpType
AX = mybir.AxisListType

D_MODEL = 1024
L_FULL = 2048
N_CORES = 8
EPS = 1e-6
NEG_BIG = -1.0e30

OFF_Q, OFF_K, OFF_V, OFF_QI, OFF_KI, OFF_WI, OFF_Z, OFF_XBC, OFF_DT = (
    0, 512, 640, 768, 1024, 1088, 1092, 1604, 2628)
N_FM = 17
C_TM1 = N_FM * 128
C_Z = C_TM1 + 140
W_IN_COLS = C_Z + 512


def _w_in_perm():
    cols = []
    for p in range(4):
        g, j = p // 2, p % 2
        for h in (4 * g + j, 4 * g + 2 + j):
            cols += list(range(OFF_Q + 64 * h, OFF_Q + 64 * h + 64))
    for g in range(2):
        for _ in range(2):
            cols += list(range(OFF_K + 64 * g, OFF_K + 64 * g + 64))
    for p in range(2):
        for h in (2 * p, 2 * p + 1):
            cols += list(range(OFF_QI + 64 * h, OFF_QI + 64 * h + 64))
    for _ in range(2):
        cols += list(range(OFF_KI, OFF_KI + 64))
    cols += list(range(OFF_XBC, OFF_XBC + 1024))
    cols += list(range(OFF_V, OFF_V + 128))
    cols += list(range(OFF_WI, OFF_WI + 4))
    cols += list(range(OFF_DT, OFF_DT + 8))
    cols += list(range(OFF_Z, OFF_Z + 512))
    assert len(cols) == W_IN_COLS
    return np.array(cols, dtype=np.int64)


def _t5_bucket_np(d):
    d = np.asarray(d)
    max_exact = 16
    df = np.maximum(d, 1).astype(np.float32)
    large = max_exact + (np.log(df / max_exact) / np.float32(np.log(128 / max_exact))
                         * (32 - max_exact)).astype(np.int32)
    large = np.minimum(large, 31)
    return np.where(d < max_exact, d, large)


CO_IDENT = 0
CO_U = 128
CO_NEGTRI = 256
CO_NEGL = 384
CO_BD64 = 512
CO_PERT = 640
CO_OH = 640 + 2048
CO_ONES = CO_OH + 384
CO_NU = CO_ONES + 128
CO_N = CO_NU + 128


def _consts():
    c = np.zeros((128, CO_N), np.float32)
    idx = np.arange(128)
    c[:, CO_IDENT:CO_IDENT + 128] = np.eye(128, dtype=np.float32)
    c[:, CO_U:CO_U + 128] = (idx[:, None] <= idx[None, :]).astype(np.float32)
    c[:, CO_NEGTRI:CO_NEGTRI + 128] = np.where(idx[None, :] > idx[:, None], NEG_BIG, 0.0)
    c[:, CO_NEGL:CO_NEGL + 128] = np.where(idx[None, :] < idx[:, None], -30000.0, 0.0)
    bd = np.zeros((128, 128), np.float32)
    bd[:64, :64] = 1.0 / 64
    bd[64:, 64:] = 1.0 / 64
    c[:, CO_BD64:CO_BD64 + 128] = bd
    c[:, CO_PERT:CO_PERT + 2048] = (-(2.0 ** -23) * (np.arange(2048) + 1)).astype(np.float32)[None, :]
    m = np.arange(383)
    b = _t5_bucket_np(np.maximum(m - 127, 0))
    oh = np.zeros((32, 384), np.float32)
    oh[b, m] = 1.0
    c[:32, CO_OH:CO_OH + 384] = oh
    c[:, CO_ONES:CO_ONES + 128] = 1.0
    c[:, CO_NU:CO_NU + 128] = np.eye(128, dtype=np.float32) - bd
    return c


class _Op:
    __slots__ = ("eng", "fn", "deps", "flag", "is_dma", "key", "tick")

    def __init__(self, eng, fn, is_dma, key):
        self.eng = eng
        self.fn = fn
        self.deps = []
        self.flag = False
        self.is_dma = is_dma
        self.key = key
        self.tick = 0


class Prog:
    ENGS = ("pe", "act", "dve", "pool", "sp")
    EPOCH = 12000

    def __init__(self, nc):
        self.nc = nc
        self.q = {e: [] for e in self.ENGS}
        self.last_w = {}
        self.readers = {}
        self.dma_ops = []
        self.bg = {}
        self.bg_rate = {"dve": 6, "act": 3}

    def bg_push(self, eng, fn, reads=(), writes=()):
        self.bg.setdefault(eng, []).append((fn, reads, writes))

    def bg_flush(self, eng=None, n=None):
        for e in ([eng] if eng else list(self.bg.keys())):
            q = self.bg.get(e, [])
            k = 0
            while q and (n is None or k < n):
                fn, reads, writes = q.pop(0)
                self._add(e, fn, reads, writes, None)
                k += 1

    def add(self, eng, fn, reads=(), writes=(), dma_key=None):
        if self.bg.get(eng):
            self.bg_flush(eng, self.bg_rate.get(eng, 2))
        return self._add(eng, fn, reads, writes, dma_key)

    def _add(self, eng, fn, reads=(), writes=(), dma_key=None):
        is_dma = dma_key is not None
        op = _Op(eng, fn, is_dma, dma_key)
        cand = {}
        for t in reads:
            w = self.last_w.get(t)
            if w is not None:
                cand[id(w)] = (w, True)
        for t in writes:
            w = self.last_w.get(t)
            if w is not None and id(w) not in cand:
                cand[id(w)] = (w, False)
            for r in self.readers.get(t, {}).values():
                if id(r) not in cand:
                    cand[id(r)] = (r, False)
        for d, raw in cand.values():
            if d is op:
                continue
            keep = True
            if not d.is_dma and d.eng == eng:
                keep = (eng in ("act", "dve", "pool")) or (is_dma and eng != "sp")
            if keep:
                d.flag = True
                op.deps.append(d)
        rk = ("dma", id(op)) if is_dma else eng
        for t in reads:
            self.readers.setdefault(t, {})[rk] = op
        for t in writes:
            self.last_w[t] = op
            self.readers[t] = {}
        self.q[eng].append(op)
        if is_dma:
            self.dma_ops.append(op)
        return op

    def emit(self, es, final_wait_keys=()):
        nc = self.nc
        n_epochs = {}
        for e in self.ENGS:
            cnt = 0
            for op in self.q[e]:
                if op.is_dma:
                    continue
                if op.flag:
                    cnt += 1
                    op.tick = cnt
            n_epochs[e] = (cnt + self.EPOCH - 1) // self.EPOCH
        dma_cnt = {}
        for e in self.ENGS:
            for op in self.q[e]:
                if op.is_dma:
                    dma_cnt[op.key] = dma_cnt.get(op.key, 0) + 1
                    op.tick = dma_cnt[op.key] * 16
        sems = {}
        for e in self.ENGS:
            for k in range(n_epochs[e]):
                sems[(e, k)] = es.enter_context(nc.semaphore(f"s_{e}_{k}"))
        for k in dma_cnt:
            sems[("dma", k)] = es.enter_context(nc.semaphore(f"d_{k}"))

        def ev(op):
            if op.is_dma:
                return sems[("dma", op.key)], op.tick
            k = (op.tick - 1) // self.EPOCH
            return sems[(op.eng, k)], op.tick - k * self.EPOCH

        block = es.enter_context(nc.Block())

        def run(eng_name, eng):
            waited = {}
            for op in self.q[eng_name]:
                need = {}
                for d in op.deps:
                    s, v = ev(d)
                    if need.get(s.num, (None, 0))[1] < v:
                        need[s.num] = (s, v)
                for sn, (s, v) in need.items():
                    if waited.get(sn, 0) < v:
                        eng.wait_ge(s, v)
                        waited[sn] = v
                ins = op.fn(eng)
                if op.is_dma:
                    s, _ = ev(op)
                    ins.then_inc(s, 16)
                elif op.flag:
                    s, _ = ev(op)
                    ins.then_inc(s, 1)
            if eng_name == "sp":
                for k in final_wait_keys:
                    if k in dma_cnt:
                        eng.wait_ge(sems[("dma", k)], dma_cnt[k] * 16)

        @block.tensor
        def _(e):
            run("pe", e)

        @block.scalar
        def _(e):
            run("act", e)

        @block.vector
        def _(e):
            run("dve", e)

        @block.gpsimd
        def _(e):
            run("pool", e)

        @block.sync
        def _(e):
            run("sp", e)


I8 = mybir.dt.int8
_DT_SIZE = {F32: 4, BF16: 2, I8: 1}


def build(nseq=2, nt=16, dbg=None, stop_after=None, act_from=99, bis_from=4):
    ACT_FROM = act_from
    BIS_FROM = bis_from
    L = nt * 128
    assert nt % 4 == 0
    topk = min(256, L // 4)
    ntok = nseq * L
    nc = bass.Bass("TRN2", target_bir_lowering=False)
    es = ExitStack()
    P = Prog(nc)

    def dram_in(name, shape, dt=F32):
        return nc.dram_tensor(name, list(shape), dt, kind="ExternalInput").ap()

    x_d = dram_in("x", [ntok, D_MODEL])
    win_d = dram_in("w_in", [D_MODEL, W_IN_COLS])
    wout_d = dram_in("w_out", [1024, 1024])
    wup_d = dram_in("w_up", [1024, 4096])
    wdn_d = dram_in("w_down", [4096, 1024])
    consts_d = dram_in("consts", [128, CO_N])
    pp_d = dram_in("pp", [128, PP_N])
    rows_d = dram_in("rows", [1, ROWS_N])
    gpost_d = dram_in("gpost", [2, 1024])
    convb_d = dram_in("convb", [1, 1024])
    relb_d = dram_in("rel_bias", [32, 8])
    out_d = nc.dram_tensor("out", [ntok, D_MODEL], F32, kind="ExternalOutput").ap()

    wi_bf = nc.dram_tensor("wi_bf", [128, 8, W_IN_COLS], BF16).ap()
    wo_bf = nc.dram_tensor("wo_bf", [128, 8, 1024], BF16).ap()
    wu_bf = nc.dram_tensor("wu_bf", [128, 8, 4096], BF16).ap()
    wd_bf = nc.dram_tensor("wd_bf", [128, 32, 1024], BF16).ap()
    E_d = nc.dram_tensor("E_d", [8, 128 * 384], F32)

    AW = 53200
    arena = es.enter_context(nc.sbuf_tensor("arena", [128, AW], F32))
    cur = [0]

    def take(dt, shape):
        nfree = int(np.prod(shape[1:]))
        nbytes = (nfree * _DT_SIZE[dt] + 63) // 64 * 64
        off = cur[0]
        cur[0] += nbytes
        assert cur[0] <= AW * 4, f"arena overflow {cur[0]} > {AW * 4}"
        v = arena[:, off // 4:(off + nbytes) // 4]
        if dt != F32:
            v = v.bitcast(dt)
        v = v[:, 0:nfree]
        if len(shape) == 3:
            v = v.rearrange("p (a b) -> p a b", a=shape[1])
        elif len(shape) == 4:
            v = v.rearrange("p (a b c) -> p a b c", a=shape[1], b=shape[2])
        if shape[0] < 128:
            v = v[0:shape[0]]
        return v

    def flat(v):
        if len(v.shape) == 3:
            return v.rearrange("p a b -> p (a b)")
        if len(v.shape) == 4:
            return v.rearrange("p a b c -> p (a b c)")
        return v

    consts = take(F32, [128, CO_N])
    pp = take(F32, [128, PP_N])
    rows = take(F32, [128, ROWS_N])
    ident_bf = take(BF16, [128, 128])
    I4 = take(BF16, [128, 4, 128])
    negl4_bf = take(BF16, [128, 4, 128])
    row0 = take(BF16, [1, 1024 + 768 + 128])
    c8row = row0[0:1, 0:1024].rearrange("p (a b) -> p a b", a=8)
    convb_bf = row0[0:1, 1024:1792]
    ones_bf = row0[0:1, 1792:1920]
    B8 = take(BF16, [128, 2, 8, 128])
    Dg = take(BF16, [128, 8, 4, 128])
    A_b = take(F32, [128, 8])
    rb_sb = take(F32, [32, 8])
    r31 = take(F32, [1, 8])
    small = take(F32, [128, 176])

    def sm(a, n):
        return small[:, a:a + n]
    ss = [sm(0, 1), sm(1, 1)]
    sd = [sm(2, 1), sm(3, 1)]
    rstd = [sm(4, 1), sm(5, 1)]
    wI = [sm(8, 4), sm(12, 4)]
    dtraw = [sm(16, 8), sm(24, 8)]
    m8 = [sm(32, 8), sm(40, 8)]
    rec = sm(48, 8)
    dt_sb = sm(56, 8)
    a_sb = sm(64, 8)
    cst = sm(72, 16)
    ecs = sm(88, 8)
    dte = sm(96, 8)
    dtot = sm(104, 8)
    negcs = sm(112, 8)
    ssg = sm(120, 2)
    sdg = sm(122, 2)
    rsg = sm(124, 2)
    ssa = [sm(128, 2), sm(130, 2)]
    ssq = [sm(132, 1), sm(133, 1)]
    sq2 = [sm(134, 1), sm(135, 1)]
    r1 = [sm(136, 1), sm(137, 1)]
    ss2 = [sm(138, 1), sm(139, 1)]
    sd2 = [sm(140, 1), sm(141, 1)]
    r2 = [sm(142, 1), sm(143, 1)]
    ssc = [sm(144, 2), sm(146, 2)]
    ss3 = [sm(148, 1), sm(149, 1)]
    sd3 = [sm(150, 1), sm(151, 1)]
    r3 = [sm(152, 1), sm(153, 1)]
    negth = sm(160, 1)
    sgn = sm(161, 1)
    sg2 = sm(162, 1)
    thf = sm(163, 1)
    lnd = sm(164, 8)
    thb = sm(172, 1)
    cntb = sm(173, 1)
    ubis = sm(174, 1)

    mixT = take(BF16, [128, 8, L])
    xt = [take(F32, [128, 1024])]
    phase_base = cur[0]

    NST = 4
    stage32 = [take(F32, [128, 4096]) for _ in range(NST)]
    stage16 = [take(BF16, [128, 4096]) for _ in range(NST)]
    lhs_h = [take(F32, [32, 128]) for _ in range(2)]
    Rsb = [take(F32, [128, 384]) for _ in range(2)]
    Bt32 = take(F32, [128, 2, 8, 128])
    convb32 = take(F32, [1, 1024])
    cur[0] = phase_base
    w_in_sb = take(BF16, [128, 8, W_IN_COLS])
    kT = take(BF16, [128, 2, L])
    kiT = take(BF16, [128, L])
    v_aug = take(BF16, [128, nt, 2, 65])
    S2 = [take(F32, [128, L]) for _ in range(2)]
    negm = take(BF16, [128, L])
    junk8 = take(I8, [128, L])
    xb = [take(BF16, [128, 1024]) for _ in range(2)]
    uT = [take(BF16, [128, 8, 128]) for _ in range(2)]
    qTz = [take(BF16, [128, 2, 4, 128]) for _ in range(2)]
    qiT = [take(BF16, [128, 2, 128]) for _ in range(2)]
    kiraw = take(F32, [128, 128])
    kicen = take(F32, [128, 128])
    kisq = take(F32, [128, 128])
    kisd = take(F32, [128, 128])
    kirs = take(F32, [128, 128])
    xbcT = [take(BF16, [128, 8, 131]) for _ in range(2)]
    sz = [take(F32, [128, 512]) for _ in range(2)]
    rrelu = [take(F32, [128, 512]) for _ in range(2)]
    E_sb = [take(BF16, [128, 1024]) for _ in range(2)]
    attn_tok = take(BF16, [128, 512])
    xs_tok = take(F32, [128, 8, 64])
    B_tok = take(BF16, [128, 256])
    BCT = take(BF16, [128, 4, 128])
    rL = take(F32, [128, 8, 128])
    GT_sb = take(F32, [128, 2, 128])
    WT = take(BF16, [128, 8, 128])
    X_sb = take(BF16, [128, 8, 64])
    Xd_sb = take(BF16, [128, 8, 64])
    yoff_sb = take(F32, [128, 8, 64])
    t2_sb = take(F32, [128, 8, 64])
    y_sb = take(F32, [128, 512])
    ssd_tok = take(BF16, [128, 512])
    H_sb = take(F32, [128, 8, 64])
    Ht_sb = t2_sb
    Hbf = take(BF16, [128, 8, 64])
    endM = cur[0]

    cur[0] = phase_base
    xt.append(take(F32, [128, 1024]))
    wo_sb = take(BF16, [128, 8, 1024])
    gpm = take(F32, [128, 1024])
    gpl = take(F32, [128, 1024])
    h1 = take(F32, [128, 4, 1024])
    hn = [take(BF16, [128, 1024]) for _ in range(2)]
    hnT = take(BF16, [128, 8, 512])
    aT = take(BF16, [128, 32, 512])
    wu_sb = [take(BF16, [128, 8, 512]) for _ in range(2)]
    wd_sb = [take(BF16, [128, 4, 1024]) for _ in range(2)]
    r32 = [take(F32, [128, 512]) for _ in range(2)]
    ot = [take(F32, [128, 1024]) for _ in range(2)]
    junkF = take(BF16, [128, 1024])
    endF = cur[0]
    print(f"[build] arena: base={phase_base} endM={endM} endF={endF} cap={AW * 4}")

    ps = [es.enter_context(nc.psum_tensor(f"ps{i}", [128, 512], F32)) for i in range(8)]

    dbg_out = {}

    def dump(name, src_ap, shape, reads=()):
        if dbg is None:
            return
        t = nc.dram_tensor("dbg_" + name, list(shape), src_ap.dtype, kind="ExternalOutput").ap()
        dbg_out[name] = (list(shape), src_ap.dtype)
        P.add("sp", lambda e, t=t, s=src_ap: e.dma_start(out=t, in_=s), reads=reads,
              writes=[("dbg", name)], dma_key="dbg_" + name)
        P.dma_tokens.append(("dbg", name))

    bar_n = [0]

    def barrier():
        n = bar_n[0]
        bar_n[0] += 1
        P.add("pe", lambda e: e.matmul(out=ps[7][0:1, 0:2], lhsT=ones_bf[0:1, 0:1], rhs=ones_bf[0:1, 0:2],
                                       start=True, stop=True),
              reads=["ones_bf"], writes=[("ps", 7), ("bar", n, "pe")])
        P.add("act", lambda e: e.activation(out=small[:, 156:157], in_=small[:, 156:157], func=AF.Copy),
              writes=[("bar", n, "act")])
        P.add("dve", lambda e: e.tensor_copy(out=small[:, 157:158], in_=small[:, 157:158]),
              writes=[("bar", n, "dve")])
        P.add("pool", lambda e: e.tensor_copy(out=small[:, 158:159], in_=small[:, 158:159]),
              writes=[("bar", n, "pool")])
        toks = list(P.dma_tokens)
        P.dma_tokens = []
        P.add("sp", lambda e: e.nop(), reads=toks, writes=[("bar", n, "sp")])
        allb = [("bar", n, e) for e in ("pe", "act", "dve", "pool", "sp")]
        P.add("pe", lambda e: e.matmul(out=ps[7][0:1, 0:2], lhsT=ones_bf[0:1, 0:1], rhs=ones_bf[0:1, 0:2],
                                       start=True, stop=True), reads=allb + ["ones_bf"], writes=[("ps", 7)])
        P.add("act", lambda e: e.activation(out=small[:, 156:157], in_=small[:, 156:157], func=AF.Copy), reads=allb)
        P.add("dve", lambda e: e.tensor_copy(out=small[:, 157:158], in_=small[:, 157:158]), reads=allb)
        P.add("pool", lambda e: e.tensor_copy(out=small[:, 158:159], in_=small[:, 158:159]), reads=allb)
        P.add("sp", lambda e: e.nop(), reads=allb)

    def dma(eng, out, in_, reads, writes, key):
        P.add(eng, lambda e: e.dma_start(out=out, in_=in_), reads=reads, writes=writes, dma_key=key)
        P.dma_tokens.extend(writes)

    P.dma_tokens = []

    dma("sp", consts, consts_d, [], ["consts"], "c0")
    dma("sp", pp, pp_d, [], ["pp"], "c1")
    dma("sp", rows, rows_d.partition_broadcast(128), [], ["rows"], "c2")
    dma("sp", rb_sb, relb_d, [], ["rb_sb"], "c3")
    dma("sp", r31, relb_d[31:32, :], [], ["r31"], "c4")
    dma("sp", convb32, convb_d, [], ["convb32"], "c5")
    P.add("dve", lambda e: e.memset(small, 0.0), writes=["small0"])
    P.add("dve", lambda e: e.tensor_copy(out=ident_bf, in_=consts[:, CO_IDENT:CO_IDENT + 128]),
          reads=["consts"], writes=["ident_bf"])
    P.add("dve", lambda e: e.tensor_copy(out=ones_bf, in_=consts[0:1, CO_ONES:CO_ONES + 128]),
          reads=["consts"], writes=["ones_bf"])
    P.add("dve", lambda e: e.tensor_copy(out=convb_bf, in_=convb32[0:1, 0:768]),
          reads=["convb32"], writes=["convb_bf"])
    for r4 in range(4):
        P.add("dve", lambda e, r4=r4: e.tensor_copy(out=I4[:, r4, :], in_=consts[:, CO_IDENT:CO_IDENT + 128]),
              reads=["consts"], writes=["I4"])
        P.add("dve", lambda e, r4=r4: e.tensor_copy(out=negl4_bf[:, r4, :], in_=consts[:, CO_NEGL:CO_NEGL + 128]),
              reads=["consts"], writes=["negl4"])
    for h in range(8):
        k = h % 2
        P.add("dve", lambda e, h=h, k=k: e.tensor_scalar(out=lhs_h[k], in0=consts[0:32, CO_ONES:CO_ONES + 128],
                                                         scalar1=rb_sb[:, h:h + 1], scalar2=None, op0=ALU.mult),
              reads=["consts", "rb_sb"], writes=[("lhs_h", k)])
        P.add("pe", lambda e, k=k: e.matmul(out=ps[k][:, 0:384], lhsT=lhs_h[k],
                                            rhs=consts[0:32, CO_OH:CO_OH + 384], start=True, stop=True),
              reads=[("lhs_h", k), "consts"], writes=[("ps", k)])
        P.add("act", lambda e, k=k: e.activation(out=Rsb[k], in_=ps[k][:, 0:384], func=AF.Copy),
              reads=[("ps", k)], writes=[("Rsb", k)])
        dma("sp", E_d.ap()[h, :].rearrange("(p m) -> p m", p=128), Rsb[k], [("Rsb", k)], [("E_d", h)], f"Rsb{k}")
        for dl in range(2):
            src = bass.AP(E_d, h * 128 * 384 + 127 + 128 * dl, [[383, 128], [1, 128]])
            dma("sp", Bt32[:, dl, h, :], src, [("E_d", h)], [("Bt32", dl, h)], "Bt32")
        P.add("dve", lambda e, h=h: e.tensor_scalar(out=c8row[0:1, h, :], in0=consts[0:1, CO_ONES:CO_ONES + 128],
                                                    scalar1=r31[0:1, h:h + 1], scalar2=8.0, op0=ALU.mult, op1=ALU.mult),
              reads=["consts", "r31"], writes=["c8row"])
    P.add("dve", lambda e: e.tensor_scalar(out=flat(B8), in0=flat(Bt32), scalar1=8.0, scalar2=None, op0=ALU.mult),
          reads=[("Bt32", dl, h) for dl in range(2) for h in range(8)], writes=["B8"])
    for cc in range(8):
        for k in range(4):
            P.add("dve", lambda e, cc=cc, k=k: e.tensor_scalar(
                out=Dg[:, cc, k, :], in0=consts[:, CO_IDENT:CO_IDENT + 128],
                scalar1=pp[:, PP_CONVW + cc * 4 + k:PP_CONVW + cc * 4 + k + 1], scalar2=None, op0=ALU.mult),
                reads=["consts", "pp"], writes=["Dg"])
    P.add("act", lambda e: e.activation(out=A_b, in_=rows[:, R_ALOG:R_ALOG + 8], func=AF.Exp),
          reads=["rows"], writes=["A_b"])
    P.add("dve", lambda e: e.tensor_scalar(out=A_b, in0=A_b, scalar1=-1.0, scalar2=None, op0=ALU.mult),
          reads=["A_b"], writes=["A_b"])

    win_v = win_d.rearrange("(kc p) n -> p kc n", p=128)
    wout_v = wout_d.rearrange("(cc p) n -> p cc n", p=128)
    wup_v = wup_d.rearrange("(kc p) n -> p kc n", p=128)
    wdn_v = wdn_d.rearrange("(fc p) n -> p fc n", p=128)
    chunks = []
    for kc in range(8):
        chunks.append((win_v[:, kc, :], wi_bf[:, kc, :], [W_IN_COLS], pp[:, PP_GMIX + kc:PP_GMIX + kc + 1], "act"))
    for c2 in range(2):
        chunks.append((wout_v[:, 4 * c2:4 * c2 + 4, :], wo_bf[:, 4 * c2:4 * c2 + 4, :], [4, 1024], None, "pool"))
    for kc in range(8):
        chunks.append((wup_v[:, kc, :], wu_bf[:, kc, :], [4096], pp[:, PP_GMLP + kc:PP_GMLP + kc + 1], "act"))
        chunks.append((wdn_v[:, 4 * kc:4 * kc + 4, :], wd_bf[:, 4 * kc:4 * kc + 4, :], [4, 1024], None,
                       "dve" if kc % 2 else "pool"))

    def stage_views(idx):
        src, dst, shp, sc, ce = chunks[idx]
        k = idx % NST
        nfree = int(np.prod(shp))
        s32 = stage32[k][:, 0:nfree]
        s16 = stage16[k][:, 0:nfree]
        if len(shp) == 2:
            return k, s32, s16, s32.rearrange("p (a b) -> p a b", a=shp[0]), s16.rearrange("p (a b) -> p a b", a=shp[0])
        return k, s32, s16, s32, s16

    def prep_load(idx):
        k, s32, s16, s32v, s16v = stage_views(idx)
        dma("sp", s32v, chunks[idx][0], [], [("st32", k)], f"st32_{k}")

    for idx in range(min(NST, len(chunks))):
        prep_load(idx)
    for idx in range(len(chunks)):
        src, dst, shp, sc, ce = chunks[idx]
        k, s32, s16, s32v, s16v = stage_views(idx)
        if sc is not None:
            P.add("act", lambda e, s16=s16, s32=s32, sc=sc: e.activation(out=s16, in_=s32, func=AF.Copy, scale=sc),
                  reads=[("st32", k), "pp"], writes=[("st16", k)])
        else:
            P.add(ce, lambda e, s16=s16, s32=s32: e.tensor_copy(out=s16, in_=s32), reads=[("st32", k)],
                  writes=[("st16", k)])
        dma("pool", dst, s16v, [("st16", k)], [("wscr", idx)], f"st16_{k}")
        if idx + NST < len(chunks):
            prep_load(idx + NST)
    barrier()

    for s in range(nseq):
        dma("sp", w_in_sb, wi_bf, [], ["w_in_sb"], "w_in_sb")
        P.add("pool", lambda e: e.memset(v_aug[:, :, :, 64:65], 1.0), writes=["v_ones"])
        for kq in range(2):
            P.add("pool", lambda e, kq=kq: e.memset(flat(qTz[kq]), 0.0), writes=[("qT", kq)])
        P.add("pool", lambda e: e.memset(flat(H_sb), 0.0), writes=["H"])
        P.add("pool", lambda e: e.memset(flat(Hbf), 0.0), writes=["Hbf"])
        pend_attn = []
        finalizers = []
        for i in range(nt):
            k2 = i % 2
            r0 = s * L + i * 128
            dma("sp", xt[0], x_d[r0:r0 + 128, :], [], [("xt", 0)], "xt0")
            P.add("act", lambda e, k2=k2: e.activation(out=xb[k2], in_=xt[0], func=AF.Square, accum_out=ss[k2]),
                  reads=[("xt", 0)], writes=[("ss", k2), ("xb", k2)])
            P.add("act", lambda e, k2=k2: e.activation(out=sd[k2], in_=ss[k2], func=AF.Ln, scale=1.0 / 1024,
                                                       bias=pp[:, PP_EPS:PP_EPS + 1]),
                  reads=[("ss", k2), "pp"], writes=[("sd", k2)])
            P.add("act", lambda e, k2=k2: e.activation(out=rstd[k2], in_=sd[k2], func=AF.Exp, scale=-0.5),
                  reads=[("sd", k2)], writes=[("rstd", k2)])
            P.add("act", lambda e, k2=k2: e.activation(out=xb[k2], in_=xt[0], func=AF.Copy, scale=rstd[k2]),
                  reads=[("xt", 0), ("rstd", k2)], writes=[("xb", k2)])
            pT = ps[0][:].bitcast(BF16)
            for kc in range(8):
                P.add("pe", lambda e, k2=k2, kc=kc: e.transpose(out=pT[:, kc * 128:(kc + 1) * 128],
                                                                 in_=xb[k2][:, kc * 128:(kc + 1) * 128],
                                                                 identity=ident_bf),
                      reads=[("xb", k2), "ident_bf"], writes=[("ps", 0)])
            P.add("act", lambda e, k2=k2: e.activation(out=flat(uT[k2]), in_=pT, func=AF.Copy),
                  reads=[("ps", 0)], writes=[("uT", k2)])

            def fm_group(bank, slot, g, k2=k2):
                for kc in range(8):
                    P.add("pe", lambda e, kc=kc: e.matmul(out=ps[bank][:, slot * 128:(slot + 1) * 128],
                                                          lhsT=w_in_sb[:, kc, g * 128:(g + 1) * 128],
                                                          rhs=uT[k2][:, kc, :], start=(kc == 0), stop=(kc == 7)),
                          reads=["w_in_sb", ("uT", k2)], writes=[("ps", bank)])
            for g in range(4):
                fm_group(1, g, g)
            for half in range(2):
                P.add("act", lambda e, k2=k2, half=half: e.activation(
                    out=qTz[k2][half * 64:(half + 1) * 64, half, :, :],
                    in_=ps[1][half * 64:(half + 1) * 64, :].rearrange("p (a b) -> p a b", a=4), func=AF.Copy),
                    reads=[("ps", 1)], writes=[("qT", k2)])
            for g in range(4):
                fm_group(2, g, 4 + g)
            P.add("act", lambda e, i=i: e.activation(out=kT[:, :, i * 128:(i + 1) * 128],
                                                     in_=ps[2][:, 0:256].rearrange("p (a b) -> p a b", a=2),
                                                     func=AF.Copy),
                  reads=[("ps", 2)], writes=[("kT", i)])
            P.add("act", lambda e, k2=k2: e.activation(out=flat(qiT[k2]), in_=ps[2][:, 256:512], func=AF.Copy),
                  reads=[("ps", 2)], writes=[("qiT", k2)])
            fm_group(3, 0, 8)
            P.add("act", lambda e: e.activation(out=kiraw, in_=ps[3][:, 0:128], func=AF.Copy),
                  reads=[("ps", 3)], writes=["kiraw"])
            bd = consts[:, CO_BD64:CO_BD64 + 128]
            imbd = consts[:, CO_NU:CO_NU + 128]
            P.add("pe", lambda e: e.matmul(out=ps[3][:, 128:256], lhsT=imbd, rhs=kiraw, start=True, stop=True),
                  reads=["consts", "kiraw"], writes=[("ps", 3)])
            P.add("act", lambda e: e.activation(out=kisq, in_=ps[3][:, 128:256], func=AF.Square),
                  reads=[("ps", 3)], writes=["kisq"])
            P.add("act", lambda e: e.activation(out=kicen, in_=ps[3][:, 128:256], func=AF.Copy),
                  reads=[("ps", 3)], writes=["kicen"])
            P.add("pe", lambda e: e.matmul(out=ps[3][:, 256:384], lhsT=bd, rhs=kisq, start=True, stop=True),
                  reads=["consts", "kisq"], writes=[("ps", 3)])
            P.add("act", lambda e: e.activation(out=kisd, in_=ps[3][:, 256:384], func=AF.Ln,
                                                bias=pp[:, PP_EPS:PP_EPS + 1]),
                  reads=[("ps", 3), "pp"], writes=["kisd"])
            P.add("act", lambda e: e.activation(out=kirs, in_=kisd, func=AF.Exp, scale=-0.5),
                  reads=["kisd"], writes=["kirs"])
            P.add("pool", lambda e: e.tensor_tensor(out=kicen, in0=kicen, in1=kirs, op=ALU.mult),
                  reads=["kicen", "kirs"], writes=["kicen"])
            P.add("act", lambda e, i=i: e.activation(out=kiT[:, i * 128:(i + 1) * 128], in_=kicen,
                                                     func=AF.Identity, scale=pp[:, PP_LNW:PP_LNW + 1],
                                                     bias=pp[:, PP_LNB:PP_LNB + 1]),
                  reads=["kicen", "pp"], writes=[("kiT", i)])
            for cc in range(8):
                fm_group(4 + cc // 4, cc % 4, 9 + cc)
            if i == 0:
                P.add("pool", lambda e, k2=k2: e.memset(xbcT[k2][:, :, 0:3], 0.0), writes=[("xbcT", k2)])
            else:
                P.add("pool", lambda e, k2=k2: e.tensor_copy(out=xbcT[k2][:, :, 0:3], in_=xbcT[1 - k2][:, :, 128:131]),
                      reads=[("xbcT", 1 - k2)], writes=[("xbcT", k2)])
            for hb in range(2):
                P.add("act", lambda e, k2=k2, hb=hb: e.activation(
                    out=xbcT[k2][:, 4 * hb:4 * hb + 4, 3:131],
                    in_=ps[4 + hb][:].rearrange("p (a b) -> p a b", a=4), func=AF.Copy),
                    reads=[("ps", 4 + hb)], writes=[("xbcT", k2)])
            for kc in range(8):
                P.add("pe", lambda e, kc=kc, k2=k2: e.matmul(out=ps[6][:, 0:140], lhsT=uT[k2][:, kc, :],
                                                             rhs=w_in_sb[:, kc, C_TM1:C_TM1 + 140],
                                                             start=(kc == 0), stop=(kc == 7)),
                      reads=["w_in_sb", ("uT", k2)], writes=[("ps", 6)])
            for kc in range(8):
                P.add("pe", lambda e, kc=kc, k2=k2: e.matmul(out=ps[7][:], lhsT=uT[k2][:, kc, :],
                                                             rhs=w_in_sb[:, kc, C_Z:C_Z + 512],
                                                             start=(kc == 0), stop=(kc == 7)),
                      reads=["w_in_sb", ("uT", k2)], writes=[("ps", 7)])
            P.add("act", lambda e, i=i: e.activation(out=v_aug[:, i, :, 0:64],
                                                     in_=ps[6][:, 0:128].rearrange("p (a b) -> p a b", a=2),
                                                     func=AF.Copy),
                  reads=[("ps", 6)], writes=[("v", i)])
            P.add("act", lambda e, k2=k2: e.activation(out=wI[k2], in_=ps[6][:, 128:132], func=AF.Copy, scale=1.0 / 16),
                  reads=[("ps", 6)], writes=[("wI", k2)])
            P.add("act", lambda e, k2=k2: e.activation(out=dtraw[k2], in_=ps[6][:, 132:140], func=AF.Copy),
                  reads=[("ps", 6)], writes=[("dtraw", k2)])
            P.add("act", lambda e, k2=k2: e.activation(out=sz[k2], in_=ps[7][:], func=AF.Silu),
                  reads=[("ps", 7)], writes=[("sz", k2)])

            n = (i + 1) * 128
            Sb = S2[k2]
            cidx = 0
            nchunk = (n + 511) // 512
            for c in range(nchunk):
                wd = min(512, n - c * 512)
                for h in range(4):
                    pair, half = h // 2, h % 2
                    bk = cidx % 2
                    cidx += 1
                    P.add("pe", lambda e, k2=k2, pair=pair, half=half, bk=bk, c=c, wd=wd: e.matmul(
                        out=ps[bk][:, 0:wd], lhsT=qiT[k2][half * 64:(half + 1) * 64, pair, :],
                        rhs=kiT[half * 64:(half + 1) * 64, c * 512:c * 512 + wd], start=True, stop=True),
                        reads=[("qiT", k2)] + [("kiT", jj) for jj in range(4 * c, min(4 * c + 4, i + 1))],
                        writes=[("ps", bk)])
                    P.add("act", lambda e, bk=bk, wd=wd: e.activation(out=rrelu[bk][:, 0:wd], in_=ps[bk][:, 0:wd],
                                                                       func=AF.Relu),
                          reads=[("ps", bk)], writes=[("rrelu", bk)])
                    prev = (consts[:, CO_PERT + c * 512:CO_PERT + c * 512 + wd] if h == 0
                            else Sb[:, c * 512:c * 512 + wd])
                    P.add("dve", lambda e, k2=k2, h=h, bk=bk, c=c, wd=wd, prev=prev, Sb=Sb: e.scalar_tensor_tensor(
                        out=Sb[:, c * 512:c * 512 + wd], in0=rrelu[bk][:, 0:wd], scalar=wI[k2][:, h:h + 1],
                        in1=prev, op0=ALU.mult, op1=ALU.add),
                        reads=[("rrelu", bk), ("wI", k2), "consts", ("S", k2, c)], writes=[("S", k2, c)])
            cl = i // 4
            P.add("pool", lambda e, i=i, Sb=Sb: e.tensor_tensor(out=Sb[:, i * 128:(i + 1) * 128],
                                                                in0=Sb[:, i * 128:(i + 1) * 128],
                                                                in1=consts[:, CO_NEGTRI:CO_NEGTRI + 128], op=ALU.add),
                  reads=[("S", k2, cl), "consts"], writes=[("S", k2, cl)])
            allS = [("S", k2, c) for c in range(nchunk)]

            def queue_topk(i=i, n=n, Sb=Sb, allS=allS):
                if n > topk and i >= ACT_FROM:
                    K = 28
                    for k in range(K):
                        dk = 8.0 / (2 ** k)
                        if k == 0:
                            P.bg_push("act", lambda e: e.activation(out=junk8[:, 0:n], in_=Sb[:, 0:n], func=AF.Sign,
                                                                    accum_out=sgn),
                                      reads=allS, writes=["sgn", "junk8"])
                        else:
                            P.bg_push("act", lambda e: e.activation(out=junk8[:, 0:n], in_=Sb[:, 0:n], func=AF.Sign,
                                                                    bias=negth, accum_out=sgn),
                                      reads=allS + ["negth"], writes=["sgn", "junk8"])
                        P.bg_push("act", lambda e: e.activation(out=sg2, in_=sgn, func=AF.Sign,
                                                                bias=float(n - (2 * topk - 1))),
                                  reads=["sgn"], writes=["sg2"])
                        if k == 0:
                            P.bg_push("act", lambda e, dk=dk: e.activation(out=negth, in_=sg2, func=AF.Identity,
                                                                          scale=-dk / 2),
                                      reads=["sg2"], writes=["negth"])
                        else:
                            P.bg_push("act", lambda e, dk=dk: e.activation(out=negth, in_=sg2, func=AF.Identity,
                                                                          scale=-dk / 2, bias=negth),
                                      reads=["sg2", "negth"], writes=["negth"])
                    dK = 8.0 / (2 ** K)
                    P.bg_push("act", lambda e: e.activation(out=thf, in_=negth, func=AF.Identity, scale=-1.0, bias=-dK),
                              reads=["negth"], writes=["thf"])
                    finalizers.append(lambda: P.add("dve", lambda e: e.tensor_scalar(
                        out=negm[:, 0:n], in0=Sb[:, 0:n], scalar1=thf, scalar2=-30000.0,
                        op0=ALU.is_lt, op1=ALU.mult),
                        reads=allS + ["thf"], writes=["negm"]))
                elif n > topk and i >= BIS_FROM:
                    K = 27
                    P.add("dve", lambda e: e.memset(thb, 0.0), writes=["thb"])
                    for k in range(K):
                        dk = 4.0 / (2 ** k)
                        P.add("dve", lambda e: e.tensor_scalar(out=junk8[:, 0:n], in0=Sb[:, 0:n], scalar1=thb, scalar2=None,
                                                               op0=ALU.is_ge, op1=ALU.add, accum_out=cntb),
                              reads=allS + ["thb"], writes=["cntb", "junk8"])
                        P.add("dve", lambda e, dk=dk: e.tensor_scalar(out=ubis, in0=cntb, scalar1=float(topk) - 0.5, scalar2=dk,
                                                                      op0=ALU.is_ge, op1=ALU.mult),
                              reads=["cntb"], writes=["ub"])
                        P.add("dve", lambda e, dk=dk: e.scalar_tensor_tensor(out=thb, in0=thb, scalar=-dk / 2, in1=ubis,
                                                                             op0=ALU.add, op1=ALU.add),
                              reads=["thb", "ub"], writes=["thb"])
                    dK = 4.0 / (2 ** K)
                    P.add("dve", lambda e: e.scalar_tensor_tensor(out=ubis, in0=thb, scalar=-1.0, in1=thb,
                                                                  op0=ALU.mult, op1=ALU.max),
                          reads=["thb"], writes=["ub"])
                    P.add("dve", lambda e: e.tensor_scalar(out=ubis, in0=ubis, scalar1=2.4e-7, scalar2=None, op0=ALU.mult),
                          reads=["ub"], writes=["ub"])
                    P.add("dve", lambda e: e.scalar_tensor_tensor(out=thf, in0=thb, scalar=-dK, in1=ubis,
                                                                  op0=ALU.add, op1=ALU.subtract),
                          reads=["thb", "ub"], writes=["thf"])
                    finalizers.append(lambda: P.add("dve", lambda e: e.tensor_scalar(
                        out=negm[:, 0:n], in0=Sb[:, 0:n], scalar1=thf, scalar2=-30000.0,
                        op0=ALU.is_lt, op1=ALU.mult),
                        reads=allS + ["thf"], writes=["negm"]))
                elif n > topk:
                    for r in range(topk // 8):
                        P.add("dve", lambda e, r=r: e.max(out=m8[r % 2], in_=Sb[:, 0:n]),
                              reads=allS, writes=[("m8", r % 2)])
                        P.add("dve", lambda e, r=r: e.match_replace(
                            out=Sb[:, 0:n], in_to_replace=m8[r % 2], in_values=Sb[:, 0:n], imm_value=-3.0e38),
                            reads=[("m8", r % 2)] + allS, writes=allS)
                    finalizers.append(lambda: P.add("dve", lambda e: e.tensor_scalar(
                        out=negm[:, 0:n], in0=Sb[:, 0:n], scalar1=-1.0e38, scalar2=-30000.0,
                        op0=ALU.is_gt, op1=ALU.mult),
                        reads=allS, writes=["negm"]))
                else:
                    finalizers.append(lambda: P.add("dve", lambda e: e.tensor_scalar(
                        out=negm[:, 0:n], in0=Sb[:, 0:n], scalar1=-1.0e29, scalar2=-30000.0,
                        op0=ALU.is_lt, op1=ALU.mult),
                        reads=allS, writes=["negm"]))

            def attention(i=i, k2=k2):
                for j in range(i + 1):
                    ek = j % 2
                    for g in range(2):
                        bank = 2 + g
                        for half in range(2):
                            P.add("pe", lambda e, g=g, half=half, j=j, bank=bank: e.matmul(
                                out=ps[bank][:, half * 256:(half + 1) * 256],
                                lhsT=kT[:, g, j * 128:(j + 1) * 128],
                                rhs=qTz[k2][:, half, 2 * g:2 * g + 2, :],
                                start=(half == 0), stop=False),
                                reads=[("kT", j), ("qT", k2)], writes=[("ps", bank)])
                        P.add("pe", lambda e, j=j, bank=bank: e.matmul(
                            out=ps[bank][:], lhsT=negm[:, j * 128:(j + 1) * 128], rhs=flat(I4), start=False, stop=False),
                            reads=["negm", "I4"], writes=[("ps", bank)])
                        if i - j <= 1:
                            P.add("pe", lambda e, g=g, dl=i - j, bank=bank: e.matmul(
                                out=ps[bank][:], lhsT=ident_bf, rhs=B8[:, dl, 4 * g:4 * g + 4, :], start=False, stop=True),
                                reads=["ident_bf", "B8"], writes=[("ps", bank)])
                        else:
                            P.add("pe", lambda e, g=g, bank=bank: e.matmul(
                                out=ps[bank][:], lhsT=ones_bf[0:1, :], rhs=c8row[0:1, 4 * g:4 * g + 4, :],
                                start=False, stop=True),
                                reads=["ones_bf", "c8row"], writes=[("ps", bank)])
                        P.add("act", lambda e, g=g, ek=ek, bank=bank: e.activation(
                            out=E_sb[ek][:, g * 512:(g + 1) * 512], in_=ps[bank][:], func=AF.Exp, scale=0.125),
                            reads=[("ps", bank)], writes=[("E", ek, g)])
                    for h in range(8):
                        bpv = 4 + h // 4
                        hh = h % 4
                        P.add("pe", lambda e, h=h, hh=hh, bpv=bpv, ek=ek, j=j: e.matmul(
                            out=ps[bpv][:, hh * 65:hh * 65 + 65], lhsT=E_sb[ek][:, h * 128:(h + 1) * 128],
                            rhs=v_aug[:, j, h // 4, :], start=(j == 0 and hh == 0), stop=(j == i), skip_group_check=True),
                            reads=[("E", ek, h // 4), ("v", j), "v_ones"], writes=[("ps", bpv)])
                for b2 in range(2):
                    psv = ps[4 + b2][:, 0:260].rearrange("p (h c) -> p h c", c=65)
                    P.add("act", lambda e, b2=b2, psv=psv: e.activation(out=lnd[:, 4 * b2:4 * b2 + 4], in_=psv[:, :, 64],
                                                                       func=AF.Ln),
                          reads=[("ps", 4 + b2)], writes=[("lnd", b2)])
                    P.add("act", lambda e, b2=b2: e.activation(out=rec[:, 4 * b2:4 * b2 + 4], in_=lnd[:, 4 * b2:4 * b2 + 4],
                                                               func=AF.Exp, scale=-1.0),
                          reads=[("lnd", b2)], writes=[("rec", b2)])
                    for hh in range(4):
                        h = 4 * b2 + hh
                        P.add("act", lambda e, h=h, hh=hh, psv=psv: e.activation(
                            out=attn_tok[:, h * 64:(h + 1) * 64], in_=psv[:, hh, 0:64], func=AF.Copy,
                            scale=rec[:, h:h + 1]),
                            reads=[("ps", 4 + b2), ("rec", b2)], writes=["attn_tok"])
                pT6 = ps[6][:].bitcast(BF16)
                for c4 in range(4):
                    P.add("pe", lambda e, c4=c4: e.transpose(out=pT6[:, c4 * 128:(c4 + 1) * 128],
                                                             in_=attn_tok[:, c4 * 128:(c4 + 1) * 128], identity=ident_bf),
                          reads=["attn_tok", "ident_bf"], writes=[("ps", 6)])
                P.add("act", lambda e: e.activation(out=mixT[:, 0:4, i * 128:(i + 1) * 128],
                                                    in_=pT6[:, 0:512].rearrange("p (a b) -> p a b", a=4),
                                                    func=AF.Copy),
                      reads=[("ps", 6)], writes=[("mixT_a", i)])

            pend_attn.append(attention)
            queue_topk()

            xk = xbcT[k2]
            for cc in range(6):
                bank = 0 if cc < 4 else 1
                col = (cc % 4) * 128
                for k in range(4):
                    P.add("pe", lambda e, cc=cc, k=k, bank=bank, col=col, xk=xk: e.matmul(
                        out=ps[bank][:, col:col + 128], lhsT=xk[:, cc, k:k + 128], rhs=Dg[:, cc, k, :],
                        start=(k == 0), stop=False),
                        reads=[("xbcT", k2), "Dg"], writes=[("ps", bank)])
                P.add("pe", lambda e, cc=cc, bank=bank, col=col: e.matmul(
                    out=ps[bank][:, col:col + 128], lhsT=ones_bf[0:1, :], rhs=convb_bf[0:1, cc * 128:(cc + 1) * 128],
                    start=False, stop=True),
                    reads=["ones_bf", "convb_bf"], writes=[("ps", bank)])
            P.add("act", lambda e: e.activation(out=flat(xs_tok), in_=ps[0][:], func=AF.Silu),
                  reads=[("ps", 0)], writes=["xs_tok"])
            P.add("act", lambda e: e.activation(out=B_tok, in_=ps[1][:, 0:256], func=AF.Silu),
                  reads=[("ps", 1)], writes=["B_tok"])
            for c4 in range(4):
                cc = 4 + c4
                for k in range(4):
                    P.add("pe", lambda e, cc=cc, c4=c4, k=k, xk=xk: e.matmul(
                        out=ps[2][:, c4 * 128:(c4 + 1) * 128], lhsT=Dg[:, cc, k, :], rhs=xk[:, cc, k:k + 128],
                        start=(k == 0), stop=(k == 3)),
                        reads=[("xbcT", k2), "Dg"], writes=[("ps", 2)])
                P.add("act", lambda e, cc=cc, c4=c4: e.activation(
                    out=BCT[:, c4, :], in_=ps[2][:, c4 * 128:(c4 + 1) * 128], func=AF.Silu,
                    bias=pp[:, PP_CONVB + cc:PP_CONVB + cc + 1]),
                    reads=[("ps", 2), "pp"], writes=["BCT"])
            P.add("pool", lambda e, k2=k2: e.tensor_tensor(out=dt_sb, in0=dtraw[k2], in1=rows[:, R_DTB:R_DTB + 8],
                                                           op=ALU.add),
                  reads=[("dtraw", k2), "rows"], writes=["dt_sb"])
            P.add("act", lambda e: e.activation(out=dt_sb, in_=dt_sb, func=AF.Exp), reads=["dt_sb"], writes=["dt_sb"])
            P.add("act", lambda e: e.activation(out=dt_sb, in_=dt_sb, func=AF.Ln, bias=1.0),
                  reads=["dt_sb"], writes=["dt_sb"])
            P.add("pool", lambda e: e.tensor_tensor(out=a_sb, in0=dt_sb, in1=A_b, op=ALU.mult),
                  reads=["dt_sb", "A_b"], writes=["a_sb"])
            for g in range(2):
                P.add("pe", lambda e, g=g: e.matmul(out=ps[3][:, g * 128:(g + 1) * 128], lhsT=BCT[:, g, :],
                                                    rhs=BCT[:, 2 + g, :], start=True, stop=True),
                      reads=["BCT"], writes=[("ps", 3)])
            P.add("pe", lambda e: e.matmul(out=ps[3][:, 256:264], lhsT=consts[:, CO_U:CO_U + 128], rhs=a_sb,
                                           start=True, stop=True),
                  reads=["consts", "a_sb"], writes=[("ps", 3)])
            P.add("pe", lambda e: e.matmul(out=ps[3][:, 264:272], lhsT=consts[:, CO_ONES:CO_ONES + 128], rhs=a_sb,
                                           start=True, stop=True),
                  reads=["consts", "a_sb"], writes=[("ps", 3)])
            P.add("act", lambda e: e.activation(out=flat(GT_sb), in_=ps[3][:, 0:256], func=AF.Copy),
                  reads=[("ps", 3)], writes=["GT_sb"])
            P.add("act", lambda e: e.activation(out=cst, in_=ps[3][:, 256:272], func=AF.Copy),
                  reads=[("ps", 3)], writes=["cst"])
            P.add("act", lambda e: e.activation(out=ecs, in_=cst[:, 0:8], func=AF.Exp), reads=["cst"], writes=["ecs"])
            P.add("act", lambda e: e.activation(out=dtot, in_=cst[:, 8:16], func=AF.Exp), reads=["cst"], writes=["dtot"])
            P.add("pool", lambda e: e.tensor_tensor(out=dte, in0=cst[:, 8:16], in1=cst[:, 0:8], op=ALU.subtract),
                  reads=["cst"], writes=["dte"])
            P.add("act", lambda e: e.activation(out=dte, in_=dte, func=AF.Exp), reads=["dte"], writes=["dte"])
            P.add("pool", lambda e: e.tensor_scalar(out=negcs, in0=cst[:, 0:8], scalar1=-1.0, scalar2=1.0, op0=ALU.mult,
                                                   op1=ALU.mult),
                  reads=["cst"], writes=["negcs"])
            Ub = consts[:, CO_U:CO_U + 128].unsqueeze(1).broadcast_to([128, 8, 128])
            ab = a_sb.unsqueeze(2).broadcast_to([128, 8, 128])
            P.add("pool", lambda e, Ub=Ub, ab=ab: e.tensor_tensor(out=rL, in0=Ub, in1=ab, op=ALU.mult),
                  reads=["consts", "a_sb"], writes=["rL"])
            for b2 in range(2):
                P.add("pe", lambda e, b2=b2: e.matmul(out=ps[4 + b2][:], lhsT=consts[:, CO_ONES:CO_ONES + 128],
                                                      rhs=rL[:, 4 * b2:4 * b2 + 4, :], start=True, stop=False),
                      reads=["consts", "rL"], writes=[("ps", 4 + b2)])
                P.add("pe", lambda e, b2=b2: e.matmul(out=ps[4 + b2][:], lhsT=ident_bf, rhs=flat(negl4_bf),
                                                      start=False, stop=True),
                      reads=["ident_bf", "negl4"], writes=[("ps", 4 + b2)])
            for h in range(8):
                b2, hh = h // 4, h % 4
                P.add("act", lambda e, h=h, b2=b2, hh=hh: e.activation(
                    out=rL[:, h, :], in_=ps[4 + b2][:, hh * 128:(hh + 1) * 128], func=AF.Exp,
                    bias=negcs[:, h:h + 1]),
                    reads=[("ps", 4 + b2), "negcs"], writes=["rL"])
            for b2 in range(2):
                gtb = GT_sb[:, b2, :].unsqueeze(1).broadcast_to([128, 4, 128])
                P.add("pool", lambda e, b2=b2, gtb=gtb: e.tensor_tensor(out=WT[:, 4 * b2:4 * b2 + 4, :],
                                                                       in0=rL[:, 4 * b2:4 * b2 + 4, :], in1=gtb,
                                                                       op=ALU.mult),
                      reads=["rL", "GT_sb"], writes=[("WT", b2)])
            dtb = dt_sb.unsqueeze(2).broadcast_to([128, 8, 64])
            dteb = dte.unsqueeze(2).broadcast_to([128, 8, 64])
            P.add("pool", lambda e, dtb=dtb: e.tensor_tensor(out=X_sb, in0=xs_tok, in1=dtb, op=ALU.mult),
                  reads=["xs_tok", "dt_sb"], writes=["X_sb"])
            P.add("pool", lambda e, dteb=dteb: e.tensor_tensor(out=Xd_sb, in0=X_sb, in1=dteb, op=ALU.mult),
                  reads=["X_sb", "dte"], writes=["Xd_sb"])
            for h in range(8):
                P.add("pe", lambda e, h=h: e.matmul(out=ps[6][:, h * 64:(h + 1) * 64], lhsT=WT[:, h, :], rhs=X_sb[:, h, :],
                                                    start=True, stop=True),
                      reads=[("WT", h // 4), "X_sb"], writes=[("ps", 6)])
            for g in range(2):
                P.add("pe", lambda e, g=g: e.matmul(out=ps[7][:, g * 256:(g + 1) * 256], lhsT=BCT[:, 2 + g, :],
                                                    rhs=Hbf[:, 4 * g:4 * g + 4, :], start=True, stop=True),
                      reads=["BCT", "Hbf"], writes=[("ps", 7)])
            P.add("act", lambda e: e.activation(out=flat(yoff_sb), in_=ps[7][:], func=AF.Copy),
                  reads=[("ps", 7)], writes=["yoff_sb"])
            ecsb = ecs.unsqueeze(2).broadcast_to([128, 8, 64])
            dskb = rows[:, R_DSKIP:R_DSKIP + 8].unsqueeze(2).broadcast_to([128, 8, 64])
            P.add("pool", lambda e, ecsb=ecsb: e.tensor_tensor(out=yoff_sb, in0=yoff_sb, in1=ecsb, op=ALU.mult),
                  reads=["yoff_sb", "ecs"], writes=["yoff_sb"])
            P.add("pool", lambda e, dskb=dskb: e.tensor_tensor(out=t2_sb, in0=xs_tok, in1=dskb, op=ALU.mult),
                  reads=["xs_tok", "rows"], writes=["t2_sb"])
            P.add("pool", lambda e: e.tensor_tensor(out=yoff_sb, in0=yoff_sb, in1=t2_sb, op=ALU.add),
                  reads=["yoff_sb", "t2_sb"], writes=["yoff_sb"])
            P.add("act", lambda e: e.activation(out=y_sb, in_=ps[6][:], func=AF.Copy),
                  reads=[("ps", 6)], writes=["y_sb"])
            P.add("pool", lambda e: e.tensor_tensor(out=y_sb, in0=y_sb, in1=flat(yoff_sb), op=ALU.add),
                  reads=["y_sb", "yoff_sb"], writes=["y_sb"])
            P.add("pool", lambda e, k2=k2: e.tensor_tensor(out=y_sb, in0=y_sb, in1=sz[k2], op=ALU.mult),
                  reads=["y_sb", ("sz", k2)], writes=["y_sb"])
            for g in range(2):
                P.add("act", lambda e, g=g: e.activation(out=flat(t2_sb)[:, g * 256:(g + 1) * 256],
                                                         in_=y_sb[:, g * 256:(g + 1) * 256],
                                                         func=AF.Square, accum_out=ssg[:, g:g + 1]),
                      reads=["y_sb"], writes=[("ssg", g), "t2_sb"])
            P.add("act", lambda e: e.activation(out=sdg, in_=ssg, func=AF.Ln, scale=1.0 / 256,
                                                bias=pp[:, PP_EPS:PP_EPS + 1]),
                  reads=[("ssg", 0), ("ssg", 1), "pp"], writes=["sdg"])
            P.add("act", lambda e: e.activation(out=rsg, in_=sdg, func=AF.Exp, scale=-0.5), reads=["sdg"], writes=["rsg"])
            for g in range(2):
                P.add("act", lambda e, g=g: e.activation(out=flat(t2_sb)[:, g * 256:(g + 1) * 256],
                                                         in_=y_sb[:, g * 256:(g + 1) * 256], func=AF.Copy,
                                                         scale=rsg[:, g:g + 1]),
                      reads=["y_sb", "rsg"], writes=["t2_sb"])
                P.add("pool", lambda e, g=g: e.tensor_tensor(
                    out=ssd_tok[:, g * 256:(g + 1) * 256], in0=flat(t2_sb)[:, g * 256:(g + 1) * 256],
                    in1=rows[:, R_SSDNW + g * 256:R_SSDNW + (g + 1) * 256], op=ALU.mult),
                    reads=["t2_sb", "rows"], writes=["ssd_tok"])
            pT7 = ps[7][:].bitcast(BF16)
            for c4 in range(4):
                P.add("pe", lambda e, c4=c4: e.transpose(out=pT7[:, c4 * 128:(c4 + 1) * 128],
                                                         in_=ssd_tok[:, c4 * 128:(c4 + 1) * 128], identity=ident_bf),
                      reads=["ssd_tok", "ident_bf"], writes=[("ps", 7)])
            P.add("act", lambda e, i=i: e.activation(out=mixT[:, 4:8, i * 128:(i + 1) * 128],
                                                     in_=pT7[:, 0:512].rearrange("p (a b) -> p a b", a=4), func=AF.Copy),
                  reads=[("ps", 7)], writes=[("mixT_s", i)])
            for g in range(2):
                P.add("pe", lambda e, g=g: e.matmul(out=ps[0][:, g * 256:(g + 1) * 256], lhsT=B_tok[:, g * 128:(g + 1) * 128],
                                                    rhs=Xd_sb[:, 4 * g:4 * g + 4, :], start=True, stop=True),
                      reads=["B_tok", "Xd_sb"], writes=[("ps", 0)])
            dtotb = dtot.unsqueeze(2).broadcast_to([128, 8, 64])
            P.add("pool", lambda e, dtotb=dtotb: e.tensor_tensor(out=Ht_sb, in0=H_sb, in1=dtotb, op=ALU.mult),
                  reads=["H", "dtot"], writes=["t2_sb"])
            P.add("act", lambda e: e.activation(out=flat(yoff_sb), in_=ps[0][:], func=AF.Copy),
                  reads=[("ps", 0)], writes=["yoff_sb"])
            P.add("pool", lambda e: e.tensor_tensor(out=flat(H_sb), in0=flat(yoff_sb), in1=flat(Ht_sb), op=ALU.add),
                  reads=["yoff_sb", "t2_sb"], writes=["H"])
            P.add("act", lambda e: e.activation(out=flat(Hbf), in_=flat(H_sb), func=AF.Copy),
                  reads=["H"], writes=["Hbf"])
            if len(pend_attn) == 2:
                pend_attn.pop(0)()
            P.bg_flush()
            while finalizers:
                finalizers.pop(0)()
        P.bg_flush()
        while finalizers:
            finalizers.pop(0)()
        if dbg is not None and s == 0:
            dump("negm", negm, [128, L], reads=["negm"])
            dump("S", S2[(nt - 1) % 2], [128, L], reads=[("S", (nt - 1) % 2, c) for c in range((L + 511) // 512)])
            dump("thf", small[:, 160:164], [128, 4], reads=["thf", "negth", "sgn", "sg2"])
        while pend_attn:
            pend_attn.pop(0)()
        if dbg is not None and s == 0:
            dump("mixT", mixT, [128, 8, L], reads=[("mixT_a", jj) for jj in range(nt)] + [("mixT_s", jj) for jj in range(nt)])
        barrier()
        if stop_after == "M":
            continue

        dma("sp", wo_sb, wo_bf, [], ["wo_sb"], "wo_sb")
        dma("sp", gpm, gpost_d[0:1, :].partition_broadcast(128), [], ["gpm"], "gpm")
        dma("sp", gpl, gpost_d[1:2, :].partition_broadcast(128), [], ["gpl"], "gpl")
        for c in range(nt // 4):
            for tt in range(4):
                i = 4 * c + tt
                k2 = i % 2
                r0 = s * L + i * 128
                if tt < 2:
                    dma("sp", xt[k2], x_d[r0:r0 + 128, :], [], [("xt", k2)], f"xt{k2}")
                for nh in range(2):
                    bk = 2 * tt + nh
                    for cc in range(8):
                        P.add("pe", lambda e, nh=nh, cc=cc, i=i, bk=bk: e.matmul(
                            out=ps[bk][:], lhsT=mixT[:, cc, i * 128:(i + 1) * 128],
                            rhs=wo_sb[:, cc, nh * 512:(nh + 1) * 512], start=(cc == 0), stop=(cc == 7)),
                            reads=[("mixT_a", i), ("mixT_s", i), "wo_sb"], writes=[("ps", bk)])
            for tt in range(4):
                i = 4 * c + tt
                k2 = i % 2
                for nh in range(2):
                    bk = 2 * tt + nh
                    P.add("act", lambda e, nh=nh, k2=k2, bk=bk: e.activation(
                        out=junkF[:, nh * 512:(nh + 1) * 512], in_=ps[bk][:], func=AF.Square,
                        accum_out=ssa[k2][:, nh:nh + 1]),
                        reads=[("ps", bk)], writes=[("ssa", k2, nh), ("junkF", nh)])
                P.add("dve", lambda e, k2=k2: e.tensor_tensor(out=ssq[k2], in0=ssa[k2][:, 0:1], in1=ssa[k2][:, 1:2],
                                                              op=ALU.add),
                      reads=[("ssa", k2, 0), ("ssa", k2, 1)], writes=[("ssq", k2)])
                P.add("act", lambda e, k2=k2: e.activation(out=sq2[k2], in_=ssq[k2], func=AF.Sqrt, scale=1.0 / 1024,
                                                           bias=pp[:, PP_EPS:PP_EPS + 1]),
                      reads=[("ssq", k2), "pp"], writes=[("sq2", k2)])
                P.add("dve", lambda e, k2=k2: e.reciprocal(out=r1[k2], in_=sq2[k2]), reads=[("sq2", k2)],
                      writes=[("r1", k2)])
                for nh in range(2):
                    bk = 2 * tt + nh
                    P.add("dve", lambda e, nh=nh, k2=k2, tt=tt, bk=bk: e.scalar_tensor_tensor(
                        out=h1[:, tt, nh * 512:(nh + 1) * 512], in0=ps[bk][:], scalar=r1[k2],
                        in1=gpm[:, nh * 512:(nh + 1) * 512], op0=ALU.mult, op1=ALU.mult),
                        reads=[("ps", bk), ("r1", k2), "gpm"], writes=[("h1", tt)])
                P.add("pool", lambda e, k2=k2, tt=tt: e.tensor_tensor(out=h1[:, tt, :], in0=h1[:, tt, :], in1=xt[k2],
                                                                      op=ALU.add),
                      reads=[("h1", tt), ("xt", k2)], writes=[("h1", tt)])
                if tt < 2:
                    r2_ = s * L + (i + 2) * 128
                    dma("sp", xt[k2], x_d[r2_:r2_ + 128, :], [], [("xt", k2)], f"xt{k2}")
                P.add("act", lambda e, k2=k2, tt=tt: e.activation(out=hn[k2], in_=h1[:, tt, :], func=AF.Square,
                                                                  accum_out=ss2[k2]),
                      reads=[("h1", tt)], writes=[("ss2", k2), ("hn", k2)])
                P.add("act", lambda e, k2=k2: e.activation(out=sd2[k2], in_=ss2[k2], func=AF.Sqrt, scale=1.0 / 1024,
                                                           bias=pp[:, PP_EPS:PP_EPS + 1]),
                      reads=[("ss2", k2), "pp"], writes=[("sd2", k2)])
                P.add("dve", lambda e, k2=k2: e.reciprocal(out=r2[k2], in_=sd2[k2]), reads=[("sd2", k2)],
                      writes=[("r2", k2)])
                P.add("act", lambda e, k2=k2, tt=tt: e.activation(out=hn[k2], in_=h1[:, tt, :], func=AF.Copy,
                                                                  scale=r2[k2]),
                      reads=[("h1", tt), ("r2", k2)], writes=[("hn", k2)])
                bkT = 2 * tt
                pT2 = ps[bkT][:].bitcast(BF16)
                for kc in range(8):
                    P.add("pe", lambda e, k2=k2, kc=kc, pT2=pT2: e.transpose(out=pT2[:, kc * 128:(kc + 1) * 128],
                                                                              in_=hn[k2][:, kc * 128:(kc + 1) * 128],
                                                                              identity=ident_bf),
                          reads=[("hn", k2), "ident_bf"], writes=[("ps", bkT)])
                P.add("act", lambda e, tt=tt, pT2=pT2: e.activation(out=hnT[:, :, tt * 128:(tt + 1) * 128],
                                                                    in_=pT2.rearrange("p (a b) -> p a b", a=8), func=AF.Copy),
                      reads=[("ps", bkT)], writes=[("hnT", tt)])
            ub = 0
            for fg in range(8):
                wk = fg % 2
                dma("sp", wu_sb[wk], wu_bf[:, :, fg * 512:(fg + 1) * 512], [], [("wu", wk)], f"wu{wk}")
                for f4 in range(4):
                    fc = fg * 4 + f4
                    bank = 3 + (ub % 4)
                    rk = ub % 2
                    ub += 1
                    for kc in range(8):
                        P.add("pe", lambda e, wk=wk, f4=f4, kc=kc, bank=bank: e.matmul(
                            out=ps[bank][:], lhsT=wu_sb[wk][:, kc, f4 * 128:(f4 + 1) * 128], rhs=hnT[:, kc, :],
                            start=(kc == 0), stop=(kc == 7)),
                            reads=[("wu", wk)] + [("hnT", t4) for t4 in range(4)], writes=[("ps", bank)])
                    P.add("act", lambda e, bank=bank, rk=rk: e.activation(out=r32[rk], in_=ps[bank][:], func=AF.Relu),
                          reads=[("ps", bank)], writes=[("r32", rk)])
                    P.add("pool", lambda e, fc=fc, rk=rk: e.tensor_tensor(out=aT[:, fc, :], in0=r32[rk], in1=r32[rk],
                                                                          op=ALU.mult),
                          reads=[("r32", rk)], writes=[("aT", fc)])
            for dg in range(8):
                wk = dg % 2
                dma("sp", wd_sb[wk], wd_bf[:, dg * 4:(dg + 1) * 4, :], [], [("wd", wk)], f"wd{wk}")
                for tt in range(4):
                    for nh in range(2):
                        for f4 in range(4):
                            fc = dg * 4 + f4
                            P.add("pe", lambda e, wk=wk, tt=tt, nh=nh, f4=f4, fc=fc, dg=dg: e.matmul(
                                out=ps[2 * tt + nh][:], lhsT=aT[:, fc, tt * 128:(tt + 1) * 128],
                                rhs=wd_sb[wk][:, f4, nh * 512:(nh + 1) * 512],
                                start=(dg == 0 and f4 == 0), stop=(dg == 7 and f4 == 3)),
                                reads=[("wd", wk), ("aT", fc)], writes=[("ps", 2 * tt + nh)])
            for tt in range(4):
                i = 4 * c + tt
                k2 = i % 2
                r0 = s * L + i * 128
                for nh in range(2):
                    P.add("act", lambda e, nh=nh, k2=k2, tt=tt: e.activation(
                        out=junkF[:, nh * 512:(nh + 1) * 512], in_=ps[2 * tt + nh][:], func=AF.Square,
                        accum_out=ssc[k2][:, nh:nh + 1]),
                        reads=[("ps", 2 * tt + nh)], writes=[("ssc", k2, nh), ("junkF", nh)])
                P.add("dve", lambda e, k2=k2: e.tensor_tensor(out=ss3[k2], in0=ssc[k2][:, 0:1], in1=ssc[k2][:, 1:2],
                                                              op=ALU.add),
                      reads=[("ssc", k2, 0), ("ssc", k2, 1)], writes=[("ss3", k2)])
                P.add("act", lambda e, k2=k2: e.activation(out=sd3[k2], in_=ss3[k2], func=AF.Sqrt, scale=1.0 / 1024,
                                                           bias=pp[:, PP_EPS:PP_EPS + 1]),
                      reads=[("ss3", k2), "pp"], writes=[("sd3", k2)])
                P.add("dve", lambda e, k2=k2: e.reciprocal(out=r3[k2], in_=sd3[k2]), reads=[("sd3", k2)],
                      writes=[("r3", k2)])
                for nh in range(2):
                    P.add("dve", lambda e, nh=nh, k2=k2, tt=tt: e.scalar_tensor_tensor(
                        out=ot[k2][:, nh * 512:(nh + 1) * 512], in0=ps[2 * tt + nh][:], scalar=r3[k2],
                        in1=gpl[:, nh * 512:(nh + 1) * 512], op0=ALU.mult, op1=ALU.mult),
                        reads=[("ps", 2 * tt + nh), ("r3", k2), "gpl"], writes=[("ot", k2)])
                P.add("pool", lambda e, k2=k2, tt=tt: e.tensor_tensor(out=ot[k2], in0=ot[k2], in1=h1[:, tt, :], op=ALU.add),
                      reads=[("ot", k2), ("h1", tt)], writes=[("ot", k2)])
                dma("sp", out_d[r0:r0 + 128, :], ot[k2], [("ot", k2)], [("ot", k2)], f"ot{k2}")
        barrier()

    final_keys = ["dbg_" + n for n in dbg_out] + ["ot0", "ot1"]
    P.emit(es, final_wait_keys=final_keys)
    es.close()
    if dbg is not None:
        dbg.update(dbg_out)
    return nc


PP_GMIX = 0
PP_GMLP = 8
PP_EPS = 16
PP_LNW = 17
PP_LNB = 18
PP_CONVW = 19
PP_CONVB = 51
PP_N = 59
R_DTB = 0
R_ALOG = 8
R_DSKIP = 16
R_SSDNW = 24
ROWS_N = 536


def _host_inputs(inputs, core, nseq, L):
    f = lambda a: np.ascontiguousarray(np.asarray(a, dtype=np.float32))
    x = f(inputs["x"])
    xs = x[core * nseq:(core + 1) * nseq, :L].reshape(nseq * L, D_MODEL)
    w_in = f(inputs["w_in"])[0][:, _w_in_perm()]
    pp = np.zeros((128, PP_N), np.float32)
    pp[:, PP_GMIX:PP_GMIX + 8] = f(inputs["norm_pre_mix"])[0].reshape(8, 128).T
    pp[:, PP_GMLP:PP_GMLP + 8] = f(inputs["norm_pre_mlp"])[0].reshape(8, 128).T
    pp[:, PP_EPS] = EPS
    pp[:, PP_LNW] = np.tile(f(inputs["k_idx_ln_w"])[0], 2)
    pp[:, PP_LNB] = np.tile(f(inputs["k_idx_ln_b"])[0], 2)
    cw = f(inputs["conv_w"])[0]
    pp[:, PP_CONVW:PP_CONVW + 32] = cw.reshape(4, 8, 128).transpose(2, 1, 0).reshape(128, 32)
    pp[:, PP_CONVB:PP_CONVB + 8] = f(inputs["conv_b"])[0].reshape(8, 128).T
    rows = np.zeros((1, ROWS_N), np.float32)
    rows[0, R_DTB:R_DTB + 8] = f(inputs["dt_bias"])[0]
    rows[0, R_ALOG:R_ALOG + 8] = f(inputs["a_log"])[0]
    rows[0, R_DSKIP:R_DSKIP + 8] = f(inputs["d_skip"])[0]
    rows[0, R_SSDNW:R_SSDNW + 512] = f(inputs["ssd_norm_w"])[0]
    return {
        "x": np.ascontiguousarray(xs),
        "w_in": np.ascontiguousarray(w_in),
        "w_out": f(inputs["w_out"])[0],
        "w_up": f(inputs["w_mlp_up"])[0],
        "w_down": f(inputs["w_mlp_down"])[0],
        "consts": _consts(),
        "pp": pp,
        "rows": rows,
        "gpost": np.ascontiguousarray(np.stack([f(inputs["norm_post_mix"])[0], f(inputs["norm_post_mlp"])[0]], 0)),
        "convb": f(inputs["conv_b"])[0].reshape(1, 1024),
        "rel_bias": f(inputs["rel_bias"]),
    }


def kernel(**inputs):
    nseq, nt = 2, 16
    nc = build(nseq=nseq, nt=nt)
    in_maps = [_host_inputs(inputs, c, nseq, nt * 128) for c in range(N_CORES)]
    res = run_bass_kernel_spmd(nc, in_maps, core_ids=list(range(N_CORES)))
    outs = [np.asarray(r["out"]).reshape(nseq, nt * 128, D_MODEL) for r in res.results]
    return np.concatenate(outs, axis=0).astype(np.float32)
```
